# Optimizing a Trainium2 kernel written in Bass

```python
import jax
import jax.numpy as jnp
from jax import lax
import numpy as np

D_MODEL = 1024
BATCH = 1
SEQ = 16384
DEPTH = 2
DEC_BATCH = 32
DEC_SEQ = 16
PAST_LEN = 4096

CHUNK = 64
N_MEM = 256
HEAD_DIM = 64
A_HEADS = 8
A_WIDTH = A_HEADS * HEAD_DIM
A_PREV_CHUNKS = 8
A_WINDOW = A_PREV_CHUNKS * CHUNK
A_BAND = (A_PREV_CHUNKS + 1) * CHUNK
REL_CLIP = 128
POOL_WINDOWS = (2, 4, 8, 16)
B_WIDTH = D_MODEL - A_WIDTH
B_GROUP = B_WIDTH // len(POOL_WINDOWS)
POOL_STATE = max(POOL_WINDOWS) - 1
C_HEADS = D_MODEL // HEAD_DIM
C_WIDTH = C_HEADS * HEAD_DIM
Q_BLOCK = 128
FORGET_BIAS = 4.0
X_HEADS = 4
X_HEAD_DIM = D_MODEL // X_HEADS
D_FF = 2816
N_EXPERTS = 8
TOP_K = 2
N_PAIRS = DEPTH // 2
ALPHA = (2.0 * DEPTH) ** 0.25
BETA = (8.0 * DEPTH) ** -0.25
LN_EPS = 1e-5
NEG_INF = -1e30

kernel_name = 'streaming_hybrid_band_pool_fox_step'


def layer_norm(x, g, b):
    xf = x.astype(jnp.float32)
    mu = jnp.mean(xf, axis=-1, keepdims=True)
    var = jnp.mean(jnp.square(xf - mu), axis=-1, keepdims=True)
    return ((xf - mu) * lax.rsqrt(var + LN_EPS)).astype(x.dtype) * g + b


def deepnorm_residual(x, sub, g, b):
    return layer_norm(ALPHA * x + sub, g, b)


def rel_index(d):
    return jnp.clip(d, -REL_CLIP, REL_CLIP) + REL_CLIP


def band_attention_prompt(q, k, v, rel_table):
    B, S, H, Dh = q.shape
    nc = S // CHUNK
    qc = q.reshape(B, nc, CHUNK, H, Dh)
    pad = jnp.zeros((B, A_WINDOW, H, Dh), k.dtype)
    kp = jnp.concatenate([pad, k], 1).reshape(B, nc + A_PREV_CHUNKS, CHUNK, H, Dh)
    vp = jnp.concatenate([pad, v], 1).reshape(B, nc + A_PREV_CHUNKS, CHUNK, H, Dh)
    kb = jnp.concatenate([kp[:, j:j + nc] for j in range(A_PREV_CHUNKS + 1)], axis=2)
    vb = jnp.concatenate([vp[:, j:j + nc] for j in range(A_PREV_CHUNKS + 1)], axis=2)
    q_off = jnp.arange(CHUNK)
    k_off = jnp.arange(A_BAND) - A_WINDOW
    bias = rel_table[:, rel_index(q_off[:, None] - k_off[None, :])].astype(jnp.float32)
    valid = (jnp.arange(nc)[:, None] * CHUNK + k_off[None, :]) >= 0
    s = jnp.einsum('bcqhd,bckhd->bchqk', qc, kb).astype(jnp.float32) * (Dh ** -0.5)
    s = jnp.where(valid[None, :, None, None, :], s + bias, NEG_INF)
    p = jax.nn.softmax(s, axis=-1).astype(v.dtype)
    o = jnp.einsum('bchqk,bckhd->bcqhd', p, vb)
    return o.reshape(B, S, H * Dh)


def band_attention_sample(q, k, v, k_cache, v_cache, rel_table, past_len):
    B, T, H, Dh = q.shape
    W = k_cache.shape[1]
    kk = jnp.concatenate([k_cache, k], 1)
    vv = jnp.concatenate([v_cache, v], 1)
    q_pos = past_len + jnp.arange(T)
    k_pos = past_len - W + jnp.arange(W + T)
    bias = rel_table[:, rel_index(q_pos[:, None] - k_pos[None, :])].astype(jnp.float32)
    s = jnp.einsum('bqhd,bkhd->bhqk', q, kk).astype(jnp.float32) * (Dh ** -0.5) + bias
    p = jax.nn.softmax(s, axis=-1).astype(vv.dtype)
    o = jnp.einsum('bhqk,bkhd->bqhd', p, vv)
    return o.reshape(B, T, H * Dh)


def multiscale_pool(u, u_hist, pos0, pool_w, pool_scale):
    B, T, _ = u.shape
    P = u_hist.shape[1]
    ue = jnp.concatenate([u_hist, u], 1).astype(jnp.float32)
    cs = jnp.pad(jnp.cumsum(ue, axis=1), ((0, 0), (POOL_STATE + 1, 0), (0, 0)))
    pos = pos0 + jnp.arange(T).astype(jnp.float32)
    off = POOL_STATE + 1 + P
    outs = []
    for g, w in enumerate(POOL_WINDOWS):
        sl = slice(g * B_GROUP, (g + 1) * B_GROUP)
        win = cs[:, off:off + T, sl] - cs[:, off - w:off - w + T, sl]
        cnt = jnp.minimum(float(w), pos + 1.0)
        outs.append(win / cnt[None, :, None] - ue[:, P:, sl])
    d = jnp.stack(outs, axis=2).astype(u.dtype)
    y = jnp.einsum('btgc,gce->btge', d, pool_w).reshape(B, T, B_WIDTH)
    return y * pool_scale


def forgetting_attention_prompt(q, k, v, logf):
    B, S, H, Dh = q.shape
    dcum = jnp.cumsum(logf, axis=1).transpose(0, 2, 1)
    k_pos = jnp.arange(S)

    def block(i):
        q0 = i * Q_BLOCK
        qb = lax.dynamic_slice_in_dim(q, q0, Q_BLOCK, axis=1)
        dq = lax.dynamic_slice_in_dim(dcum, q0, Q_BLOCK, axis=2)
        s = jnp.einsum('bqhd,bkhd->bhqk', qb, k).astype(jnp.float32) * (Dh ** -0.5)
        s = s + dq[..., :, None] - dcum[..., None, :]
        q_pos = q0 + jnp.arange(Q_BLOCK)
        s = jnp.where(k_pos[None, :] <= q_pos[:, None], s, NEG_INF)
        p = jax.nn.softmax(s, axis=-1).astype(v.dtype)
        return jnp.einsum('bhqk,bkhd->bqhd', p, v)

    o = lax.map(block, jnp.arange(S // Q_BLOCK))
    return o.transpose(1, 0, 2, 3, 4).reshape(B, S, H * Dh)


def forgetting_attention_sample(q, k, v, logf, k_cache, v_cache, lf_cache):
    B, T, H, Dh = q.shape
    P = k_cache.shape[1]
    kk = jnp.concatenate([k_cache, k], 1)
    vv = jnp.concatenate([v_cache, v], 1)
    dcum = jnp.cumsum(jnp.concatenate([lf_cache.astype(jnp.float32), logf], 1), axis=1).transpose(0, 2, 1)
    s = jnp.einsum('bqhd,bkhd->bhqk', q, kk).astype(jnp.float32) * (Dh ** -0.5)
    s = s + dcum[..., P:, None] - dcum[..., None, :]
    mask = jnp.arange(P + T)[None, :] <= (P + jnp.arange(T))[:, None]
    s = jnp.where(mask, s, NEG_INF)
    p = jax.nn.softmax(s, axis=-1).astype(vv.dtype)
    o = jnp.einsum('bhqk,bkhd->bqhd', p, vv)
    return o.reshape(B, T, H * Dh)


def mixer_ab(x, cache, past_len, w_in, rel_table, pool_w, pool_scale, w_out):
    B, T, _ = x.shape
    h = x @ w_in
    q = h[..., :A_WIDTH].reshape(B, T, A_HEADS, HEAD_DIM)
    k = h[..., A_WIDTH:2 * A_WIDTH].reshape(B, T, A_HEADS, HEAD_DIM)
    v = h[..., 2 * A_WIDTH:3 * A_WIDTH].reshape(B, T, A_HEADS, HEAD_DIM)
    u = h[..., 3 * A_WIDTH:]
    if cache is None:
        o_a = band_attention_prompt(q, k, v, rel_table)
        o_b = multiscale_pool(u, u[:, :0], 0, pool_w, pool_scale)
        keep = min(A_WINDOW, T)
        new = (k[:, T - keep:], v[:, T - keep:], u[:, T - POOL_STATE:])
    else:
        k_cache, v_cache, u_hist = cache
        o_a = band_attention_sample(q, k, v, k_cache, v_cache, rel_table, past_len)
        o_b = multiscale_pool(u, u_hist, past_len, pool_w, pool_scale)
        new = (k, v, jnp.concatenate([u_hist, u], 1)[:, -POOL_STATE:])
    y = jnp.concatenate([o_a, o_b], axis=-1) @ w_out
    return y, new


def mixer_c(x, cache, w_in, b_f, w_out):
    B, T, _ = x.shape
    h = x @ w_in
    q = h[..., :C_WIDTH].reshape(B, T, C_HEADS, HEAD_DIM)
    k = h[..., C_WIDTH:2 * C_WIDTH].reshape(B, T, C_HEADS, HEAD_DIM)
    v = h[..., 2 * C_WIDTH:3 * C_WIDTH].reshape(B, T, C_HEADS, HEAD_DIM)
    logf = jax.nn.log_sigmoid(h[..., 3 * C_WIDTH:].astype(jnp.float32) + b_f.astype(jnp.float32))
    if cache is None:
        o = forgetting_attention_prompt(q, k, v, logf)
    else:
        k_cache, v_cache, lf_cache = cache
        o = forgetting_attention_sample(q, k, v, logf, k_cache, v_cache, lf_cache)
    return o @ w_out, (k, v, logf)


def memory_cross_attention(x, mem_k, mem_v, w_q, w_o):
    B, T, _ = x.shape
    q = (x @ w_q).reshape(B, T, X_HEADS, X_HEAD_DIM)
    s = jnp.einsum('bqhd,bmhd->bhqm', q, mem_k).astype(jnp.float32) * (X_HEAD_DIM ** -0.5)
    p = jax.nn.softmax(s, axis=-1).astype(mem_v.dtype)
    o = jnp.einsum('bhqm,bmhd->bqhd', p, mem_v).reshape(B, T, D_MODEL)
    return o @ w_o


def swiglu(x, w1, w3, w2):
    return (jax.nn.silu(x @ w1) * (x @ w3)) @ w2


def moe_swiglu(x, w_router, b_router, w1, w3, w2):
    logits = (x @ w_router).astype(jnp.float32) + b_router.astype(jnp.float32)
    top_val, top_idx = lax.top_k(logits, TOP_K)
    gates = jax.nn.softmax(top_val, axis=-1)
    comb = jnp.einsum('btk,btke->bte', gates, jax.nn.one_hot(top_idx, N_EXPERTS, dtype=jnp.float32)).astype(x.dtype)
    y = jnp.zeros_like(x)
    for e in range(N_EXPERTS):
        y = y + comb[..., e:e + 1] * swiglu(x, w1[e], w3[e], w2[e])
    return y


def trunk(x, mem_k, mem_v, caches, past_len, w_in_ab, rel_bias_a, pool_w, pool_scale, w_out_ab,
          w_in_c, b_f, w_out_c, w_xq, w_xo, ln_g, ln_b, ffn_w1, ffn_w3, ffn_w2,
          w_router, b_router, moe_w1, moe_w3, moe_w2):
    a_k, a_v, pool_st, c_k, c_v, c_lf = [], [], [], [], [], []
    for layer in range(DEPTH):
        p = layer // 2
        if layer % 2 == 0:
            cache = None if caches is None else (caches[0][p], caches[1][p], caches[2][p])
            y, (nk, nv, nu) = mixer_ab(x, cache, past_len, w_in_ab[p], rel_bias_a[p], pool_w[p], pool_scale[p], w_out_ab[p])
            a_k.append(nk)
            a_v.append(nv)
            pool_st.append(nu)
        else:
            cache = None if caches is None else (caches[3][p], caches[4][p], caches[5][p])
            y, (nk, nv, nl) = mixer_c(x, cache, w_in_c[p], b_f[p], w_out_c[p])
            c_k.append(nk)
            c_v.append(nv)
            c_lf.append(nl)
        x = deepnorm_residual(x, y, ln_g[layer, 0], ln_b[layer, 0])
        x = deepnorm_residual(x, memory_cross_attention(x, mem_k[layer], mem_v[layer], w_xq[layer], w_xo[layer]),
                              ln_g[layer, 1], ln_b[layer, 1])
        if layer % 2 == 0:
            f = swiglu(x, ffn_w1[p], ffn_w3[p], ffn_w2[p])
        else:
            f = moe_swiglu(x, w_router[p], b_router[p], moe_w1[p], moe_w3[p], moe_w2[p])
        x = deepnorm_residual(x, f, ln_g[layer, 2], ln_b[layer, 2])
    states = (jnp.stack(a_k), jnp.stack(a_v), jnp.stack(pool_st), jnp.stack(c_k), jnp.stack(c_v), jnp.stack(c_lf))
    return x, states


def setup_inputs(seed: int = 0) -> dict:
    key = jax.random.key(seed)
    ks = iter(jax.random.split(key, 48))

    def nrm(shape, scale=1.0):
        return jax.random.normal(next(ks), shape, jnp.float32) * scale

    a_rows = min(A_WINDOW, PAST_LEN)
    d_in = D_MODEL ** -0.5
    return {
        'x_prompt': nrm((BATCH, SEQ, D_MODEL)),
        'x_sample': nrm((DEC_BATCH, DEC_SEQ, D_MODEL)),
        'cache_a_k': nrm((N_PAIRS, DEC_BATCH, a_rows, A_HEADS, HEAD_DIM)),
        'cache_a_v': nrm((N_PAIRS, DEC_BATCH, a_rows, A_HEADS, HEAD_DIM)),
        'state_pool': nrm((N_PAIRS, DEC_BATCH, POOL_STATE, B_WIDTH)),
        'cache_c_k': nrm((N_PAIRS, DEC_BATCH, PAST_LEN, C_HEADS, HEAD_DIM)),
        'cache_c_v': nrm((N_PAIRS, DEC_BATCH, PAST_LEN, C_HEADS, HEAD_DIM)),
        'cache_c_logf': jax.nn.log_sigmoid(FORGET_BIAS + nrm((N_PAIRS, DEC_BATCH, PAST_LEN, C_HEADS))),
        'cache_mem_k': nrm((DEPTH, DEC_BATCH, N_MEM, X_HEADS, X_HEAD_DIM)),
        'cache_mem_v': nrm((DEPTH, DEC_BATCH, N_MEM, X_HEADS, X_HEAD_DIM)),
        'mem_prompt': nrm((BATCH, N_MEM, D_MODEL)),
        'w_in_ab': nrm((N_PAIRS, D_MODEL, 3 * A_WIDTH + B_WIDTH), d_in),
        'rel_bias_a': nrm((N_PAIRS, A_HEADS, 2 * REL_CLIP + 1), 0.2),
        'pool_w': nrm((N_PAIRS, len(POOL_WINDOWS), B_GROUP, B_GROUP), B_GROUP ** -0.5),
        'pool_scale': 1.0 + nrm((N_PAIRS, B_WIDTH), 0.02),
        'w_out_ab': nrm((N_PAIRS, D_MODEL, D_MODEL), d_in * BETA),
        'w_in_c': nrm((N_PAIRS, D_MODEL, 3 * C_WIDTH + C_HEADS), d_in),
        'b_f': FORGET_BIAS + nrm((N_PAIRS, C_HEADS), 0.5),
        'w_out_c': nrm((N_PAIRS, C_WIDTH, D_MODEL), C_WIDTH ** -0.5 * BETA),
        'w_xq': nrm((DEPTH, D_MODEL, D_MODEL), d_in),
        'w_xk': nrm((DEPTH, D_MODEL, D_MODEL), d_in),
        'w_xv': nrm((DEPTH, D_MODEL, D_MODEL), d_in),
        'w_xo': nrm((DEPTH, D_MODEL, D_MODEL), d_in * BETA),
        'ln_g': 1.0 + nrm((DEPTH, 3, D_MODEL), 0.02),
        'ln_b': nrm((DEPTH, 3, D_MODEL), 0.02),
        'ffn_w1': nrm((N_PAIRS, D_MODEL, D_FF), d_in),
        'ffn_w3': nrm((N_PAIRS, D_MODEL, D_FF), d_in),
        'ffn_w2': nrm((N_PAIRS, D_FF, D_MODEL), D_FF ** -0.5 * BETA),
        'w_router': nrm((N_PAIRS, D_MODEL, N_EXPERTS), d_in),
        'b_router': nrm((N_PAIRS, N_EXPERTS), 0.01),
        'moe_w1': nrm((N_PAIRS, N_EXPERTS, D_MODEL, D_FF), d_in),
        'moe_w3': nrm((N_PAIRS, N_EXPERTS, D_MODEL, D_FF), d_in),
        'moe_w2': nrm((N_PAIRS, N_EXPERTS, D_FF, D_MODEL), D_FF ** -0.5 * BETA),
    }


def reference(x_prompt, x_sample, cache_a_k, cache_a_v, state_pool, cache_c_k, cache_c_v, cache_c_logf,
              cache_mem_k, cache_mem_v, mem_prompt, w_in_ab, rel_bias_a, pool_w, pool_scale, w_out_ab,
              w_in_c, b_f, w_out_c, w_xq, w_xk, w_xv, w_xo, ln_g, ln_b, ffn_w1, ffn_w3, ffn_w2,
              w_router, b_router, moe_w1, moe_w3, moe_w2):
    bp = mem_prompt.shape[0]
    p_mem_k = jnp.einsum('bmd,lde->lbme', mem_prompt, w_xk).reshape(DEPTH, bp, N_MEM, X_HEADS, X_HEAD_DIM)
    p_mem_v = jnp.einsum('bmd,lde->lbme', mem_prompt, w_xv).reshape(DEPTH, bp, N_MEM, X_HEADS, X_HEAD_DIM)

    y_prompt, (p_a_k, p_a_v, p_pool, p_c_k, p_c_v, p_c_logf) = trunk(
        x_prompt, p_mem_k, p_mem_v, None, 0, w_in_ab, rel_bias_a, pool_w, pool_scale, w_out_ab,
        w_in_c, b_f, w_out_c, w_xq, w_xo, ln_g, ln_b, ffn_w1, ffn_w3, ffn_w2,
        w_router, b_router, moe_w1, moe_w3, moe_w2)

    past_len = cache_c_k.shape[2]
    y_sample, (s_a_k, s_a_v, s_pool, s_c_k, s_c_v, s_c_logf) = trunk(
        x_sample, cache_mem_k, cache_mem_v,
        (cache_a_k, cache_a_v, state_pool, cache_c_k, cache_c_v, cache_c_logf), past_len,
        w_in_ab, rel_bias_a, pool_w, pool_scale, w_out_ab,
        w_in_c, b_f, w_out_c, w_xq, w_xo, ln_g, ln_b, ffn_w1, ffn_w3, ffn_w2,
        w_router, b_router, moe_w1, moe_w3, moe_w2)

    return (y_prompt, y_sample, p_a_k, p_a_v, p_pool, p_c_k, p_c_v, p_c_logf, p_mem_k, p_mem_v,
            s_a_k, s_a_v, s_pool, s_c_k, s_c_v, s_c_logf)
```

```python
import contextlib
import numpy as np
import concourse.bass as bass
import concourse.mybir as mybir
from concourse.bass_utils import run_bass_kernel_spmd

F32 = mybir.dt.float32
BF16 = mybir.dt.bfloat16
ALU = mybir.AluOpType
AF = mybir.ActivationFunctionType
AX = mybir.AxisListType

NCORES = 8
D = 1024
SEQ = 16384
BLK = 2048
NBLK = 8
NS = 64
NT = BLK + NS
HALO = 512
NA = HALO + NT
DFF = 2816
ALPHA = 4.0 ** 0.25
LN_EPS = 1e-5
NEG = -30000.0
PAST = 4096
COLT = [(0, 512), (512, 512), (1024, 512), (1536, 512), (2048, 64)]
COLA = [(0, 512), (512, 512), (1024, 512), (1536, 512), (2048, 512), (2560, 64)]
STAGE = 1
import os
PARTS = set(os.environ.get("KPARTS", "mem,pool,band,sband,outproj,ln1,xattn,ln2,ffn,ln3,kv,l1").split(","))
PASSES = os.environ.get("KPASSES", "0,1,2,3,4,5,6,7,own").split(",")


class Res:
    __slots__ = ("name", "w", "r", "excl")

    def __init__(self, name, excl=False):
        self.name = name
        self.w = None
        self.r = {}
        self.excl = excl


class SkipBlock(Exception):
    pass


class Eng:
    def __init__(self, name, sem):
        self.name = name
        self.sem = sem
        self.count = 0
        self.seen = {}
        self.ops = []
        self.dma_sems = []
        self.dma_rr = 0


class FW:
    def __init__(self, nc, stack, ndma=8):
        self.nc = nc
        self.engs = {}
        self.sems = {}
        for name in ("pe", "act", "dve", "pool", "sp"):
            s = stack.enter_context(nc.semaphore("prog_" + name))
            self.sems[id(s)] = s
            self.engs[name] = Eng(name, s)
        for name in ("sp", "pool"):
            e = self.engs[name]
            for i in range(ndma):
                s = stack.enter_context(nc.semaphore(f"dma_{name}_{i}"))
                self.sems[id(s)] = s
                e.dma_sems.append([s, 0])

    def _deps(self, eng, reads, writes, skip_self_waw=False):
        need = {}

        def add(tok):
            if tok is None:
                return
            k, v = tok
            if need.get(k, 0) < v:
                need[k] = v
        for r in reads:
            add(r.w)
            if r.excl:
                for k, v in r.r.items():
                    if k != id(eng.sem):
                        add((k, v))
        for w in writes:
            if not (skip_self_waw and w.w is not None and w.w[0] == id(eng.sem)):
                add(w.w)
            for k, v in w.r.items():
                add((k, v))
        waits = []
        for k, v in need.items():
            if eng.seen.get(k, 0) < v:
                eng.seen[k] = v
                waits.append((self.sems[k], v))
        return waits

    def _commit(self, tok, reads, writes):
        for w in writes:
            w.w = tok
            w.r = {}
        for r in reads:
            if r.r.get(tok[0], 0) < tok[1]:
                r.r[tok[0]] = tok[1]

    def op(self, engname, fn, reads=(), writes=(), accum=False):
        eng = self.engs[engname]
        waits = self._deps(eng, reads, writes, skip_self_waw=accum)
        eng.count += 1
        tok = (id(eng.sem), eng.count)
        eng.ops.append((waits, fn, (eng.sem, 1)))
        self._commit(tok, reads, writes)
        return tok

    def dma(self, engname, out, in_, reads=(), writes=(), **kw):
        eng = self.engs[engname]
        waits = self._deps(eng, reads, writes)
        slot = eng.dma_sems[eng.dma_rr % len(eng.dma_sems)]
        eng.dma_rr += 1
        sem, cnt = slot
        if cnt > 0 and eng.seen.get(id(sem), 0) < cnt:
            eng.seen[id(sem)] = cnt
            waits.append((sem, cnt))
        slot[1] = cnt + 16
        tok = (id(sem), cnt + 16)

        def fn(h, out=out, in_=in_, kw=kw):
            return h.dma_start(out=out, in_=in_, **kw)
        eng.ops.append((waits, fn, (sem, 16)))
        self._commit(tok, reads, writes)
        return tok

    def barrier(self):
        targets = []
        for e in self.engs.values():
            if e.count:
                targets.append((id(e.sem), e.count))
            for s, c in e.dma_sems:
                if c:
                    targets.append((id(s), c))
        for e in self.engs.values():
            waits = []
            for k, v in targets:
                if e.seen.get(k, 0) < v:
                    e.seen[k] = v
                    waits.append((self.sems[k], v))
            if waits:
                e.ops.append((waits, None, None))

    def emit(self, block):
        handles = {"pe": "tensor", "act": "scalar", "dve": "vector", "pool": "gpsimd", "sp": "sync"}
        for name, eng in self.engs.items():
            def body(h, eng=eng):
                for waits, fn, inc in eng.ops:
                    for (s, v) in waits:
                        h.wait_ge(s, v)
                    if fn is not None:
                        if isinstance(fn, tuple):
                            name_, args_, kw_ = fn
                            ins = getattr(h, name_)(*args_, **kw_)
                        else:
                            ins = fn(h)
                        ins.then_inc(inc[0], inc[1])
            getattr(block, handles[name])(body)


class T:
    def __init__(self, t, name, excl=False):
        self.t = t
        self.r = Res(name, excl)

    def __getitem__(self, k):
        return self.t[k]


def build_program():
    nc = bass.Bass("TRN2", target_bir_lowering=False)
    IN = {}
    OUT = {}

    def din(name, shape, dt=F32):
        IN[name] = nc.dram_tensor(name, list(shape), dt, kind="ExternalInput").ap()
        return IN[name]

    def dout(name, shape, dt=F32):
        OUT[name] = nc.dram_tensor(name, list(shape), dt, kind="ExternalOutput").ap()
        return OUT[name]

    xT = din("xT", [D, HALO + SEQ])
    xo = din("xo", [D, NA])
    ca_k = din("ca_k", [4, 512, 512])
    ca_v = din("ca_v", [4, 512, 512])
    sp_T = din("sp_T", [4, 512, 15])
    cc_k = din("cc_k", [4, 1024, PAST])
    cc_v = din("cc_v", [4, PAST, 1024])
    cc_lf = din("cc_lf", [4, PAST, 16])
    cm_k = din("cm_k", [2, 4, 1024, 256])
    cm_v = din("cm_v", [2, 4, 256, 1024])
    memT = din("memT", [D, 256])
    w_in_ab = din("w_in_ab", [D, 2048])
    BTd = din("BT", [128, 5 * 8 * 128])
    BTSd = din("BTS", [128, 5 * 8 * 16])
    pool_w = din("pool_w", [4, 128, 128])
    pool_sc = din("pool_sc", [128, 4])
    w_out_ab = din("w_out_ab", [D, D])
    w_in_c = din("w_in_c", [D, 3088])
    bf_rep = din("bf_rep", [128, 16])
    w_out_c = din("w_out_c", [D, D])
    w_xq = din("w_xq", [2, D, D])
    w_xk = din("w_xk", [2, D, D])
    w_xv = din("w_xv", [2, D, D])
    w_xo = din("w_xo", [2, D, D])
    ln_g = din("ln_g", [128, 48])
    ln_b = din("ln_b", [128, 48])
    ffn_w1 = din("ffn_w1", [D, DFF])
    ffn_w3 = din("ffn_w3", [D, DFF])
    ffn_w2 = din("ffn_w2", [DFF, D])
    w_router = din("w_router", [D, 8])
    br_rep = din("br_rep", [128, 8])
    moe_w1 = din("moe_w1", [8, D, DFF])
    moe_w3 = din("moe_w3", [8, D, DFF])
    moe_w2 = din("moe_w2", [8, DFF, D])
    VALT = din("VALT", [9, 128, 21])
    FIXd = din("FIX", [9, 128, 64])
    INVIS = din("INVIS", [128, 128])
    SELT = din("SELT", [128, 128])
    CONST = din("CONST", [128, 4 * 128])

    o_yT = dout("yT", [D, NT])
    o_pak = dout("p_a_kT", [512, 512])
    o_pav = dout("p_a_v", [512, 512])
    o_ppool = dout("p_poolT", [512, 15])
    o_pck = dout("p_c_kT", [D, SEQ])
    o_pcv = dout("p_c_v", [SEQ, D])
    o_pclf = dout("p_c_lf", [SEQ, 16])
    o_pmk = dout("p_mem_kT", [2, D, 256])
    o_pmv = dout("p_mem_v", [2, 256, D])
    o_sak = dout("s_a_kT", [512, NS])
    o_sav = dout("s_a_v", [NS, 512])
    o_spool = dout("s_poolT", [4, 512, 15])
    o_sck = dout("s_c_kT", [D, NS])
    o_scv = dout("s_c_v", [NS, D])
    o_sclf = dout("s_c_lf", [NS, 16])
    R_OUT = {k: Res("out_" + k) for k in OUT}

    KS = nc.dram_tensor("KS", [8, 128, SEQ], BF16).ap()
    VS = nc.dram_tensor("VS", [SEQ, D], BF16).ap()
    KSO = nc.dram_tensor("KSO", [8, 128, NT], BF16).ap()
    VSO = nc.dram_tensor("VSO", [BLK, D], BF16).ap()
    QSO = nc.dram_tensor("QSO", [8, 128, NT], BF16).ap()
    X1S = nc.dram_tensor("X1S", [128, 8, NT], F32).ap()
    MKD = nc.dram_tensor("MKD", [2, 128, 8, 256], BF16).ap()
    MVD = nc.dram_tensor("MVD", [2, 128, 2, 1024], BF16).ap()
    R_MKD, R_MVD = Res("MKD"), Res("MVD")
    VSNd = nc.dram_tensor("VSNd", [16, 4, D], BF16).ap()
    R_VSNd = Res("VSNd")
    R_KS, R_VS, R_KSO, R_VSO, R_QSO, R_X1S = (Res(n) for n in ("KS", "VS", "KSO", "VSO", "QSO", "X1S"))

    with contextlib.ExitStack() as top:
        fw = FW(nc, top)

        uid = [0]

        def sb(stack, name, shape, dt):
            uid[0] += 1
            nm = f"sb{uid[0]}_{name}"
            return T(stack.enter_context(nc.sbuf_tensor(nm, list(shape), dt)), nm)

        PS = [T(top.enter_context(nc.psum_tensor(f"ps{i}", [128, 512], F32)), f"ps{i}", excl=True) for i in range(8)]
        ps_rr = [0]

        def next_ps(pool=(0, 1, 2, 3, 4, 5, 6, 7)):
            p = PS[pool[ps_rr[0] % len(pool)]]
            ps_rr[0] += 1
            return p

        XB = sb(top, "XB", [128, 8, NT], BF16)
        WB = [sb(top, f"WB{i}", [128, 4096], BF16) for i in range(3)]
        wb_rr = [0]

        def next_wb():
            w = WB[wb_rr[0] % len(WB)]
            wb_rr[0] += 1
            return w
        CST = sb(top, "CST", [128, 512], F32)
        CSTB = sb(top, "CSTB", [128, 512], BF16)
        LNG = sb(top, "LNG", [128, 48], F32)
        LNB = sb(top, "LNB", [128, 48], F32)
        BFR = sb(top, "BFR", [128, 16], F32)
        PSC = sb(top, "PSC", [128, 4], F32)
        PW = sb(top, "PW", [128, 4, 128], BF16)
        LF = sb(top, "LF", [128, 128, 16], F32)
        LFO = sb(top, "LFO", [128, 16, 16], F32)
        LFN = sb(top, "LFN", [16, 4, 16], F32)

        TOUCH = sb(top, "TOUCH", [1, 8 * 64], F32)
        KDBG = int(os.environ.get("KDBG", "99"))
        for ti_, (nm_, ap_) in enumerate(IN.items()):
            if KDBG < 1:
                break
            a_ = ap_
            while a_.ndim > 2:
                a_ = a_[0]
            fw.dma("sp", TOUCH[0:1, ti_ * 8:ti_ * 8 + 4], a_[0:1, 0:4], writes=[TOUCH.r])
        fw.dma("sp", CST[:], CONST, writes=[CST.r])
        fw.dma("pool", CSTB[:], CONST, writes=[CSTB.r])
        fw.dma("sp", LNG[:], ln_g, writes=[LNG.r])
        fw.dma("sp", LNB[:], ln_b, writes=[LNB.r])
        fw.dma("sp", BFR[:], bf_rep, writes=[BFR.r])
        fw.dma("sp", PSC[:], pool_sc, writes=[PSC.r])
        fw.dma("pool", PW[:], pool_w.rearrange("g c e -> c g e"), writes=[PW.r])
        fw.op("dve", ("memset", (LF[:], 0.0), {}), writes=[LF.r])
        ONES_B = CSTB[:, 0:128]
        TRI_B = CSTB[:, 128:256]
        ONES_F = CST[:, 0:128]
        TRI_F = CST[:, 128:256]
        IDENT_F = CST[:, 256:384]

        def OP(eng, name, _reads=(), _writes=(), _accum=False, _a=None, _p=()):
            fw.op(eng, (name, tuple(_p), dict(_a or {})), reads=_reads, writes=_writes, accum=_accum)

        def mm(ps_ap, lhsT, rhs, start, stop, reads, psT):
            fw.op("pe", ("matmul", (ps_ap, lhsT, rhs), dict(start=start, stop=stop)), reads=reads, writes=[psT.r], accum=True)

        def wload(dst_ap, src_ap, wbT, eng="pool"):
            fw.dma(eng, dst_ap, src_ap, writes=[wbT.r])

        def linear_fm(Wap, KC, o0, n_out, rhs_fn, rhs_res, col_tiles, evac_fn, kpart=128):
            gw_max = (4096 // KC) // 128 * 128
            og = 0
            while og < n_out:
                gw = min(gw_max, n_out - og)
                wb = next_wb()
                wv = wb[0:kpart, 0:KC * gw].rearrange("p (k n) -> p k n", n=gw)
                wload(wv, Wap[:, o0 + og:o0 + og + gw].rearrange("(k p) n -> p k n", p=kpart), wb)
                for oc in range(0, gw, 128):
                    m = min(128, gw - oc)
                    for (c0, cw) in col_tiles:
                        if KDBG < 3:
                            continue
                        ps = next_ps()
                        for kc in range(KC):
                            mm(ps[0:m, 0:cw], wv[:, kc, oc:oc + m], rhs_fn(kc, c0, cw), kc == 0, kc == KC - 1,
                               [wb.r] + rhs_res, ps)
                        if KDBG >= 4:
                            evac_fn(o0 + og + oc, m, c0, cw, ps)
                og += gw

        def linear_tm(Wap, KC, o0, n_out, lhsT_fn, lhs_res, tok_tiles, evac_fn):
            og = 0
            while og < n_out:
                gw = min(512, n_out - og)
                wb = next_wb()
                wv = wb[:, 0:KC * gw].rearrange("p (k n) -> p k n", n=gw)
                wload(wv, Wap[:, o0 + og:o0 + og + gw].rearrange("(k p) n -> p k n", p=128), wb)
                for ti, (t0, tw) in enumerate(tok_tiles):
                    if KDBG < 3:
                        continue
                    ps = next_ps()
                    for kc in range(KC):
                        mm(ps[0:tw, 0:gw], lhsT_fn(kc, t0, tw), wv[:, kc, 0:gw], kc == 0, kc == KC - 1,
                           [wb.r] + lhs_res, ps)
                    if KDBG >= 4:
                        evac_fn(ti, t0, tw, og, gw, ps)
                og += gw

        def layernorm(XRES, lidx):
            with contextlib.ExitStack() as st:
                CB = sb(st, "ln_cb", [128, 8, 512], BF16)
                SQ = sb(st, "ln_sq", [128, 8, 512], BF16)
                LM = sb(st, "ln_m", [128, 512], F32)
                LV = sb(st, "ln_v", [128, 512], F32)
                LR = sb(st, "ln_r", [128, 512], F32)
                T0 = sb(st, "ln_t0", [128, 512], F32)
                T1 = sb(st, "ln_t1", [128, 512], F32)
                TT = [T0, T1]
                for (c0, cw) in COLT:
                    for kc in range(8):
                        OP("act", "activation", _reads=[XRES.r], _writes=[CB.r], _a=dict(out=CB[:, kc, 0:cw], in_=XRES[:, kc, c0:c0 + cw], func=AF.Copy))
                        OP("act", "activation", _reads=[XRES.r], _writes=[SQ.r], _a=dict(out=SQ[:, kc, 0:cw], in_=XRES[:, kc, c0:c0 + cw], func=AF.Square))
                    p1 = next_ps()
                    p2 = next_ps()
                    for kc in range(8):
                        mm(p1[:, 0:cw], ONES_B, CB[:, kc, 0:cw], kc == 0, kc == 7, [CB.r, CSTB.r], p1)
                    for kc in range(8):
                        mm(p2[:, 0:cw], ONES_B, SQ[:, kc, 0:cw], kc == 0, kc == 7, [SQ.r, CSTB.r], p2)
                    OP("dve", "tensor_scalar", _reads=[p1.r], _writes=[LM.r], _a=dict(out=LM[:, 0:cw], in0=p1[:, 0:cw], scalar1=1.0 / D, scalar2=None, op0=ALU.mult))
                    OP("dve", "tensor_tensor", _reads=[LM.r], _writes=[LV.r], _a=dict(out=LV[:, 0:cw], in0=LM[:, 0:cw], in1=LM[:, 0:cw], op=ALU.mult))
                    OP("dve", "scalar_tensor_tensor", _reads=[p2.r, LV.r], _writes=[LV.r], _a=dict(out=LV[:, 0:cw], in0=p2[:, 0:cw], scalar=1.0 / D, in1=LV[:, 0:cw],
                                                                   op0=ALU.mult, op1=ALU.subtract))
                    OP("dve", "tensor_scalar", _reads=[LV.r], _writes=[LV.r], _a=dict(out=LV[:, 0:cw], in0=LV[:, 0:cw], scalar1=0.0, scalar2=LN_EPS, op0=ALU.max, op1=ALU.add))
                    OP("act", "activation", _reads=[LV.r], _writes=[LV.r], _a=dict(out=LV[:, 0:cw], in_=LV[:, 0:cw], func=AF.Sqrt))
                    OP("dve", "reciprocal", _reads=[LV.r], _writes=[LR.r], _a=dict(out=LR[:, 0:cw], in_=LV[:, 0:cw]))
                    for kc in range(8):
                        tt = TT[kc % 2]
                        gi = lidx * 8 + kc
                        OP("dve", "tensor_tensor", _reads=[XRES.r, LM.r], _writes=[tt.r], _a=dict(out=tt[:, 0:cw], in0=XRES[:, kc, c0:c0 + cw], in1=LM[:, 0:cw], op=ALU.subtract))
                        OP("dve", "tensor_tensor", _reads=[tt.r, LR.r], _writes=[tt.r], _a=dict(out=tt[:, 0:cw], in0=tt[:, 0:cw], in1=LR[:, 0:cw], op=ALU.mult))
                        OP("dve", "tensor_scalar", _reads=[tt.r, LNG.r, LNB.r], _writes=[XRES.r], _a=dict(out=XRES[:, kc, c0:c0 + cw], in0=tt[:, 0:cw],
                                                                                 scalar1=LNG[:, gi:gi + 1], scalar2=LNB[:, gi:gi + 1],
                                                                                 op0=ALU.mult, op1=ALU.add))
                        OP("act", "activation", _reads=[tt.r, LNG.r, LNB.r], _writes=[XB.r], _a=dict(out=XB[:, kc, c0:c0 + cw], in_=tt[:, 0:cw], func=AF.Identity,
                                                                              scale=LNG[:, gi:gi + 1], bias=LNB[:, gi:gi + 1]))
                fw.barrier()

        def resid_evac(XRES):
            def ev(o, m, c0, cw, ps):
                kc = o // 128
                OP("dve", "scalar_tensor_tensor", _reads=[ps.r, XRES.r], _writes=[XRES.r], _a=dict(out=XRES[:, kc, c0:c0 + cw], in0=XRES[:, kc, c0:c0 + cw], scalar=ALPHA,
                                                               in1=ps[:, 0:cw], op0=ALU.mult, op1=ALU.add))
            return ev

        with contextlib.ExitStack() as st:
          if "mem" in PARTS and KDBG >= 2:
            MEMB = sb(st, "MEMB", [128, 8, 256], BF16)
            MKP = sb(st, "MKP", [128, 2, 8, 256], BF16)
            MVP = sb(st, "MVP", [128, 2, 2, 1024], BF16)
            STG = [sb(st, f"mstg{i}", [128, 512], F32) for i in range(2)]
            sg = [0]
            fw.dma("pool", MEMB[:], memT.rearrange("(k p) n -> p k n", p=128), writes=[MEMB.r])
            for l in range(2):
                def ev_k(o, m, c0, cw, ps, l=l):
                    s = STG[sg[0] % 2]
                    sg[0] += 1
                    OP("dve", "tensor_copy", _reads=[ps.r], _writes=[s.r], _a=dict(out=s[:, 0:256], in_=ps[:, 0:256]))
                    if KDBG >= 5:
                        if os.environ.get("KV") == "2":
                            OP("act", "activation", _reads=[s.r], _writes=[MKP.r], _a=dict(out=MKP[:, l, o // 128, :], in_=s[:, 0:256], func=AF.Copy))
                        else:
                            OP("act", "activation", _reads=[ps.r] + ([s.r] if os.environ.get("KV") == "3" else []), _writes=[MKP.r], _a=dict(out=MKP[:, l, o // 128, :], in_=ps[:, 0:256], func=AF.Copy))
                    if KDBG >= 6:
                        fw.dma("sp", o_pmk[l, o:o + 128, :], s[:, 0:256], reads=[s.r], writes=[R_OUT["p_mem_kT"]])
                linear_fm(w_xk[l], 8, 0, D, lambda kc, c0, cw: MEMB[:, kc, c0:c0 + cw], [MEMB.r], [(0, 256)], ev_k)

                def ev_v(ti, t0, tw, og, gw, ps, l=l):
                    s = STG[sg[0] % 2]
                    sg[0] += 1
                    OP("dve", "tensor_copy", _reads=[ps.r], _writes=[s.r], _a=dict(out=s[:, 0:gw], in_=ps[:, 0:gw]))
                    if KDBG >= 5:
                        if os.environ.get("KV") == "2":
                            OP("act", "activation", _reads=[s.r], _writes=[MVP.r], _a=dict(out=MVP[:, l, ti, og:og + gw], in_=s[:, 0:gw], func=AF.Copy))
                        else:
                            OP("act", "activation", _reads=[ps.r] + ([s.r] if os.environ.get("KV") == "3" else []), _writes=[MVP.r], _a=dict(out=MVP[:, l, ti, og:og + gw], in_=ps[:, 0:gw], func=AF.Copy))
                    if KDBG >= 6:
                        fw.dma("sp", o_pmv[l, t0:t0 + tw, og:og + gw], s[:, 0:gw], reads=[s.r], writes=[R_OUT["p_mem_v"]])
                linear_tm(w_xv[l], 8, 0, D, lambda kc, t0, tw: MEMB[:, kc, t0:t0 + tw], [MEMB.r], [(0, 128), (128, 128)], ev_v)
            if KDBG >= 7:
                fw.dma("sp", MKD.rearrange("l p k m -> p l k m"), MKP[:], reads=[MKP.r], writes=[R_MKD])
                fw.dma("sp", MVD.rearrange("l p t n -> p l t n"), MVP[:], reads=[MVP.r], writes=[R_MVD])
            fw.barrier()

        def cross_attention(XRES, l, own):
            with contextlib.ExitStack() as st:
                QX = sb(st, "QX", [128, 8, NT], BF16)
                MKP = sb(st, "MKPl", [128, 8, 256], BF16)
                MVP = sb(st, "MVPl", [128, 2, 1024], BF16)
                fw.dma("sp", MKP[:], MKD[l], reads=[R_MKD], writes=[MKP.r])
                fw.dma("sp", MVP[:], MVD[l], reads=[R_MVD], writes=[MVP.r])
                PX = sb(st, "PX", [128, 2, 512], BF16)
                RX = sb(st, "RX", [128, 512], F32)

                def ev_q(o, m, c0, cw, ps):
                    eng = "act" if (o // 128) % 2 else "dve"
                    if eng == "act":
                        OP("act", "activation", _reads=[ps.r], _writes=[QX.r], _a=dict(out=QX[:, o // 128, c0:c0 + cw], in_=ps[:, 0:cw], func=AF.Copy))
                    else:
                        OP("dve", "tensor_copy", _reads=[ps.r], _writes=[QX.r], _a=dict(out=QX[:, o // 128, c0:c0 + cw], in_=ps[:, 0:cw]))
                linear_fm(w_xq[l], 8, 0, D, lambda kc, c0, cw: XB[:, kc, c0:c0 + cw], [XB.r], COLT, ev_q)

                def group(c0, cw, mk_fn, mv_fn, mres):
                    for hd in range(4):
                        for mt in range(2):
                            ps = next_ps()
                            for c in range(2):
                                mm(ps[:, 0:cw], mk_fn(2 * hd + c, mt), QX[:, 2 * hd + c, c0:c0 + cw], c == 0, c == 1, mres + [QX.r], ps)
                            OP("act", "activation", _reads=[ps.r], _writes=[PX.r], _a=dict(out=PX[:, mt, 0:cw], in_=ps[:, 0:cw], func=AF.Exp, scale=1.0 / 16.0))
                        pd = next_ps()
                        for mt in range(2):
                            mm(pd[:, 0:cw], ONES_B, PX[:, mt, 0:cw], mt == 0, mt == 1, [PX.r, CSTB.r], pd)
                        OP("dve", "reciprocal", _reads=[pd.r], _writes=[RX.r], _a=dict(out=RX[:, 0:cw], in_=pd[:, 0:cw]))
                        for c in range(2):
                            po = next_ps()
                            for mt in range(2):
                                mm(po[:, 0:cw], mv_fn(2 * hd + c, mt), PX[:, mt, 0:cw], mt == 0, mt == 1, mres + [PX.r], po)
                            OP("dve", "tensor_tensor", _reads=[po.r, RX.r], _writes=[XB.r], _a=dict(out=XB[:, 2 * hd + c, c0:c0 + cw], in0=po[:, 0:cw], in1=RX[:, 0:cw], op=ALU.mult))
                for (c0, cw) in COLT[:4]:
                    group(c0, cw, lambda ch, mt: MKP[:, ch, mt * 128:(mt + 1) * 128],
                          lambda ch, mt: MVP[:, mt, ch * 128:(ch + 1) * 128], [MKP.r, MVP.r])
                if own:
                    MKS = sb(st, "MKS", [128, 8, 256], BF16)
                    MVS = sb(st, "MVS", [128, 2, 1024], BF16)
                    for s in range(4):
                        fw.dma("pool", MKS[:], cm_k[l, s].rearrange("(k p) m -> p k m", p=128), writes=[MKS.r])
                        fw.dma("pool", MVS[:], cm_v[l, s].rearrange("(t p) n -> p t n", p=128), writes=[MVS.r])
                        group(BLK + 16 * s, 16, lambda ch, mt: MKS[:, ch, mt * 128:(mt + 1) * 128],
                              lambda ch, mt: MVS[:, mt, ch * 128:(ch + 1) * 128], [MKS.r, MVS.r])
                else:
                    OP("dve", "tensor_copy", _reads=[QX.r], _writes=[XB.r], _a=dict(out=XB[:, :, BLK:NT], in_=QX[:, :, BLK:NT]))
                fw.barrier()
            linear_fm(w_xo[l], 8, 0, D, lambda kc, c0, cw: XB[:, kc, c0:c0 + cw], [XB.r], COLT, resid_evac(XRES))
            fw.barrier()

        def ffn(XRES, w1, w3, w2, gate=None):
            TT_ = [(0, 1056), (1056, 1056)]
            with contextlib.ExitStack() as st:
                W2H = sb(st, "W2H", [128, 11, 1024], BF16)
                G = sb(st, "G", [128, 11, 1056], BF16)
                SL = [sb(st, f"SL{i}", [128, 512], BF16) for i in range(2)]
                sl = [0]
                for (t0, tn) in TT_:
                    subt = [(0, 512), (512, 512), (1024, 32)]
                    for hf in range(2):
                        f0 = hf * 11
                        for g4 in range(0, 11, 4):
                            n4 = min(4, 11 - g4)
                            fw.dma("pool", W2H[:, g4:g4 + n4, :], w2[(f0 + g4) * 128:(f0 + g4 + n4) * 128, :].rearrange("(k p) n -> p k n", p=128),
                                   writes=[W2H.r])
                        for g4 in range(0, 11, 4):
                            n4 = min(4, 11 - g4)
                            gw = n4 * 128
                            wb1 = next_wb()
                            wb3 = next_wb()
                            v1 = wb1[:, 0:8 * gw].rearrange("p (k n) -> p k n", n=gw)
                            v3 = wb3[:, 0:8 * gw].rearrange("p (k n) -> p k n", n=gw)
                            cc0 = (f0 + g4) * 128
                            wload(v1, w1[:, cc0:cc0 + gw].rearrange("(k p) n -> p k n", p=128), wb1)
                            wload(v3, w3[:, cc0:cc0 + gw].rearrange("(k p) n -> p k n", p=128), wb3)
                            for j in range(n4):
                                for (s0, sw) in subt:
                                    pa = next_ps()
                                    pb = next_ps()
                                    for kc in range(8):
                                        mm(pa[:, 0:sw], v1[:, kc, j * 128:(j + 1) * 128], XB[:, kc, t0 + s0:t0 + s0 + sw], kc == 0, kc == 7, [wb1.r, XB.r], pa)
                                    for kc in range(8):
                                        mm(pb[:, 0:sw], v3[:, kc, j * 128:(j + 1) * 128], XB[:, kc, t0 + s0:t0 + s0 + sw], kc == 0, kc == 7, [wb3.r, XB.r], pb)
                                    s_ = SL[sl[0] % 2]
                                    sl[0] += 1
                                    OP("act", "activation", _reads=[pa.r], _writes=[s_.r], _a=dict(out=s_[:, 0:sw], in_=pa[:, 0:sw], func=AF.Silu))
                                    if gate is not None:
                                        OP("pool", "tensor_tensor", _reads=[s_.r, gate.r], _writes=[s_.r], _a=dict(out=s_[:, 0:sw], in0=s_[:, 0:sw], in1=gate[:, t0 + s0:t0 + s0 + sw], op=ALU.mult))
                                    OP("dve", "tensor_tensor", _reads=[s_.r, pb.r], _writes=[G.r], _a=dict(out=G[:, g4 + j, s0:s0 + sw], in0=s_[:, 0:sw], in1=pb[:, 0:sw], op=ALU.mult))
                        for oc in range(8):
                            for (s0, sw) in subt:
                                py = next_ps()
                                for f in range(11):
                                    mm(py[:, 0:sw], W2H[:, f, oc * 128:(oc + 1) * 128], G[:, f, s0:s0 + sw], f == 0, f == 10, [W2H.r, G.r], py)
                                OP("dve", "tensor_tensor", _reads=[py.r, XRES.r], _writes=[XRES.r], _a=dict(out=XRES[:, oc, t0 + s0:t0 + s0 + sw], in0=XRES[:, oc, t0 + s0:t0 + s0 + sw],
                                                                                  in1=py[:, 0:sw], op=ALU.add))
                fw.barrier()
        gate_res = None

        def layer0_pass(b, own):
            pi = 8 if own else b
            with contextlib.ExitStack() as sA:
                XBA = sb(sA, "XBA", [128, 8, NA], BF16)
                if own:
                    fw.dma("pool", XBA[:], xo.rearrange("(k p) n -> p k n", p=128), writes=[XBA.r])
                else:
                    fw.dma("pool", XBA[:, :, 0:HALO + BLK], xT[:, b * BLK:b * BLK + HALO + BLK].rearrange("(k p) n -> p k n", p=128), writes=[XBA.r])
                    fw.dma("pool", XBA[:, :, HALO + BLK:NA], xo[:, HALO + BLK:NA].rearrange("(k p) n -> p k n", p=128), writes=[XBA.r])
                with contextlib.suppress(SkipBlock), contextlib.ExitStack() as s1:
                    if "pool" not in PARTS:
                        raise SkipBlock()
                    UP = sb(s1, "UP", [128, 4, 16 + BLK], F32)
                    UH = sb(s1, "UH", [128, 4, 4, 32], F32)
                    TA = sb(s1, "TA", [128, 16 + BLK], F32)
                    TB = sb(s1, "TB", [128, 16 + BLK], F32)
                    DD = sb(s1, "DD", [128, 4, NT], BF16)
                    FX = sb(s1, "FX", [128, 64], F32)
                    fw.dma("sp", FX[:], FIXd[pi], writes=[FX.r])
                    OP("dve", "memset", _writes=[UH.r], _p=(UH[:], 0.0,))
                    if own:
                        for s in range(4):
                            fw.dma("sp", UH[:, :, s, 1:16], sp_T[s].rearrange("(g p) t -> p g t", p=128), writes=[UH.r])
                    ucols = [(496, 512), (1008, 512), (1520, 512), (2032, 512), (2544, 80)]

                    def ev_u(o, m, c0, cw, ps):
                        g = (o - 1536) // 128
                        if c0 < 2544:
                            OP("act", "activation", _reads=[ps.r], _writes=[UP.r], _a=dict(out=UP[:, g, c0 - 496:c0 - 496 + cw], in_=ps[:, 0:cw], func=AF.Copy))
                        else:
                            OP("act", "activation", _reads=[ps.r], _writes=[UP.r], _a=dict(out=UP[:, g, 2048:2064], in_=ps[:, 0:16], func=AF.Copy))
                            OP("dve", "tensor_copy", _reads=[ps.r], _writes=[UH.r], _a=dict(out=UH[:, g, :, 16:32], in_=ps[:, 16:80].rearrange("p (s t) -> p s t", t=16)))
                    linear_fm(w_in_ab, 8, 1536, 512, lambda kc, c0, cw: XBA[:, kc, c0:c0 + cw], [XBA.r], ucols, ev_u)
                    if own:
                        for s in range(4):
                            fw.dma("sp", o_spool[s].rearrange("(g p) t -> p g t", p=128), UH[:, :, s, 17:32], reads=[UH.r], writes=[R_OUT["s_poolT"]])
                    if (not own) and b == NBLK - 1:
                        fw.dma("sp", o_ppool.rearrange("(g p) t -> p g t", p=128), UP[:, :, 16 + BLK - 15:16 + BLK], reads=[UP.r], writes=[R_OUT["p_poolT"]])
                    for g in range(4):
                        w = 2 << g
                        L = 16 + BLK
                        src = UP[:, g, :]
                        srcr = UP.r
                        bufs = [TA, TB]
                        step = 1
                        k = 0
                        lo = 0
                        while step < w:
                            dst = bufs[k % 2]
                            lo += step
                            OP("dve", "tensor_tensor", _reads=[srcr], _writes=[dst.r], _a=dict(out=dst[:, lo:L], in0=src[:, lo:L], in1=src[:, lo - step:L - step], op=ALU.add))
                            src = dst[:, :]
                            srcr = dst.r
                            step *= 2
                            k += 1
                        win = src
                        winr = srcr
                        other = bufs[k % 2]
                        OP("dve", "scalar_tensor_tensor", _reads=[winr, UP.r], _writes=[other.r], _a=dict(out=other[:, 16:L], in0=win[:, 16:L], scalar=1.0 / w, in1=UP[:, g, 16:L],
                                                                                            op0=ALU.mult, op1=ALU.subtract))
                        OP("dve", "tensor_tensor", _reads=[winr, FX.r], _writes=[winr], _a=dict(out=win[:, 16:32], in0=win[:, 16:32], in1=FX[:, g * 16:(g + 1) * 16], op=ALU.mult))
                        OP("dve", "tensor_tensor", _reads=[winr, UP.r, other.r], _writes=[other.r], _a=dict(out=other[:, 16:32], in0=win[:, 16:32], in1=UP[:, g, 16:32], op=ALU.subtract))
                        OP("act", "activation", _reads=[other.r], _writes=[DD.r], _a=dict(out=DD[:, g, 0:BLK], in_=other[:, 16:L], func=AF.Copy))
                        SA = TA[:, 0:128].rearrange("p (s t) -> p s t", t=32)
                        SB_ = TB[:, 0:128].rearrange("p (s t) -> p s t", t=32)
                        srcs = UH[:, g, :, :]
                        srcr = UH.r
                        sbufs = [(SA, TA.r), (SB_, TB.r)]
                        step = 1
                        k = 0
                        lo = 0
                        while step < w:
                            dst, dstr = sbufs[k % 2]
                            lo += step
                            OP("dve", "tensor_tensor", _reads=[srcr], _writes=[dstr], _a=dict(out=dst[:, :, lo:32], in0=srcs[:, :, lo:32], in1=srcs[:, :, lo - step:32 - step], op=ALU.add))
                            srcs = dst
                            srcr = dstr
                            step *= 2
                            k += 1
                        odst, odstr = sbufs[k % 2]
                        OP("dve", "scalar_tensor_tensor", _reads=[srcr, UH.r], _writes=[odstr], _a=dict(out=odst[:, :, 16:32], in0=srcs[:, :, 16:32], scalar=1.0 / w, in1=UH[:, g, :, 16:32],
                                                                                            op0=ALU.mult, op1=ALU.subtract))
                        OP("act", "activation", _reads=[odstr], _writes=[DD.r], _a=dict(out=DD[:, g, BLK:NT].rearrange("p (s t) -> p s t", t=16), in_=odst[:, :, 16:32], func=AF.Copy))
                        for (c0, cw) in COLT:
                            ps = next_ps()
                            mm(ps[:, 0:cw], PW[:, g, :], DD[:, g, c0:c0 + cw], True, True, [PW.r, DD.r], ps)
                            OP("act", "activation", _reads=[ps.r, PSC.r], _writes=[XB.r], _a=dict(out=XB[:, 4 + g, c0:c0 + cw], in_=ps[:, 0:cw], func=AF.Identity, scale=PSC[:, g:g + 1]))
                    fw.barrier()
                fw.barrier()
            with contextlib.ExitStack() as s2:
                QF = sb(s2, "QF", [128, 4, NT], BF16)
                KF = sb(s2, "KF", [128, 4, NA], BF16)
                VT = sb(s2, "VT", [128, 21, 512], BF16)
                VAL = sb(s2, "VAL", [128, 21, 64], BF16)
                VLT = sb(s2, "VLT", [128, 21], F32)
                ST32 = [sb(s2, f"st32_{i}", [128, 512], F32) for i in range(2)]
                stc = [0]
                if own:
                    VSN = sb(s2, "VSN", [16, 4, 512], BF16)
                    VSF = sb(s2, "VSF", [16, 4, 512], F32)
                sX = contextlib.ExitStack()
                XBA = sb(sX, "XBA2", [128, 8, NA], BF16)
                if own:
                    fw.dma("pool", XBA[:], xo.rearrange("(k p) n -> p k n", p=128), writes=[XBA.r])
                else:
                    fw.dma("pool", XBA[:, :, 0:HALO + BLK], xT[:, b * BLK:b * BLK + HALO + BLK].rearrange("(k p) n -> p k n", p=128), writes=[XBA.r])
                    fw.dma("pool", XBA[:, :, HALO + BLK:NA], xo[:, HALO + BLK:NA].rearrange("(k p) n -> p k n", p=128), writes=[XBA.r])
                fw.dma("sp", VLT[:], VALT[pi], writes=[VLT.r])
                OP("dve", "tensor_copy", _reads=[VLT.r], _writes=[VAL.r], _a=dict(out=VAL[:], in_=VLT[:].unsqueeze(2).to_broadcast([128, 21, 64])))

                def ev_q(o, m, c0, cw, ps):
                    OP("act", "activation", _reads=[ps.r], _writes=[QF.r], _a=dict(out=QF[:, o // 128, c0 - HALO:c0 - HALO + cw], in_=ps[:, 0:cw], func=AF.Copy))
                linear_fm(w_in_ab, 8, 0, 512, lambda kc, c0, cw: XBA[:, kc, c0:c0 + cw], [XBA.r], [(HALO + c, w_) for (c, w_) in COLT], ev_q)

                def ev_k(o, m, c0, cw, ps):
                    ch = (o - 512) // 128
                    OP("dve", "tensor_copy", _reads=[ps.r], _writes=[KF.r], _a=dict(out=KF[:, ch, c0:c0 + cw], in_=ps[:, 0:cw]))
                    if (not own) and b == NBLK - 1 and c0 == 2048:
                        s = ST32[stc[0] % 2]
                        stc[0] += 1
                        OP("act", "activation", _reads=[ps.r], _writes=[s.r], _a=dict(out=s[:, 0:512], in_=ps[:, 0:512], func=AF.Copy))
                        fw.dma("sp", o_pak[ch * 128:(ch + 1) * 128, :], s[:, 0:512], reads=[s.r], writes=[R_OUT["p_a_kT"]])
                    if own and c0 == 2560:
                        s = ST32[stc[0] % 2]
                        stc[0] += 1
                        OP("act", "activation", _reads=[ps.r], _writes=[s.r], _a=dict(out=s[:, 0:64], in_=ps[:, 0:64], func=AF.Copy))
                        fw.dma("sp", o_sak[ch * 128:(ch + 1) * 128, :], s[:, 0:64], reads=[s.r], writes=[R_OUT["s_a_kT"]])
                linear_fm(w_in_ab, 8, 512, 512, lambda kc, c0, cw: XBA[:, kc, c0:c0 + cw], [XBA.r], COLA, ev_k)

                tokt = [(t * 128, 128) for t in range(20)]

                def ev_v(ti, t0, tw, og, gw, ps):
                    OP("act", "activation", _reads=[ps.r], _writes=[VT.r], _a=dict(out=VT[:, ti, :], in_=ps[:, 0:512], func=AF.Copy))
                    if (not own) and b == NBLK - 1 and 16 <= ti < 20:
                        s = ST32[stc[0] % 2]
                        stc[0] += 1
                        OP("dve", "tensor_copy", _reads=[ps.r], _writes=[s.r], _a=dict(out=s[:, 0:512], in_=ps[:, 0:512]))
                        fw.dma("sp", o_pav[(ti - 16) * 128:(ti - 15) * 128, :], s[:, 0:512], reads=[s.r], writes=[R_OUT["p_a_v"]])
                linear_tm(w_in_ab, 8, 1024, 512, lambda kc, t0, tw: XBA[:, kc, t0:t0 + tw], [XBA.r], tokt, ev_v)
                if own:
                    wbv = next_wb()
                    wvv = wbv[:, 0:4096].rearrange("p (k n) -> p k n", n=512)
                    wload(wvv, w_in_ab[:, 1024:1536].rearrange("(k p) n -> p k n", p=128), wbv)
                    for s in range(4):
                        ps = next_ps()
                        for kc in range(8):
                            mm(ps[0:16, 0:512], XBA[:, kc, HALO + BLK + 16 * s:HALO + BLK + 16 * s + 16], wvv[:, kc, :], kc == 0, kc == 7, [XBA.r, wbv.r], ps)
                        OP("act", "activation", _reads=[ps.r], _writes=[VSN.r], _a=dict(out=VSN[:, s, :], in_=ps[0:16, 0:512], func=AF.Copy))
                        OP("dve", "tensor_copy", _reads=[ps.r], _writes=[VSF.r], _a=dict(out=VSF[:, s, :], in_=ps[0:16, 0:512]))
                    fw.dma("sp", o_sav.rearrange("(s t) n -> t s n", t=16), VSF[:], reads=[VSF.r], writes=[R_OUT["s_a_v"]])
                fw.barrier()
                sX.close()

                with contextlib.suppress(SkipBlock), contextlib.ExitStack() as s3:
                    if "band" not in PARTS:
                        raise SkipBlock()
                    BT = sb(s3, "BT", [128, 5, 8, 128], F32)
                    PB = sb(s3, "PB", [128, 5, 8, 128], BF16)
                    TMP = [sb(s3, f"batmp{i}", [128, 512], F32) for i in range(2)]
                    RD = sb(s3, "bard", [128, 512], F32)
                    tc_ = [0]
                    fw.dma("sp", BT[:], BTd.rearrange("p (j h q) -> p j h q", j=5, h=8), writes=[BT.r])
                    for i in range(16):
                        qc0 = i * 128
                        for j in range(5):
                            kt = i + j
                            for hg in range(2):
                                ps = next_ps((0, 1, 2, 3))
                                for hh in range(4):
                                    hd = 2 * hh + hg
                                    hp = (hd % 2) * 64
                                    mm(ps[:, hh * 128:(hh + 1) * 128], KF[hp:hp + 64, hd // 2, kt * 128:(kt + 1) * 128], QF[hp:hp + 64, hd // 2, qc0:qc0 + 128],
                                       True, True, [KF.r, QF.r], ps)
                                tm = TMP[tc_[0] % 2]
                                tc_[0] += 1
                                OP("dve", "scalar_tensor_tensor", _reads=[ps.r, BT.r], _writes=[tm.r], _a=dict(
                                    out=tm[:].rearrange("p (a q) -> p a q", q=128), in0=ps[:].rearrange("p (a q) -> p a q", q=128), scalar=0.125,
                                    in1=BT[:, j, hg * 4:(hg + 1) * 4, :], op0=ALU.mult, op1=ALU.add))
                                OP("act", "activation", _reads=[tm.r], _writes=[PB.r], _a=dict(out=PB[:, j, hg * 4:(hg + 1) * 4, :], in_=tm[:].rearrange("p (a q) -> p a q", q=128), func=AF.Exp))
                        po = next_ps((4, 5))
                        pd = next_ps((6, 7))
                        for hd in range(8):
                            hp = (hd % 2) * 64
                            cs = (hd // 2) * 128
                            for j in range(5):
                                mm(po[hp:hp + 64, cs:cs + 128], VT[:, i + j, hd * 64:(hd + 1) * 64], PB[:, j, (hd % 2) * 4 + hd // 2, :], j == 0, j == 4, [VT.r, PB.r], po)
                            for j in range(5):
                                mm(pd[hp:hp + 64, cs:cs + 128], VAL[:, i + j, :], PB[:, j, (hd % 2) * 4 + hd // 2, :], j == 0, j == 4, [VAL.r, PB.r], pd)
                        OP("dve", "reciprocal", _reads=[pd.r], _writes=[RD.r], _a=dict(out=RD[:], in_=pd[:]))
                        OP("dve", "tensor_tensor", _reads=[po.r, RD.r], _writes=[XB.r], _a=dict(out=XB[:, 0:4, qc0:qc0 + 128], in0=po[:].rearrange("p (a q) -> p a q", q=128),
                                                                             in1=RD[:].rearrange("p (a q) -> p a q", q=128), op=ALU.mult))
                    fw.barrier()
                if own:
                    with contextlib.suppress(SkipBlock), contextlib.ExitStack() as s3:
                        if "sband" not in PARTS:
                            raise SkipBlock()
                        KCA = sb(s3, "KCA", [128, 4, 512], BF16)
                        VCA = sb(s3, "VCA", [128, 4, 512], BF16)
                        BTS = sb(s3, "BTS", [128, 5, 8, 16], F32)
                        PSB = sb(s3, "PSB", [128, 5, 8, 16], BF16)
                        TMPS = sb(s3, "tmps", [128, 128], F32)
                        RDS = sb(s3, "rds", [128, 64], F32)
                        fw.dma("sp", BTS[:], BTSd.rearrange("p (j h q) -> p j h q", j=5, h=8), writes=[BTS.r])
                        OP("dve", "memset", _writes=[PSB.r], _p=(PSB[:], 0.0,))
                        for s in range(4):
                            fw.dma("pool", KCA[:], ca_k[s].rearrange("(k p) n -> p k n", p=128), writes=[KCA.r])
                            fw.dma("pool", VCA[:], ca_v[s].rearrange("(t p) n -> p t n", p=128), writes=[VCA.r])
                            qc0 = BLK + 16 * s
                            kn0 = HALO + BLK + 16 * s
                            for j in range(5):
                                kp = 128 if j < 4 else 16
                                for hg in range(2):
                                    ps = next_ps((0, 1, 2, 3))
                                    for hh in range(4):
                                        hd = 2 * hh + hg
                                        hp = hg * 64
                                        lhs = KCA[hp:hp + 64, hd // 2, j * 128:(j + 1) * 128] if j < 4 else KF[hp:hp + 64, hd // 2, kn0:kn0 + 16]
                                        mm(ps[0:kp, hh * 16:(hh + 1) * 16], lhs, QF[hp:hp + 64, hd // 2, qc0:qc0 + 16], True, True, [KCA.r, KF.r, QF.r], ps)
                                    OP("dve", "scalar_tensor_tensor", _reads=[ps.r, BTS.r], _writes=[TMPS.r], _a=dict(
                                        out=TMPS[0:kp, hg * 64:(hg + 1) * 64], in0=ps[0:kp, 0:64], scalar=0.125,
                                        in1=BTS[0:kp, j, hg * 4:(hg + 1) * 4, :].rearrange("p a q -> p (a q)"), op0=ALU.mult, op1=ALU.add))
                                OP("act", "activation", _reads=[TMPS.r], _writes=[PSB.r], _a=dict(out=PSB[0:kp, j, :, :].rearrange("p a q -> p (a q)"), in_=TMPS[0:kp, :], func=AF.Exp))
                            po = next_ps((4, 5))
                            pd = next_ps((6, 7))
                            for hd in range(8):
                                hp = (hd % 2) * 64
                                cs = (hd // 2) * 16
                                for j in range(5):
                                    kp = 128 if j < 4 else 16
                                    lv = VCA[:, j, hd * 64:(hd + 1) * 64] if j < 4 else VSN[0:16, s, hd * 64:(hd + 1) * 64]
                                    mm(po[hp:hp + 64, cs:cs + 16], lv, PSB[0:kp, j, (hd % 2) * 4 + hd // 2, :], j == 0, j == 4, [VCA.r, VSN.r, PSB.r], po)
                                for j in range(5):
                                    kp = 128 if j < 4 else 16
                                    mm(pd[hp:hp + 64, cs:cs + 16], CSTB[0:kp, 0:64], PSB[0:kp, j, (hd % 2) * 4 + hd // 2, :], j == 0, j == 4, [CSTB.r, PSB.r], pd)
                            OP("dve", "reciprocal", _reads=[pd.r], _writes=[RDS.r], _a=dict(out=RDS[:], in_=pd[:, 0:64]))
                            OP("dve", "tensor_tensor", _reads=[po.r, RDS.r], _writes=[XB.r], _a=dict(out=XB[:, 0:4, qc0:qc0 + 16], in0=po[:, 0:64].rearrange("p (a q) -> p a q", q=16),
                                                                                 in1=RDS[:].rearrange("p (a q) -> p a q", q=16), op=ALU.mult))
                        fw.barrier()
                else:
                    OP("dve", "tensor_copy", _reads=[QF.r], _writes=[XB.r], _a=dict(out=XB[:, 0:4, BLK:NT], in_=QF[:, :, BLK:NT]))
                fw.barrier()
            sB = contextlib.ExitStack()
            XRES = sb(sB, "XRES", [128, 8, NT], F32)
            if own:
                fw.dma("sp", XRES[:], xo[:, HALO:NA].rearrange("(k p) n -> p k n", p=128), writes=[XRES.r])
            else:
                fw.dma("sp", XRES[:, :, 0:BLK], xT[:, HALO + b * BLK:HALO + (b + 1) * BLK].rearrange("(k p) n -> p k n", p=128), writes=[XRES.r])
                fw.dma("sp", XRES[:, :, BLK:NT], xo[:, HALO + BLK:NA].rearrange("(k p) n -> p k n", p=128), writes=[XRES.r])
            if "outproj" in PARTS:
                linear_fm(w_out_ab, 8, 0, D, lambda kc, c0, cw: XB[:, kc, c0:c0 + cw], [XB.r], COLT, resid_evac(XRES))
            fw.barrier()
            if "ln1" in PARTS:
                layernorm(XRES, 0)
            if "xattn" in PARTS:
                cross_attention(XRES, 0, own)
            if "ln2" in PARTS:
                layernorm(XRES, 1)
            if "ffn" in PARTS:
                for kc in range(8):
                    OP("act", "activation", _reads=[XRES.r], _writes=[XRES.r], _a=dict(out=XRES[:, kc, :], in_=XRES[:, kc, :], func=AF.Identity, scale=ALPHA))
                ffn(XRES, ffn_w1, ffn_w3, ffn_w2)
            if "ln3" in PARTS:
                layernorm(XRES, 2)
            return sB, XRES

        XRES_holder = [None]

        def kvproj(b, own):
            with contextlib.ExitStack() as st:
                KST = [sb(st, f"kst{i}", [128, 512], BF16) for i in range(2)]
                KSF = [sb(st, f"ksf{i}", [128, 512], F32) for i in range(2)]
                VST = [sb(st, f"vst{i}", [128, 512], BF16) for i in range(2)]
                VSF_ = [sb(st, f"vsf{i}", [128, 512], F32) for i in range(2)]
                LFT = sb(st, "lft", [128, 17, 16], F32)
                c_ = [0]
                tok0 = b * BLK

                def ev_k(o, m, c0, cw, ps):
                    ch = (o - 1024) // 128
                    i = c_[0] % 2
                    c_[0] += 1
                    kb, kf = KST[i], KSF[i]
                    OP("act", "activation", _reads=[ps.r], _writes=[kb.r], _a=dict(out=kb[:, 0:cw], in_=ps[:, 0:cw], func=AF.Copy))
                    if own:
                        fw.dma("sp", KSO[ch, :, c0:c0 + cw], kb[:, 0:cw], reads=[kb.r], writes=[R_KSO])
                        if c0 == BLK:
                            OP("dve", "tensor_copy", _reads=[ps.r], _writes=[kf.r], _a=dict(out=kf[:, 0:cw], in_=ps[:, 0:cw]))
                            fw.dma("sp", o_sck[ch * 128:(ch + 1) * 128, :], kf[:, 0:cw], reads=[kf.r], writes=[R_OUT["s_c_kT"]])
                    elif c0 < BLK:
                        fw.dma("sp", KS[ch, :, tok0 + c0:tok0 + c0 + cw], kb[:, 0:cw], reads=[kb.r], writes=[R_KS])
                        OP("dve", "tensor_copy", _reads=[ps.r], _writes=[kf.r], _a=dict(out=kf[:, 0:cw], in_=ps[:, 0:cw]))
                        fw.dma("sp", o_pck[ch * 128:(ch + 1) * 128, tok0 + c0:tok0 + c0 + cw], kf[:, 0:cw], reads=[kf.r], writes=[R_OUT["p_c_kT"]])
                cols = COLT if own else COLT[:4]
                linear_fm(w_in_c, 8, 1024, 1024, lambda kc, c0, cw: XB[:, kc, c0:c0 + cw], [XB.r], cols, ev_k)

                def ev_v(ti, t0, tw, og, gw, ps):
                    i = c_[0] % 2
                    c_[0] += 1
                    vb, vf = VST[i], VSF_[i]
                    OP("act", "activation", _reads=[ps.r], _writes=[vb.r], _a=dict(out=vb[:, 0:gw], in_=ps[:, 0:gw], func=AF.Copy))
                    if own:
                        fw.dma("sp", VSO[t0:t0 + 128, og:og + gw], vb[:, 0:gw], reads=[vb.r], writes=[R_VSO])
                    else:
                        fw.dma("sp", VS[tok0 + t0:tok0 + t0 + 128, og:og + gw], vb[:, 0:gw], reads=[vb.r], writes=[R_VS])
                        OP("dve", "tensor_copy", _reads=[ps.r], _writes=[vf.r], _a=dict(out=vf[:, 0:gw], in_=ps[:, 0:gw]))
                        fw.dma("sp", o_pcv[tok0 + t0:tok0 + t0 + 128, og:og + gw], vf[:, 0:gw], reads=[vf.r], writes=[R_OUT["p_c_v"]])
                linear_tm(w_in_c, 8, 2048, 1024, lambda kc, t0, tw: XB[:, kc, t0:t0 + tw], [XB.r], [(t * 128, 128) for t in range(16)], ev_v)

                wb = next_wb()
                wv = wb[:, 0:128].rearrange("p (k n) -> p k n", n=16)
                wload(wv, w_in_c[:, 3072:3088].rearrange("(k p) n -> p k n", p=128), wb)
                ps = next_ps()
                for t in range(16):
                    for kc in range(8):
                        mm(ps[:, t * 16:(t + 1) * 16], XB[:, kc, t * 128:(t + 1) * 128], wv[:, kc, :], kc == 0, kc == 7, [XB.r, wb.r], ps)
                lfv = LFT[:, 0:16, :]
                LFD = LFO[:, :, :] if own else LF[:, b * 16:(b + 1) * 16, :]
                LFDr = LFO.r if own else LF.r
                OP("dve", "tensor_tensor", _reads=[ps.r, BFR.r], _writes=[LFT.r], _a=dict(out=lfv, in0=ps[:, 0:256].rearrange("p (t n) -> p t n", n=16),
                                                       in1=BFR[:].unsqueeze(1).to_broadcast([128, 16, 16]), op=ALU.add))
                OP("act", "activation", _reads=[LFT.r], _writes=[LFT.r], _a=dict(out=lfv, in_=lfv, func=AF.Exp, scale=-1.0))
                OP("act", "activation", _reads=[LFT.r], _writes=[LFT.r], _a=dict(out=lfv, in_=lfv, func=AF.Ln, bias=1.0))
                OP("dve", "tensor_scalar", _reads=[LFT.r], _writes=[LFDr], _a=dict(out=LFD, in0=lfv, scalar1=-1.0, scalar2=None, op0=ALU.mult))
                if not own:
                    fw.dma("sp", o_pclf[tok0:tok0 + BLK, :].rearrange("(t p) n -> p t n", p=128), LF[:, b * 16:(b + 1) * 16, :], reads=[LF.r], writes=[R_OUT["p_c_lf"]])
                else:
                    VN = sb(st, "VN", [16, 4, D], BF16)
                    VNF = sb(st, "VNF", [16, 4, D], F32)
                    psl = next_ps()
                    for s_ in range(4):
                        for kc in range(8):
                            mm(psl[0:16, s_ * 16:(s_ + 1) * 16], XB[:, kc, BLK + 16 * s_:BLK + 16 * s_ + 16], wv[:, kc, :], kc == 0, kc == 7, [XB.r, wb.r], psl)
                    lfn = LFT[0:16, 0:4, :]
                    OP("dve", "tensor_tensor", _reads=[psl.r, BFR.r], _writes=[LFT.r], _a=dict(out=lfn, in0=psl[0:16, 0:64].rearrange("p (t n) -> p t n", n=16),
                                                           in1=BFR[0:16, :].unsqueeze(1).to_broadcast([16, 4, 16]), op=ALU.add))
                    OP("act", "activation", _reads=[LFT.r], _writes=[LFT.r], _a=dict(out=lfn, in_=lfn, func=AF.Exp, scale=-1.0))
                    OP("act", "activation", _reads=[LFT.r], _writes=[LFT.r], _a=dict(out=lfn, in_=lfn, func=AF.Ln, bias=1.0))
                    OP("dve", "tensor_scalar", _reads=[LFT.r], _writes=[LFN.r], _a=dict(out=LFN[:], in0=lfn, scalar1=-1.0, scalar2=None, op0=ALU.mult))
                    fw.dma("sp", o_sclf.rearrange("(s t) n -> t s n", t=16), LFN[:], reads=[LFN.r], writes=[R_OUT["s_c_lf"]])
                    for og in range(0, D, 512):
                        wbv = next_wb()
                        wvv = wbv[:, 0:4096].rearrange("p (k n) -> p k n", n=512)
                        wload(wvv, w_in_c[:, 2048 + og:2048 + og + 512].rearrange("(k p) n -> p k n", p=128), wbv)
                        for s_ in range(4):
                            psv = next_ps()
                            for kc in range(8):
                                mm(psv[0:16, 0:512], XB[:, kc, BLK + 16 * s_:BLK + 16 * s_ + 16], wvv[:, kc, :], kc == 0, kc == 7, [XB.r, wbv.r], psv)
                            OP("act", "activation", _reads=[psv.r], _writes=[VN.r], _a=dict(out=VN[:, s_, og:og + 512], in_=psv[0:16, 0:512], func=AF.Copy))
                            OP("dve", "tensor_copy", _reads=[psv.r, VN.r], _writes=[VNF.r], _a=dict(out=VNF[:, s_, og:og + 512], in_=psv[0:16, 0:512]))
                    fw.dma("sp", o_scv.rearrange("(s t) n -> t s n", t=16), VNF[:], reads=[VNF.r], writes=[R_OUT["s_c_v"]])
                    fw.dma("sp", VSNd, VN[:], reads=[VN.r], writes=[R_VSNd])

                    def ev_q(o, m, c0, cw, ps):
                        i = c_[0] % 2
                        c_[0] += 1
                        kb = KST[i]
                        OP("act", "activation", _reads=[ps.r], _writes=[kb.r], _a=dict(out=kb[:, 0:cw], in_=ps[:, 0:cw], func=AF.Copy))
                        fw.dma("sp", QSO[o // 128, :, c0:c0 + cw], kb[:, 0:cw], reads=[kb.r], writes=[R_QSO])
                    linear_fm(w_in_c, 8, 0, 1024, lambda kc, c0, cw: XB[:, kc, c0:c0 + cw], [XB.r], COLT, ev_q)
                    fw.dma("sp", X1S, XRES_holder[0][:], reads=[XRES_holder[0].r], writes=[R_X1S])
                fw.barrier()

        for b in range(NBLK):
            if str(b) not in PASSES:
                continue
            sB, XRES = layer0_pass(b, False)
            if "kv" in PARTS:
                kvproj(b, False)
            sB.close()
            fw.barrier()
        if "own" in PASSES:
            sB, XRES = layer0_pass(0, True)
            XRES_holder[0] = XRES
            if "kv" in PARTS:
                kvproj(0, True)
            sB.close()
            fw.barrier()

        def prefix_tiles(st, TOTt, ntiles, name):
            A_ = sb(st, name + "_pa", [128, ntiles, 16], F32)
            B_ = sb(st, name + "_pb", [128, ntiles, 16], F32)
            cur = TOTt
            bufs = [A_, B_]
            k = 0
            step = 1
            while step < ntiles:
                dst = bufs[k % 2]
                OP("dve", "tensor_copy", _reads=[cur.r], _writes=[dst.r], _a=dict(out=dst[:, 0:step, :], in_=cur[:, 0:step, :]))
                OP("dve", "tensor_tensor", _reads=[cur.r], _writes=[dst.r], _a=dict(out=dst[:, step:ntiles, :], in0=cur[:, step:ntiles, :], in1=cur[:, 0:ntiles - step, :], op=ALU.add))
                cur = dst
                step *= 2
                k += 1
            return cur

        def tile_cumsum(st, LFsrc, LFres, ntiles, name, rows=128):
            TRI_ = sb(st, name + "_tri", [128, ntiles, 16], F32)
            TOT_ = sb(st, name + "_tot", [128, ntiles, 16], F32)
            for t0 in range(0, ntiles, 32):
                n = min(32, ntiles - t0)
                p1 = next_ps()
                p2 = next_ps()
                for t in range(n):
                    mm(p1[:, t * 16:(t + 1) * 16], TRI_F[0:rows, :], LFsrc[0:rows, t0 + t, :], True, True, [CST.r, LFres], p1)
                for t in range(n):
                    mm(p2[:, t * 16:(t + 1) * 16], ONES_F[0:rows, :], LFsrc[0:rows, t0 + t, :], True, True, [CST.r, LFres], p2)
                OP("dve", "tensor_copy", _reads=[p1.r], _writes=[TRI_.r], _a=dict(out=TRI_[:, t0:t0 + n, :], in_=p1[:, 0:n * 16].rearrange("p (t h) -> p t h", h=16)))
                OP("dve", "tensor_copy", _reads=[p2.r], _writes=[TOT_.r], _a=dict(out=TOT_[:, t0:t0 + n, :], in_=p2[:, 0:n * 16].rearrange("p (t h) -> p t h", h=16)))
            INC = prefix_tiles(st, TOT_, ntiles, name) if ntiles > 1 else TOT_
            CARX = sb(st, name + "_carx", [128, ntiles, 16], F32)
            OP("dve", "tensor_tensor", _reads=[INC.r, TOT_.r], _writes=[CARX.r], _a=dict(out=CARX[:], in0=INC[:], in1=TOT_[:], op=ALU.subtract))
            OP("dve", "tensor_tensor", _reads=[TRI_.r, CARX.r], _writes=[TRI_.r], _a=dict(out=TRI_[:], in0=TRI_[:], in1=CARX[:], op=ALU.add))
            return TRI_, CARX, INC

        def fox_prompt():
            with contextlib.ExitStack() as st:
                NB0 = sb(st, "NB0", [128, 128, 16], F32)
                NBq = sb(st, "NBq", [128, 128, 16], F32)
                DCO = sb(st, "DCO", [128, 16, 16], F32)
                XQ = sb(st, "XQ", [128, 16, 16], F32)
                NBOq = sb(st, "NBOq", [128, 16, 16], F32)
                with contextlib.ExitStack() as s1:
                    DC, CARX, INC = tile_cumsum(s1, LF, LF.r, 128, "g")
                    SELs = sb(s1, "SELs", [128, 128], F32)
                    INVs = sb(s1, "INVs", [128, 128], F32)
                    CREF = sb(s1, "CREF", [128, 16], F32)
                    TMPc = sb(s1, "TMPc", [128, 128, 16], F32)
                    fw.dma("sp", SELs[:], SELT, writes=[SELs.r])
                    fw.dma("sp", INVs[:], INVIS, writes=[INVs.r])
                    OP("dve", "tensor_tensor", _reads=[CARX.r, SELs.r], _writes=[TMPc.r], _a=dict(out=TMPc[:], in0=CARX[:], in1=SELs[:].unsqueeze(2).to_broadcast([128, 128, 16]), op=ALU.mult))
                    OP("dve", "tensor_reduce", _reads=[TMPc.r], _writes=[CREF.r], _a=dict(out=CREF[:], in_=TMPc[:].rearrange("p t h -> p h t"), axis=AX.X, op=ALU.add))
                    OP("dve", "tensor_tensor", _reads=[DC.r, CREF.r], _writes=[NB0.r], _a=dict(out=NB0[:], in0=CREF[:].unsqueeze(1).to_broadcast([128, 128, 16]), in1=DC[:], op=ALU.subtract))
                    OP("dve", "tensor_tensor", _reads=[NB0.r, INVs.r], _writes=[NB0.r], _a=dict(out=NB0[:], in0=NB0[:], in1=INVs[:].unsqueeze(2).to_broadcast([128, 128, 16]), op=ALU.add))
                    DCo_, CARXo, INCo = tile_cumsum(s1, LFO, LFO.r, 16, "o")
                    OP("dve", "tensor_copy", _reads=[DCo_.r], _writes=[DCO.r], _a=dict(out=DCO[:], in_=DCo_[:]))
                    OP("dve", "tensor_copy", _reads=[CARXo.r], _writes=[XQ.r], _a=dict(out=XQ[:], in_=CARXo[:]))
                    fw.barrier()
                if "foxp" not in L1:
                    return
                KH = sb(st, "KH", [128, SEQ], BF16)
                VHA = sb(st, "VHA", [128, 128, 2, 65], BF16)
                KHO = sb(st, "KHO", [128, BLK], BF16)
                VHOA = sb(st, "VHOA", [128, 16, 2, 65], BF16)
                QH = sb(st, "QH", [128, BLK], BF16)
                PT = [sb(st, f"PT{i}", [128, 512], BF16) for i in range(3)]
                OA = sb(st, "OA", [128, 512], F32)
                RDf = sb(st, "RDf", [64, 512], F32)
                ON = sb(st, "ON", [64, 512], BF16)
                OP("dve", "memset", _writes=[VHA.r], _p=(VHA[:], 1.0))
                OP("dve", "memset", _writes=[VHOA.r], _p=(VHOA[:], 1.0))
                pt_i = [0]
                for hp2 in range(int(os.environ.get("KFOXH", "8"))):
                    for q4 in range(4):
                        fw.dma("sp", KH[:, q4 * 4096:(q4 + 1) * 4096], KS[hp2, :, q4 * 4096:(q4 + 1) * 4096], reads=[R_KS], writes=[KH.r])
                    for q4 in range(8):
                        for h2 in range(2):
                            fw.dma("sp", VHA[:, q4 * 16:(q4 + 1) * 16, h2, 0:64],
                                   VS[q4 * 2048:(q4 + 1) * 2048, hp2 * 128 + h2 * 64:hp2 * 128 + h2 * 64 + 64].rearrange("(t p) d -> p t d", p=128), reads=[R_VS], writes=[VHA.r])
                    fw.dma("sp", KHO[:], KSO[hp2, :, 0:BLK], reads=[R_KSO], writes=[KHO.r])
                    for h2 in range(2):
                        fw.dma("sp", VHOA[:, :, h2, 0:64], VSO[:, hp2 * 128 + h2 * 64:hp2 * 128 + h2 * 64 + 64].rearrange("(t p) d -> p t d", p=128), reads=[R_VSO], writes=[VHOA.r])
                    fw.dma("sp", QH[:], QSO[hp2, :, 0:BLK], reads=[R_QSO], writes=[QH.r])
                    for hh in range(2):
                        hd = 2 * hp2 + hh
                        hpp = hh * 64
                        sbanks = (0, 1) if hh == 0 else (2, 3)
                        mbanks = (0, 1)
                        for qi in range(4):
                            xq = XQ[:, 4 * qi, :]
                            OP("dve", "tensor_tensor", _reads=[NB0.r, XQ.r], _writes=[NBq.r], _a=dict(out=NBq[:, :, hd:hd + 1], in0=NB0[:, :, hd:hd + 1],
                                                                                              in1=xq[:, hd:hd + 1].unsqueeze(1).to_broadcast([128, 128, 1]), op=ALU.add))
                            OP("dve", "tensor_tensor", _reads=[DCO.r, XQ.r], _writes=[NBOq.r], _a=dict(out=NBOq[:, :, hd:hd + 1], in0=xq[:, hd:hd + 1].unsqueeze(1).to_broadcast([128, 16, 1]),
                                                                                               in1=DCO[:, :, hd:hd + 1], op=ALU.subtract))
                            po = PS[4 + (qi % 2)]
                            pdn = PS[6 + (qi % 2)]
                            qs = qi * 512
                            nown = 4 * qi + 4
                            first = True
                            for kt in range(128 + nown):
                                own_t = kt >= 128
                                ko = kt - 128
                                d = max(0, ko - 4 * qi) if own_t else 0
                                c0 = 128 * d
                                last = (kt == 128 + nown - 1)
                                ps = next_ps(sbanks)
                                if own_t:
                                    lhs = KHO[hpp:hpp + 64, ko * 128:(ko + 1) * 128]
                                    kres = KHO.r
                                    bias = NBOq[:, ko, hd:hd + 1]
                                    bres = NBOq.r
                                    vl = VHOA[:, ko, hh, :]
                                    vres = VHOA.r
                                else:
                                    lhs = KH[hpp:hpp + 64, kt * 128:(kt + 1) * 128]
                                    kres = KH.r
                                    bias = NBq[:, kt, hd:hd + 1]
                                    bres = NBq.r
                                    vl = VHA[:, kt, hh, :]
                                    vres = VHA.r
                                mm(ps[:, c0:512], lhs, QH[hpp:hpp + 64, qs + c0:qs + 512], True, True, [kres, QH.r], ps)
                                pt = PT[pt_i[0] % 3]
                                pt_i[0] += 1
                                OP("act", "activation", _reads=[ps.r, bres], _writes=[pt.r], _a=dict(out=pt[:, c0:512], in_=ps[:, c0:512], func=AF.Exp, bias=bias, scale=0.125))
                                if own_t and ko >= 4 * qi:
                                    OP("dve", "tensor_tensor", _reads=[pt.r, CSTB.r], _writes=[pt.r], _a=dict(out=pt[:, c0:c0 + 128], in0=pt[:, c0:c0 + 128], in1=TRI_B, op=ALU.mult))
                                mm(po[0:64, c0:512], vl[:, 0:64], pt[:, c0:512], first, last, [vres, pt.r], po)
                                mm(pdn[0:64, c0:512], ONES_B[:, 0:64], pt[:, c0:512], first, last, [CSTB.r, pt.r], pdn)
                                first = False
                            OP("dve", "reciprocal", _reads=[pdn.r], _writes=[RDf.r], _a=dict(out=RDf[:], in_=pdn[0:64, :]))
                            if hh == 0:
                                OP("dve", "tensor_tensor", _reads=[po.r, RDf.r], _writes=[XB.r], _a=dict(out=XB[0:64, hp2, qs:qs + 512], in0=po[0:64, :], in1=RDf[:], op=ALU.mult))
                            else:
                                OP("dve", "tensor_tensor", _reads=[po.r, RDf.r], _writes=[ON.r], _a=dict(out=ON[:], in0=po[0:64, :], in1=RDf[:], op=ALU.mult))
                                psh = next_ps(mbanks)
                                mm(psh[64:128, :], CSTB[0:64, 256:320], ON[:], True, True, [CSTB.r, ON.r], psh)
                                OP("act", "activation", _reads=[psh.r], _writes=[XB.r], _a=dict(out=XB[64:128, hp2, qs:qs + 512], in_=psh[64:128, :], func=AF.Copy))
                fw.barrier()

        def fox_sample():
            with contextlib.ExitStack() as st:
                LFC = sb(st, "LFC", [128, 32, 16], F32)
                NBc = sb(st, "NBc", [128, 32, 16], F32)
                NBn = sb(st, "NBn", [16, 16], F32)
                KC_ = sb(st, "KCc", [128, PAST], BF16)
                VC_ = sb(st, "VCc", [128, 32, 128], BF16)
                KN = sb(st, "KN", [128, NS], BF16)
                VN = sb(st, "VN2", [16, 4, D], BF16)
                QS_ = sb(st, "QS_", [128, NS], BF16)
                TMPs = sb(st, "TMPs2", [128, 32, 16], F32)
                PTs = sb(st, "PTs", [128, 33, 16], BF16)
                TN = sb(st, "TN", [16, 16], F32)
                RDs = sb(st, "RDs2", [128, 16], F32)
                fw.dma("sp", VN[:], VSNd, reads=[R_VSNd], writes=[VN.r])
                for s_ in range(4):
                    with contextlib.ExitStack() as s1:
                        fw.dma("sp", LFC[:], cc_lf[s_].rearrange("(t p) h -> p t h", p=128), writes=[LFC.r])
                        DCc, CARXc, INCc = tile_cumsum(s1, LFC, LFC.r, 32, f"c{s_}")
                        OP("dve", "tensor_tensor", _reads=[DCc.r, INCc.r], _writes=[NBc.r], _a=dict(out=NBc[:], in0=INCc[:, 31, :].unsqueeze(1).to_broadcast([128, 32, 16]), in1=DCc[:], op=ALU.subtract))
                        pn = next_ps()
                        mm(pn[0:16, 0:16], TRI_F[0:16, 0:16], LFN[0:16, s_, :], True, True, [CST.r, LFN.r], pn)
                        OP("dve", "tensor_scalar", _reads=[pn.r], _writes=[NBn.r], _a=dict(out=NBn[:], in0=pn[0:16, 0:16], scalar1=-1.0, scalar2=None, op0=ALU.mult))
                        fw.barrier()
                    for ch in range(int(os.environ.get("KFOXH", "8"))):
                        fw.dma("pool", KC_[:], cc_k[s_, ch * 128:(ch + 1) * 128, :], writes=[KC_.r], max_dma_last_dim=8192)
                        fw.dma("pool", VC_[:], cc_v[s_, :, ch * 128:(ch + 1) * 128].rearrange("(t p) c -> p t c", p=128), writes=[VC_.r])
                        fw.dma("sp", KN[:], KSO[ch, :, BLK:NT], reads=[R_KSO], writes=[KN.r])
                        fw.dma("sp", QS_[:], QSO[ch, :, BLK:NT], reads=[R_QSO], writes=[QS_.r])
                        for hh in range(2):
                            hd = 2 * ch + hh
                            hpp = hh * 64
                            sbanks = (0, 1) if hh == 0 else (2, 3)
                            ps = next_ps(sbanks)
                            q_ap = QS_[hpp:hpp + 64, 16 * s_:16 * s_ + 16]
                            for t in range(32):
                                mm(ps[:, t * 16:(t + 1) * 16], KC_[hpp:hpp + 64, t * 128:(t + 1) * 128], q_ap, True, True, [KC_.r, QS_.r], ps)
                            psn = next_ps(sbanks)
                            mm(psn[0:16, 0:16], KN[hpp:hpp + 64, 16 * s_:16 * s_ + 16], q_ap, True, True, [KN.r, QS_.r], psn)
                            OP("dve", "scalar_tensor_tensor", _reads=[ps.r, NBc.r], _writes=[TMPs.r], _a=dict(out=TMPs[:], in0=ps[:].rearrange("p (t q) -> p t q", q=16), scalar=0.125,
                                                                                              in1=NBc[:, :, hd:hd + 1].to_broadcast([128, 32, 16]), op0=ALU.mult, op1=ALU.add))
                            OP("act", "activation", _reads=[TMPs.r], _writes=[PTs.r], _a=dict(out=PTs[:, 0:32, :], in_=TMPs[:], func=AF.Exp))
                            OP("dve", "scalar_tensor_tensor", _reads=[psn.r, NBn.r], _writes=[TN.r], _a=dict(out=TN[:], in0=psn[0:16, 0:16], scalar=0.125,
                                                                                             in1=NBn[:, hd:hd + 1].to_broadcast([16, 16]), op0=ALU.mult, op1=ALU.add))
                            OP("act", "activation", _reads=[TN.r], _writes=[TN.r], _a=dict(out=TN[:], in_=TN[:], func=AF.Exp))
                            OP("dve", "tensor_tensor", _reads=[TN.r, CST.r, PTs.r], _writes=[PTs.r], _a=dict(out=PTs[0:16, 32, :], in0=TN[:], in1=TRI_F[0:16, 0:16], op=ALU.mult))
                            po = next_ps((4, 5))
                            pd = next_ps((6, 7))
                            for t in range(33):
                                lv = VC_[:, t, hpp:hpp + 64] if t < 32 else VN[0:16, s_, hd * 64:(hd + 1) * 64]
                                rp = PTs[:, t, :] if t < 32 else PTs[0:16, 32, :]
                                mm(po[hpp:hpp + 64, 0:16], lv, rp, t == 0, t == 32, [VC_.r, VN.r, PTs.r], po)
                            for t in range(33):
                                lo_ = ONES_B[:, 0:64] if t < 32 else CSTB[0:16, 0:64]
                                rp = PTs[:, t, :] if t < 32 else PTs[0:16, 32, :]
                                mm(pd[hpp:hpp + 64, 0:16], lo_, rp, t == 0, t == 32, [CSTB.r, PTs.r], pd)
                            OP("dve", "reciprocal", _reads=[pd.r], _writes=[RDs.r], _a=dict(out=RDs[hpp:hpp + 64, :], in_=pd[hpp:hpp + 64, 0:16]))
                            OP("dve", "tensor_tensor", _reads=[po.r, RDs.r], _writes=[XB.r], _a=dict(out=XB[hpp:hpp + 64, ch, BLK + 16 * s_:BLK + 16 * s_ + 16], in0=po[hpp:hpp + 64, 0:16],
                                                                                          in1=RDs[hpp:hpp + 64, :], op=ALU.mult))
                fw.barrier()

        def moe(XRES):
            with contextlib.ExitStack() as st:
                LG = sb(st, "LG", [128, 17, 8], F32)
                CMB = sb(st, "CMB", [128, 17, 8], F32)
                WR = sb(st, "WR", [128, 8, 8], F32)
                BRR = sb(st, "BRR", [128, 8], F32)
                M1 = sb(st, "M1", [128, 17], F32)
                M2 = sb(st, "M2", [128, 17], F32)
                T8 = sb(st, "T8", [128, 17, 8], F32)
                E8 = sb(st, "E8", [128, 17, 8], F32)
                GATE = sb(st, "GATE", [128, NT], BF16)
                DG = [sb(st, f"DG{i}", [128, 128], F32) for i in range(2)]
                fw.dma("sp", WR[:], w_router.rearrange("(k p) e -> p k e", p=128), writes=[WR.r])
                fw.dma("sp", BRR[:], br_rep, writes=[BRR.r])
                OP("dve", "memset", _writes=[LG.r], _p=(LG[:], 0.0))
                for t in range(17):
                    tw = 128 if t < 16 else NS
                    ps = next_ps()
                    for kc in range(8):
                        mm(ps[0:tw, 0:8], XRES[:, kc, t * 128:t * 128 + tw], WR[:, kc, :], kc == 0, kc == 7, [XRES.r, WR.r], ps)
                    OP("dve", "tensor_tensor", _reads=[ps.r, BRR.r], _writes=[LG.r], _a=dict(out=LG[0:tw, t, :], in0=ps[0:tw, 0:8], in1=BRR[0:tw, :], op=ALU.add))
                OP("dve", "tensor_reduce", _reads=[LG.r], _writes=[M1.r], _a=dict(out=M1[:], in_=LG[:], axis=AX.X, op=ALU.max))
                OP("dve", "tensor_tensor", _reads=[LG.r, M1.r], _writes=[T8.r], _a=dict(out=T8[:], in0=LG[:], in1=M1[:].unsqueeze(2).to_broadcast([128, 17, 8]), op=ALU.is_equal))
                OP("dve", "scalar_tensor_tensor", _reads=[T8.r, LG.r], _writes=[T8.r], _a=dict(out=T8[:], in0=T8[:], scalar=-1e30, in1=LG[:], op0=ALU.mult, op1=ALU.add))
                OP("dve", "tensor_reduce", _reads=[T8.r], _writes=[M2.r], _a=dict(out=M2[:], in_=T8[:], axis=AX.X, op=ALU.max))
                OP("dve", "tensor_tensor", _reads=[LG.r, M2.r], _writes=[T8.r], _a=dict(out=T8[:], in0=LG[:], in1=M2[:].unsqueeze(2).to_broadcast([128, 17, 8]), op=ALU.is_ge))
                OP("dve", "tensor_tensor", _reads=[LG.r, M1.r], _writes=[E8.r], _a=dict(out=E8[:], in0=LG[:], in1=M1[:].unsqueeze(2).to_broadcast([128, 17, 8]), op=ALU.subtract))
                OP("act", "activation", _reads=[E8.r], _writes=[E8.r], _a=dict(out=E8[:], in_=E8[:], func=AF.Exp))
                OP("dve", "tensor_tensor", _reads=[E8.r, T8.r], _writes=[E8.r], _a=dict(out=E8[:], in0=E8[:], in1=T8[:], op=ALU.mult))
                OP("dve", "tensor_reduce", _reads=[E8.r], _writes=[M2.r], _a=dict(out=M2[:], in_=E8[:], axis=AX.X, op=ALU.add))
                OP("dve", "reciprocal", _reads=[M2.r], _writes=[M2.r], _a=dict(out=M2[:], in_=M2[:]))
                OP("dve", "tensor_tensor", _reads=[E8.r, M2.r], _writes=[CMB.r], _a=dict(out=CMB[:], in0=E8[:], in1=M2[:].unsqueeze(2).to_broadcast([128, 17, 8]), op=ALU.mult))
                for kc in range(8):
                    OP("act", "activation", _reads=[XRES.r], _writes=[XRES.r], _a=dict(out=XRES[:, kc, :], in_=XRES[:, kc, :], func=AF.Identity, scale=ALPHA))
                global_gate = GATE
                for e in range(int(os.environ.get("KEXP", "8"))):
                    dg_i = 0
                    for t in range(17):
                        tw = 128 if t < 16 else NS
                        dg = DG[dg_i % 2]
                        dg_i += 1
                        OP("dve", "tensor_scalar", _reads=[CST.r, CMB.r], _writes=[dg.r], _a=dict(out=dg[0:tw, 0:tw], in0=IDENT_F[0:tw, 0:tw], scalar1=CMB[0:tw, t, e:e + 1], scalar2=None, op0=ALU.mult))
                        ps = next_ps()
                        mm(ps[:, 0:tw], ONES_F[0:tw, :], dg[0:tw, 0:tw], True, True, [CST.r, dg.r], ps)
                        OP("act", "activation", _reads=[ps.r], _writes=[GATE.r], _a=dict(out=GATE[:, t * 128:t * 128 + tw], in_=ps[:, 0:tw], func=AF.Copy))
                    ffn(XRES, moe_w1[e], moe_w3[e], moe_w2[e], gate=GATE)
                fw.barrier()

        L1 = set(os.environ.get("KL1", "foxp,foxs,rest").split(","))
        if "own" in PASSES and "l1" in PARTS:
            if "foxp" in L1 or "cum" in L1:
                fox_prompt()
            if "foxs" in L1:
                fox_sample()
            with contextlib.suppress(SkipBlock), contextlib.ExitStack() as sC:
                if "rest" not in L1:
                    raise SkipBlock()
                XRES = sb(sC, "XRES1", [128, 8, NT], F32)
                fw.dma("sp", XRES[:], X1S, reads=[R_X1S], writes=[XRES.r])
                linear_fm(w_out_c, 8, 0, D, lambda kc, c0, cw: XB[:, kc, c0:c0 + cw], [XB.r], COLT, resid_evac(XRES))
                fw.barrier()
                layernorm(XRES, 3)
                cross_attention(XRES, 1, True)
                layernorm(XRES, 4)
                moe(XRES)
                layernorm(XRES, 5)
                fw.dma("sp", o_yT.rearrange("(k p) n -> p k n", p=128), XRES[:], reads=[XRES.r], writes=[R_OUT["yT"]])
                fw.barrier()

        fw.barrier()
        for s_ in fw.sems.values():
            nc.gpsimd.sem_clear(s_)
        nc.all_engine_barrier()
        with nc.Block() as block:
            fw.emit(block)
        nc.all_engine_barrier()
        for s_ in fw.sems.values():
            nc.gpsimd.sem_clear(s_)
    return nc


def _host_inputs(inp):
    f = lambda a: np.ascontiguousarray(a, dtype=np.float32)
    xp = inp["x_prompt"][0]
    xT = np.zeros((D, HALO + SEQ), np.float32)
    xT[:, HALO:] = xp.T
    rel = inp["rel_bias_a"][0]
    kk = np.arange(128)[:, None]
    qq = np.arange(128)[None, :]
    BT = np.zeros((128, 5, 8, 128), np.float32)
    for j in range(5):
        kpos = 128 * j + kk
        qpos = 512 + qq
        relidx = np.clip(qpos - kpos, -128, 128) + 128
        cq = qpos // 64
        ck = kpos // 64
        vis = (ck >= cq - 8) & (ck <= cq)
        for h in range(8):
            BT[:, j, (h % 2) * 4 + h // 2, :] = np.where(vis, rel[h][relidx], NEG)
    BTS = np.full((128, 5, 8, 16), NEG, np.float32)
    for j in range(5):
        nk = 128 if j < 4 else 16
        kpos = 128 * j + np.arange(nk)[:, None]
        qpos = 512 + np.arange(16)[None, :]
        relidx = np.clip(qpos - kpos, -128, 128) + 128
        for h in range(8):
            BTS[:nk, j, (h % 2) * 4 + h // 2, :] = rel[h][relidx]
    ones = np.ones((128, 128), np.float32)
    tri = (np.arange(128)[:, None] <= np.arange(128)[None, :]).astype(np.float32)
    ident = np.eye(128, dtype=np.float32)
    sel65 = np.zeros((128, 128), np.float32)
    sel65[64, :] = 1.0
    CONST = np.concatenate([ones, tri, ident, sel65], axis=1)
    lng = f(inp["ln_g"].reshape(2, 3, 8, 128).transpose(3, 0, 1, 2).reshape(128, 48))
    lnb = f(inp["ln_b"].reshape(2, 3, 8, 128).transpose(3, 0, 1, 2).reshape(128, 48))
    common = {
        "xT": xT, "memT": f(inp["mem_prompt"][0].T), "w_in_ab": f(inp["w_in_ab"][0]),
        "BT": f(BT.reshape(128, -1)), "BTS": f(BTS.reshape(128, -1)), "pool_w": f(inp["pool_w"][0]),
        "pool_sc": f(inp["pool_scale"][0].reshape(4, 128).T), "w_out_ab": f(inp["w_out_ab"][0]),
        "w_in_c": f(inp["w_in_c"][0]), "bf_rep": f(np.broadcast_to(inp["b_f"][0][None, :], (128, 16))),
        "w_out_c": f(inp["w_out_c"][0]), "w_xq": f(inp["w_xq"]), "w_xk": f(inp["w_xk"]), "w_xv": f(inp["w_xv"]),
        "w_xo": f(inp["w_xo"]), "ln_g": lng, "ln_b": lnb, "ffn_w1": f(inp["ffn_w1"][0]), "ffn_w3": f(inp["ffn_w3"][0]),
        "ffn_w2": f(inp["ffn_w2"][0]), "w_router": f(inp["w_router"][0]),
        "br_rep": f(np.broadcast_to(inp["b_router"][0][None, :], (128, 8))),
        "moe_w1": f(inp["moe_w1"][0]), "moe_w3": f(inp["moe_w3"][0]), "moe_w2": f(inp["moe_w2"][0]), "CONST": CONST,
    }
    maps = []
    for c in range(NCORES):
        m = dict(common)
        sq = slice(4 * c, 4 * c + 4)
        xo = np.zeros((D, NA), np.float32)
        xo[:, 0:HALO + BLK] = xT[:, c * BLK:c * BLK + HALO + BLK]
        xo[:, HALO + BLK:] = inp["x_sample"][sq].reshape(NS, D).T
        m["xo"] = xo
        m["ca_k"] = f(inp["cache_a_k"][0, sq].reshape(4, 512, 512).transpose(0, 2, 1))
        m["ca_v"] = f(inp["cache_a_v"][0, sq].reshape(4, 512, 512))
        m["sp_T"] = f(inp["state_pool"][0, sq].transpose(0, 2, 1))
        m["cc_k"] = f(inp["cache_c_k"][0, sq].reshape(4, PAST, 1024).transpose(0, 2, 1))
        m["cc_v"] = f(inp["cache_c_v"][0, sq].reshape(4, PAST, 1024))
        m["cc_lf"] = f(inp["cache_c_logf"][0, sq])
        m["cm_k"] = f(inp["cache_mem_k"][:, sq].reshape(2, 4, 256, 1024).transpose(0, 1, 3, 2))
        m["cm_v"] = f(inp["cache_mem_v"][:, sq].reshape(2, 4, 256, 1024))
        valt = np.ones((9, 128, 21), np.float32)
        valt[0, :, 0:4] = 0.0
        if c == 0:
            valt[8, :, 0:4] = 0.0
        m["VALT"] = valt
        fix = np.zeros((9, 128, 4, 16), np.float32)
        for g, w in enumerate((2, 4, 8, 16)):
            fix[:, :, g, :] = 1.0 / w
            first = 1.0 / np.minimum(float(w), np.arange(16) + 1.0)
            fix[0, :, g, :] = first[None, :]
            if c == 0:
                fix[8, :, g, :] = first[None, :]
        m["FIX"] = fix.reshape(9, 128, 64)
        invis = np.zeros((128, 128), np.float32)
        invis[:, 16 * c:] = NEG
        m["INVIS"] = invis
        selt = np.zeros((128, 128), np.float32)
        selt[:, 16 * c] = 1.0
        m["SELT"] = selt
        maps.append(m)
    return maps


_NC_CACHE = {}


def kernel(**inputs):
    inp = {k: np.asarray(v) for k, v in inputs.items()}
    if "nc" not in _NC_CACHE:
        _NC_CACHE["nc"] = build_program()
    nc = _NC_CACHE["nc"]
    maps = _host_inputs(inp)
    res = run_bass_kernel_spmd(nc, maps, core_ids=list(range(NCORES)))
    R = res.results
    yT = np.stack([R[c]["yT"] for c in range(NCORES)])
    y_prompt = np.ascontiguousarray(yT[:, :, :BLK].transpose(0, 2, 1).reshape(1, SEQ, D))
    y_sample = np.ascontiguousarray(yT[:, :, BLK:].transpose(0, 2, 1).reshape(32, 16, D))
    r0 = R[0]
    p_a_k = np.ascontiguousarray(r0["p_a_kT"].T).reshape(1, 1, 512, 8, 64)
    p_a_v = r0["p_a_v"].reshape(1, 1, 512, 8, 64)
    p_pool = np.ascontiguousarray(r0["p_poolT"].T).reshape(1, 1, 15, 512)
    p_c_k = np.ascontiguousarray(r0["p_c_kT"].T).reshape(1, 1, SEQ, 16, 64)
    p_c_v = r0["p_c_v"].reshape(1, 1, SEQ, 16, 64)
    p_c_lf = r0["p_c_lf"].reshape(1, 1, SEQ, 16)
    p_mem_k = np.ascontiguousarray(r0["p_mem_kT"].transpose(0, 2, 1)).reshape(2, 1, 256, 4, 256)
    p_mem_v = r0["p_mem_v"].reshape(2, 1, 256, 4, 256)
    s_a_k = np.concatenate([R[c]["s_a_kT"].T for c in range(NCORES)], 0).reshape(1, 32, 16, 8, 64)
    s_a_v = np.concatenate([R[c]["s_a_v"] for c in range(NCORES)], 0).reshape(1, 32, 16, 8, 64)
    s_pool = np.concatenate([R[c]["s_poolT"].transpose(0, 2, 1) for c in range(NCORES)], 0).reshape(1, 32, 15, 512)
    s_c_k = np.concatenate([R[c]["s_c_kT"].T for c in range(NCORES)], 0).reshape(1, 32, 16, 16, 64)
    s_c_v = np.concatenate([R[c]["s_c_v"] for c in range(NCORES)], 0).reshape(1, 32, 16, 16, 64)
    s_c_lf = np.concatenate([R[c]["s_c_lf"] for c in range(NCORES)], 0).reshape(1, 32, 16, 16)
    outs = (y_prompt, y_sample, p_a_k, p_a_v, p_pool, p_c_k, p_c_v, p_c_lf, p_mem_k, p_mem_v,
            s_a_k, s_a_v, s_pool, s_c_k, s_c_v, s_c_lf)
    return tuple(np.ascontiguousarray(o, dtype=np.float32) for o in outs)
```

```python
import contextlib
import numpy as np
import concourse.bass as bass
import concourse.mybir as mybir
from concourse.bass_utils import run_bass_kernel_spmd

F32 = mybir.dt.float32
BF16 = mybir.dt.bfloat16
ALU = mybir.AluOpType
AF = mybir.ActivationFunctionType
AX = mybir.AxisListType

NCORES = 8
D = 1024
SEQ = 16384
BLK = 2048
NBLK = 8
NS = 64
NT = BLK + NS
HALO = 512
NA = HALO + NT
DFF = 2816
ALPHA = 4.0 ** 0.25
LN_EPS = 1e-5
NEG = -30000.0
PAST = 4096
COLT = [(0, 512), (512, 512), (1024, 512), (1536, 512), (2048, 64)]
COLA = [(0, 512), (512, 512), (1024, 512), (1536, 512), (2048, 512), (2560, 64)]
STAGE = 1
import os
PARTS = set(os.environ.get("KPARTS", "mem,pool,band,sband,outproj,ln1,xattn,ln2,ffn,ln3,kv,l1").split(","))
PASSES = os.environ.get("KPASSES", "0,1,2,3,4,5,6,7,own").split(",")


class Res:
    __slots__ = ("name", "w", "r", "excl")

    def __init__(self, name, excl=False):
        self.name = name
        self.w = None
        self.r = {}
        self.excl = excl


class SkipBlock(Exception):
    pass


class Eng:
    def __init__(self, name, sem):
        self.name = name
        self.sem = sem
        self.count = 0
        self.seen = {}
        self.ops = []
        self.dma_sems = []
        self.dma_rr = 0


class FW:
    def __init__(self, nc, stack, ndma=8):
        self.nc = nc
        self.engs = {}
        self.sems = {}
        for name in ("pe", "act", "dve", "pool", "sp"):
            s = stack.enter_context(nc.semaphore("prog_" + name))
            self.sems[id(s)] = s
            self.engs[name] = Eng(name, s)
        for name in ("sp", "pool"):
            e = self.engs[name]
            for i in range(ndma):
                s = stack.enter_context(nc.semaphore(f"dma_{name}_{i}"))
                self.sems[id(s)] = s
                e.dma_sems.append([s, 0])

    def _deps(self, eng, reads, writes, skip_self_waw=False):
        need = {}

        def add(tok):
            if tok is None:
                return
            k, v = tok
            if need.get(k, 0) < v:
                need[k] = v
        for r in reads:
            add(r.w)
            if r.excl:
                for k, v in r.r.items():
                    if k != id(eng.sem):
                        add((k, v))
        for w in writes:
            if not (skip_self_waw and w.w is not None and w.w[0] == id(eng.sem)):
                add(w.w)
            for k, v in w.r.items():
                add((k, v))
        waits = []
        for k, v in need.items():
            if eng.seen.get(k, 0) < v:
                eng.seen[k] = v
                waits.append((self.sems[k], v))
        return waits

    def _commit(self, tok, reads, writes):
        for w in writes:
            w.w = tok
            w.r = {}
        for r in reads:
            if r.r.get(tok[0], 0) < tok[1]:
                r.r[tok[0]] = tok[1]

    def op(self, engname, fn, reads=(), writes=(), accum=False):
        eng = self.engs[engname]
        waits = self._deps(eng, reads, writes, skip_self_waw=accum)
        eng.count += 1
        tok = (id(eng.sem), eng.count)
        eng.ops.append((waits, fn, (eng.sem, 1)))
        self._commit(tok, reads, writes)
        return tok

    def dma(self, engname, out, in_, reads=(), writes=(), **kw):
        eng = self.engs[engname]
        waits = self._deps(eng, reads, writes)
        slot = eng.dma_sems[eng.dma_rr % len(eng.dma_sems)]
        eng.dma_rr += 1
        sem, cnt = slot
        if cnt > 0 and eng.seen.get(id(sem), 0) < cnt:
            eng.seen[id(sem)] = cnt
            waits.append((sem, cnt))
        slot[1] = cnt + 16
        tok = (id(sem), cnt + 16)

        def fn(h, out=out, in_=in_, kw=kw):
            return h.dma_start(out=out, in_=in_, **kw)
        eng.ops.append((waits, fn, (sem, 16)))
        self._commit(tok, reads, writes)
        return tok

    def barrier(self):
        targets = []
        for e in self.engs.values():
            if e.count:
                targets.append((id(e.sem), e.count))
            for s, c in e.dma_sems:
                if c:
                    targets.append((id(s), c))
        for e in self.engs.values():
            waits = []
            for k, v in targets:
                if e.seen.get(k, 0) < v:
                    e.seen[k] = v
                    waits.append((self.sems[k], v))
            if waits:
                e.ops.append((waits, None, None))

    def emit(self, block):
        handles = {"pe": "tensor", "act": "scalar", "dve": "vector", "pool": "gpsimd", "sp": "sync"}
        for name, eng in self.engs.items():
            def body(h, eng=eng):
                for waits, fn, inc in eng.ops:
                    for (s, v) in waits:
                        h.wait_ge(s, v)
                    if fn is not None:
                        if isinstance(fn, tuple):
                            name_, args_, kw_ = fn
                            ins = getattr(h, name_)(*args_, **kw_)
                        else:
                            ins = fn(h)
                        ins.then_inc(inc[0], inc[1])
            getattr(block, handles[name])(body)


class T:
    def __init__(self, t, name, excl=False):
        self.t = t
        self.r = Res(name, excl)

    def __getitem__(self, k):
        return self.t[k]


def build_program():
    nc = bass.Bass("TRN2", target_bir_lowering=False)
    IN = {}
    OUT = {}

    def din(name, shape, dt=F32):
        IN[name] = nc.dram_tensor(name, list(shape), dt, kind="ExternalInput").ap()
        return IN[name]

    def dout(name, shape, dt=F32):
        OUT[name] = nc.dram_tensor(name, list(shape), dt, kind="ExternalOutput").ap()
        return OUT[name]

    xT = din("xT", [D, HALO + SEQ])
    xo = din("xo", [D, NA])
    ca_k = din("ca_k", [4, 512, 512])
    ca_v = din("ca_v", [4, 512, 512])
    sp_T = din("sp_T", [4, 512, 15])
    cc_k = din("cc_k", [4, 1024, PAST])
    cc_v = din("cc_v", [4, PAST, 1024])
    cc_lf = din("cc_lf", [4, PAST, 16])
    cm_k = din("cm_k", [2, 4, 1024, 256])
    cm_v = din("cm_v", [2, 4, 256, 1024])
    memT = din("memT", [D, 256])
    w_in_ab = din("w_in_ab", [D, 2048])
    BTd = din("BT", [128, 5 * 8 * 128])
    BTSd = din("BTS", [128, 5 * 8 * 16])
    pool_w = din("pool_w", [4, 128, 128])
    pool_sc = din("pool_sc", [128, 4])
    w_out_ab = din("w_out_ab", [D, D])
    w_in_c = din("w_in_c", [D, 3088])
    bf_rep = din("bf_rep", [128, 16])
    w_out_c = din("w_out_c", [D, D])
    w_xq = din("w_xq", [2, D, D])
    w_xk = din("w_xk", [2, D, D])
    w_xv = din("w_xv", [2, D, D])
    w_xo = din("w_xo", [2, D, D])
    ln_g = din("ln_g", [128, 48])
    ln_b = din("ln_b", [128, 48])
    ffn_w1 = din("ffn_w1", [D, DFF])
    ffn_w3 = din("ffn_w3", [D, DFF])
    ffn_w2 = din("ffn_w2", [DFF, D])
    w_router = din("w_router", [D, 8])
    br_rep = din("br_rep", [128, 8])
    moe_w1 = din("moe_w1", [8, D, DFF])
    moe_w3 = din("moe_w3", [8, D, DFF])
    moe_w2 = din("moe_w2", [8, DFF, D])
    VALT = din("VALT", [9, 128, 21])
    FIXd = din("FIX", [9, 128, 64])
    INVIS = din("INVIS", [128, 128])
    SELT = din("SELT", [128, 128])
    CONST = din("CONST", [128, 4 * 128])

    o_yT = dout("yT", [D, NT])
    o_pak = dout("p_a_kT", [512, 512])
    o_pav = dout("p_a_v", [512, 512])
    o_ppool = dout("p_poolT", [512, 15])
    o_pck = dout("p_c_kT", [D, SEQ])
    o_pcv = dout("p_c_v", [SEQ, D])
    o_pclf = dout("p_c_lf", [SEQ, 16])
    o_pmk = dout("p_mem_kT", [2, D, 256])
    o_pmv = dout("p_mem_v", [2, 256, D])
    o_sak = dout("s_a_kT", [512, NS])
    o_sav = dout("s_a_v", [NS, 512])
    o_spool = dout("s_poolT", [4, 512, 15])
    o_sck = dout("s_c_kT", [D, NS])
    o_scv = dout("s_c_v", [NS, D])
    o_sclf = dout("s_c_lf", [NS, 16])
    R_OUT = {k: Res("out_" + k) for k in OUT}

    KS = nc.dram_tensor("KS", [8, 128, SEQ], BF16).ap()
    VS = nc.dram_tensor("VS", [SEQ, D], BF16).ap()
    KSO = nc.dram_tensor("KSO", [8, 128, NT], BF16).ap()
    VSO = nc.dram_tensor("VSO", [BLK, D], BF16).ap()
    QSO = nc.dram_tensor("QSO", [8, 128, NT], BF16).ap()
    X1S = nc.dram_tensor("X1S", [128, 8, NT], F32).ap()
    MKD = nc.dram_tensor("MKD", [2, 128, 8, 256], BF16).ap()
    MVD = nc.dram_tensor("MVD", [2, 128, 2, 1024], BF16).ap()
    R_MKD, R_MVD = Res("MKD"), Res("MVD")
    VSNd = nc.dram_tensor("VSNd", [16, 4, D], BF16).ap()
    R_VSNd = Res("VSNd")
    R_KS, R_VS, R_KSO, R_VSO, R_QSO, R_X1S = (Res(n) for n in ("KS", "VS", "KSO", "VSO", "QSO", "X1S"))

    with contextlib.ExitStack() as top:
        fw = FW(nc, top)

        uid = [0]

        def sb(stack, name, shape, dt):
            uid[0] += 1
            nm = f"sb{uid[0]}_{name}"
            return T(stack.enter_context(nc.sbuf_tensor(nm, list(shape), dt)), nm)

        PS = [T(top.enter_context(nc.psum_tensor(f"ps{i}", [128, 512], F32)), f"ps{i}", excl=True) for i in range(8)]
        ps_rr = [0]

        def next_ps(pool=(0, 1, 2, 3, 4, 5, 6, 7)):
            p = PS[pool[ps_rr[0] % len(pool)]]
            ps_rr[0] += 1
            return p

        XB = sb(top, "XB", [128, 8, NT], BF16)
        WB = [sb(top, f"WB{i}", [128, 4096], BF16) for i in range(3)]
        wb_rr = [0]

        def next_wb():
            w = WB[wb_rr[0] % len(WB)]
            wb_rr[0] += 1
            return w
        CST = sb(top, "CST", [128, 512], F32)
        CSTB = sb(top, "CSTB", [128, 512], BF16)
        LNG = sb(top, "LNG", [128, 48], F32)
        LNB = sb(top, "LNB", [128, 48], F32)
        BFR = sb(top, "BFR", [128, 16], F32)
        PSC = sb(top, "PSC", [128, 4], F32)
        PW = sb(top, "PW", [128, 4, 128], BF16)
        LF = sb(top, "LF", [128, 128, 16], F32)
        LFO = sb(top, "LFO", [128, 16, 16], F32)
        LFN = sb(top, "LFN", [16, 4, 16], F32)

        TOUCH = sb(top, "TOUCH", [1, 8 * 64], F32)
        KDBG = int(os.environ.get("KDBG", "99"))
        for ti_, (nm_, ap_) in enumerate(IN.items()):
            if KDBG < 1:
                break
            a_ = ap_
            while a_.ndim > 2:
                a_ = a_[0]
            fw.dma("sp", TOUCH[0:1, ti_ * 8:ti_ * 8 + 4], a_[0:1, 0:4], writes=[TOUCH.r])
        fw.dma("sp", CST[:], CONST, writes=[CST.r])
        fw.dma("pool", CSTB[:], CONST, writes=[CSTB.r])
        fw.dma("sp", LNG[:], ln_g, writes=[LNG.r])
        fw.dma("sp", LNB[:], ln_b, writes=[LNB.r])
        fw.dma("sp", BFR[:], bf_rep, writes=[BFR.r])
        fw.dma("sp", PSC[:], pool_sc, writes=[PSC.r])
        fw.dma("pool", PW[:], pool_w.rearrange("g c e -> c g e"), writes=[PW.r])
        fw.op("dve", ("memset", (LF[:], 0.0), {}), writes=[LF.r])
        ONES_B = CSTB[:, 0:128]
        TRI_B = CSTB[:, 128:256]
        ONES_F = CST[:, 0:128]
        TRI_F = CST[:, 128:256]
        IDENT_F = CST[:, 256:384]

        def OP(eng, name, _reads=(), _writes=(), _accum=False, _a=None, _p=()):
            fw.op(eng, (name, tuple(_p), dict(_a or {})), reads=_reads, writes=_writes, accum=_accum)

        def mm(ps_ap, lhsT, rhs, start, stop, reads, psT):
            fw.op("pe", ("matmul", (ps_ap, lhsT, rhs), dict(start=start, stop=stop)), reads=reads, writes=[psT.r], accum=True)

        def wload(dst_ap, src_ap, wbT, eng="pool"):
            fw.dma(eng, dst_ap, src_ap, writes=[wbT.r])

        def linear_fm(Wap, KC, o0, n_out, rhs_fn, rhs_res, col_tiles, evac_fn, kpart=128):
            gw_max = (4096 // KC) // 128 * 128
            og = 0
            while og < n_out:
                gw = min(gw_max, n_out - og)
                wb = next_wb()
                wv = wb[0:kpart, 0:KC * gw].rearrange("p (k n) -> p k n", n=gw)
                wload(wv, Wap[:, o0 + og:o0 + og + gw].rearrange("(k p) n -> p k n", p=kpart), wb)
                for oc in range(0, gw, 128):
                    m = min(128, gw - oc)
                    for (c0, cw) in col_tiles:
                        if KDBG < 3:
                            continue
                        ps = next_ps()
                        for kc in range(KC):
                            mm(ps[0:m, 0:cw], wv[:, kc, oc:oc + m], rhs_fn(kc, c0, cw), kc == 0, kc == KC - 1,
                               [wb.r] + rhs_res, ps)
                        if KDBG >= 4:
                            evac_fn(o0 + og + oc, m, c0, cw, ps)
                og += gw

        def linear_tm(Wap, KC, o0, n_out, lhsT_fn, lhs_res, tok_tiles, evac_fn):
            og = 0
            while og < n_out:
                gw = min(512, n_out - og)
                wb = next_wb()
                wv = wb[:, 0:KC * gw].rearrange("p (k n) -> p k n", n=gw)
                wload(wv, Wap[:, o0 + og:o0 + og + gw].rearrange("(k p) n -> p k n", p=128), wb)
                for ti, (t0, tw) in enumerate(tok_tiles):
                    if KDBG < 3:
                        continue
                    ps = next_ps()
                    for kc in range(KC):
                        mm(ps[0:tw, 0:gw], lhsT_fn(kc, t0, tw), wv[:, kc, 0:gw], kc == 0, kc == KC - 1,
                           [wb.r] + lhs_res, ps)
                    if KDBG >= 4:
                        evac_fn(ti, t0, tw, og, gw, ps)
                og += gw

        def layernorm(XRES, lidx):
            with contextlib.ExitStack() as st:
                CB = sb(st, "ln_cb", [128, 8, 512], BF16)
                SQ = sb(st, "ln_sq", [128, 8, 512], BF16)
                LM = sb(st, "ln_m", [128, 512], F32)
                LV = sb(st, "ln_v", [128, 512], F32)
                LR = sb(st, "ln_r", [128, 512], F32)
                T0 = sb(st, "ln_t0", [128, 512], F32)
                T1 = sb(st, "ln_t1", [128, 512], F32)
                TT = [T0, T1]
                for (c0, cw) in COLT:
                    for kc in range(8):
                        OP("act", "activation", _reads=[XRES.r], _writes=[CB.r], _a=dict(out=CB[:, kc, 0:cw], in_=XRES[:, kc, c0:c0 + cw], func=AF.Copy))
                        OP("act", "activation", _reads=[XRES.r], _writes=[SQ.r], _a=dict(out=SQ[:, kc, 0:cw], in_=XRES[:, kc, c0:c0 + cw], func=AF.Square))
                    p1 = next_ps()
                    p2 = next_ps()
                    for kc in range(8):
                        mm(p1[:, 0:cw], ONES_B, CB[:, kc, 0:cw], kc == 0, kc == 7, [CB.r, CSTB.r], p1)
                    for kc in range(8):
                        mm(p2[:, 0:cw], ONES_B, SQ[:, kc, 0:cw], kc == 0, kc == 7, [SQ.r, CSTB.r], p2)
                    OP("dve", "tensor_scalar", _reads=[p1.r], _writes=[LM.r], _a=dict(out=LM[:, 0:cw], in0=p1[:, 0:cw], scalar1=1.0 / D, scalar2=None, op0=ALU.mult))
                    OP("dve", "tensor_tensor", _reads=[LM.r], _writes=[LV.r], _a=dict(out=LV[:, 0:cw], in0=LM[:, 0:cw], in1=LM[:, 0:cw], op=ALU.mult))
                    OP("dve", "scalar_tensor_tensor", _reads=[p2.r, LV.r], _writes=[LV.r], _a=dict(out=LV[:, 0:cw], in0=p2[:, 0:cw], scalar=1.0 / D, in1=LV[:, 0:cw],
                                                                   op0=ALU.mult, op1=ALU.subtract))
                    OP("dve", "tensor_scalar", _reads=[LV.r], _writes=[LV.r], _a=dict(out=LV[:, 0:cw], in0=LV[:, 0:cw], scalar1=0.0, scalar2=LN_EPS, op0=ALU.max, op1=ALU.add))
                    OP("act", "activation", _reads=[LV.r], _writes=[LV.r], _a=dict(out=LV[:, 0:cw], in_=LV[:, 0:cw], func=AF.Sqrt))
                    OP("dve", "reciprocal", _reads=[LV.r], _writes=[LR.r], _a=dict(out=LR[:, 0:cw], in_=LV[:, 0:cw]))
                    for kc in range(8):
                        tt = TT[kc % 2]
                        gi = lidx * 8 + kc
                        OP("dve", "tensor_tensor", _reads=[XRES.r, LM.r], _writes=[tt.r], _a=dict(out=tt[:, 0:cw], in0=XRES[:, kc, c0:c0 + cw], in1=LM[:, 0:cw], op=ALU.subtract))
                        OP("dve", "tensor_tensor", _reads=[tt.r, LR.r], _writes=[tt.r], _a=dict(out=tt[:, 0:cw], in0=tt[:, 0:cw], in1=LR[:, 0:cw], op=ALU.mult))
                        OP("dve", "tensor_scalar", _reads=[tt.r, LNG.r, LNB.r], _writes=[XRES.r], _a=dict(out=XRES[:, kc, c0:c0 + cw], in0=tt[:, 0:cw],
                                                                                 scalar1=LNG[:, gi:gi + 1], scalar2=LNB[:, gi:gi + 1],
                                                                                 op0=ALU.mult, op1=ALU.add))
                        OP("act", "activation", _reads=[tt.r, LNG.r, LNB.r], _writes=[XB.r], _a=dict(out=XB[:, kc, c0:c0 + cw], in_=tt[:, 0:cw], func=AF.Identity,
                                                                              scale=LNG[:, gi:gi + 1], bias=LNB[:, gi:gi + 1]))
                fw.barrier()

        def resid_evac(XRES):
            def ev(o, m, c0, cw, ps):
                kc = o // 128
                OP("dve", "scalar_tensor_tensor", _reads=[ps.r, XRES.r], _writes=[XRES.r], _a=dict(out=XRES[:, kc, c0:c0 + cw], in0=XRES[:, kc, c0:c0 + cw], scalar=ALPHA,
                                                               in1=ps[:, 0:cw], op0=ALU.mult, op1=ALU.add))
            return ev

        with contextlib.ExitStack() as st:
          if "mem" in PARTS and KDBG >= 2:
            MEMB = sb(st, "MEMB", [128, 8, 256], BF16)
            MKP = sb(st, "MKP", [128, 2, 8, 256], BF16)
            MVP = sb(st, "MVP", [128, 2, 2, 1024], BF16)
            STG = [sb(st, f"mstg{i}", [128, 512], F32) for i in range(2)]
            sg = [0]
            fw.dma("pool", MEMB[:], memT.rearrange("(k p) n -> p k n", p=128), writes=[MEMB.r])
            for l in range(2):
                def ev_k(o, m, c0, cw, ps, l=l):
                    s = STG[sg[0] % 2]
                    sg[0] += 1
                    OP("dve", "tensor_copy", _reads=[ps.r], _writes=[s.r], _a=dict(out=s[:, 0:256], in_=ps[:, 0:256]))
                    if KDBG >= 5:
                        if os.environ.get("KV") == "2":
                            OP("act", "activation", _reads=[s.r], _writes=[MKP.r], _a=dict(out=MKP[:, l, o // 128, :], in_=s[:, 0:256], func=AF.Copy))
                        else:
                            OP("act", "activation", _reads=[ps.r] + ([s.r] if os.environ.get("KV") == "3" else []), _writes=[MKP.r], _a=dict(out=MKP[:, l, o // 128, :], in_=ps[:, 0:256], func=AF.Copy))
                    if KDBG >= 6:
                        fw.dma("sp", o_pmk[l, o:o + 128, :], s[:, 0:256], reads=[s.r], writes=[R_OUT["p_mem_kT"]])
                linear_fm(w_xk[l], 8, 0, D, lambda kc, c0, cw: MEMB[:, kc, c0:c0 + cw], [MEMB.r], [(0, 256)], ev_k)

                def ev_v(ti, t0, tw, og, gw, ps, l=l):
                    s = STG[sg[0] % 2]
                    sg[0] += 1
                    OP("dve", "tensor_copy", _reads=[ps.r], _writes=[s.r], _a=dict(out=s[:, 0:gw], in_=ps[:, 0:gw]))
                    if KDBG >= 5:
                        if os.environ.get("KV") == "2":
                            OP("act", "activation", _reads=[s.r], _writes=[MVP.r], _a=dict(out=MVP[:, l, ti, og:og + gw], in_=s[:, 0:gw], func=AF.Copy))
                        else:
                            OP("act", "activation", _reads=[ps.r] + ([s.r] if os.environ.get("KV") == "3" else []), _writes=[MVP.r], _a=dict(out=MVP[:, l, ti, og:og + gw], in_=ps[:, 0:gw], func=AF.Copy))
                    if KDBG >= 6:
                        fw.dma("sp", o_pmv[l, t0:t0 + tw, og:og + gw], s[:, 0:gw], reads=[s.r], writes=[R_OUT["p_mem_v"]])
                linear_tm(w_xv[l], 8, 0, D, lambda kc, t0, tw: MEMB[:, kc, t0:t0 + tw], [MEMB.r], [(0, 128), (128, 128)], ev_v)
            if KDBG >= 7:
                fw.dma("sp", MKD.rearrange("l p k m -> p l k m"), MKP[:], reads=[MKP.r], writes=[R_MKD])
                fw.dma("sp", MVD.rearrange("l p t n -> p l t n"), MVP[:], reads=[MVP.r], writes=[R_MVD])
            fw.barrier()

        def cross_attention(XRES, l, own):
            with contextlib.ExitStack() as st:
                QX = sb(st, "QX", [128, 8, NT], BF16)
                MKP = sb(st, "MKPl", [128, 8, 256], BF16)
                MVP = sb(st, "MVPl", [128, 2, 1024], BF16)
                fw.dma("sp", MKP[:], MKD[l], reads=[R_MKD], writes=[MKP.r])
                fw.dma("sp", MVP[:], MVD[l], reads=[R_MVD], writes=[MVP.r])
                PX = sb(st, "PX", [128, 2, 512], BF16)
                RX = sb(st, "RX", [128, 512], F32)

                def ev_q(o, m, c0, cw, ps):
                    eng = "act" if (o // 128) % 2 else "dve"
                    if eng == "act":
                        OP("act", "activation", _reads=[ps.r], _writes=[QX.r], _a=dict(out=QX[:, o // 128, c0:c0 + cw], in_=ps[:, 0:cw], func=AF.Copy))
                    else:
                        OP("dve", "tensor_copy", _reads=[ps.r], _writes=[QX.r], _a=dict(out=QX[:, o // 128, c0:c0 + cw], in_=ps[:, 0:cw]))
                linear_fm(w_xq[l], 8, 0, D, lambda kc, c0, cw: XB[:, kc, c0:c0 + cw], [XB.r], COLT, ev_q)

                def group(c0, cw, mk_fn, mv_fn, mres):
                    for hd in range(4):
                        for mt in range(2):
                            ps = next_ps()
                            for c in range(2):
                                mm(ps[:, 0:cw], mk_fn(2 * hd + c, mt), QX[:, 2 * hd + c, c0:c0 + cw], c == 0, c == 1, mres + [QX.r], ps)
                            OP("act", "activation", _reads=[ps.r], _writes=[PX.r], _a=dict(out=PX[:, mt, 0:cw], in_=ps[:, 0:cw], func=AF.Exp, scale=1.0 / 16.0))
                        pd = next_ps()
                        for mt in range(2):
                            mm(pd[:, 0:cw], ONES_B, PX[:, mt, 0:cw], mt == 0, mt == 1, [PX.r, CSTB.r], pd)
                        OP("dve", "reciprocal", _reads=[pd.r], _writes=[RX.r], _a=dict(out=RX[:, 0:cw], in_=pd[:, 0:cw]))
                        for c in range(2):
                            po = next_ps()
                            for mt in range(2):
                                mm(po[:, 0:cw], mv_fn(2 * hd + c, mt), PX[:, mt, 0:cw], mt == 0, mt == 1, mres + [PX.r], po)
                            OP("dve", "tensor_tensor", _reads=[po.r, RX.r], _writes=[XB.r], _a=dict(out=XB[:, 2 * hd + c, c0:c0 + cw], in0=po[:, 0:cw], in1=RX[:, 0:cw], op=ALU.mult))
                for (c0, cw) in COLT[:4]:
                    group(c0, cw, lambda ch, mt: MKP[:, ch, mt * 128:(mt + 1) * 128],
                          lambda ch, mt: MVP[:, mt, ch * 128:(ch + 1) * 128], [MKP.r, MVP.r])
                if own:
                    MKS = sb(st, "MKS", [128, 8, 256], BF16)
                    MVS = sb(st, "MVS", [128, 2, 1024], BF16)
                    for s in range(4):
                        fw.dma("pool", MKS[:], cm_k[l, s].rearrange("(k p) m -> p k m", p=128), writes=[MKS.r])
                        fw.dma("pool", MVS[:], cm_v[l, s].rearrange("(t p) n -> p t n", p=128), writes=[MVS.r])
                        group(BLK + 16 * s, 16, lambda ch, mt: MKS[:, ch, mt * 128:(mt + 1) * 128],
                              lambda ch, mt: MVS[:, mt, ch * 128:(ch + 1) * 128], [MKS.r, MVS.r])
                else:
                    OP("dve", "tensor_copy", _reads=[QX.r], _writes=[XB.r], _a=dict(out=XB[:, :, BLK:NT], in_=QX[:, :, BLK:NT]))
                fw.barrier()
            linear_fm(w_xo[l], 8, 0, D, lambda kc, c0, cw: XB[:, kc, c0:c0 + cw], [XB.r], COLT, resid_evac(XRES))
            fw.barrier()

        def ffn(XRES, w1, w3, w2, gate=None):
            TT_ = [(0, 1056), (1056, 1056)]
            with contextlib.ExitStack() as st:
                W2H = sb(st, "W2H", [128, 11, 1024], BF16)
                G = sb(st, "G", [128, 11, 1056], BF16)
                SL = [sb(st, f"SL{i}", [128, 512], BF16) for i in range(2)]
                sl = [0]
                for (t0, tn) in TT_:
                    subt = [(0, 512), (512, 512), (1024, 32)]
                    for hf in range(2):
                        f0 = hf * 11
                        for g4 in range(0, 11, 4):
                            n4 = min(4, 11 - g4)
                            fw.dma("pool", W2H[:, g4:g4 + n4, :], w2[(f0 + g4) * 128:(f0 + g4 + n4) * 128, :].rearrange("(k p) n -> p k n", p=128),
                                   writes=[W2H.r])
                        for g4 in range(0, 11, 4):
                            n4 = min(4, 11 - g4)
                            gw = n4 * 128
                            wb1 = next_wb()
                            wb3 = next_wb()
                            v1 = wb1[:, 0:8 * gw].rearrange("p (k n) -> p k n", n=gw)
                            v3 = wb3[:, 0:8 * gw].rearrange("p (k n) -> p k n", n=gw)
                            cc0 = (f0 + g4) * 128
                            wload(v1, w1[:, cc0:cc0 + gw].rearrange("(k p) n -> p k n", p=128), wb1)
                            wload(v3, w3[:, cc0:cc0 + gw].rearrange("(k p) n -> p k n", p=128), wb3)
                            for j in range(n4):
                                for (s0, sw) in subt:
                                    pa = next_ps()
                                    pb = next_ps()
                                    for kc in range(8):
                                        mm(pa[:, 0:sw], v1[:, kc, j * 128:(j + 1) * 128], XB[:, kc, t0 + s0:t0 + s0 + sw], kc == 0, kc == 7, [wb1.r, XB.r], pa)
                                    for kc in range(8):
                                        mm(pb[:, 0:sw], v3[:, kc, j * 128:(j + 1) * 128], XB[:, kc, t0 + s0:t0 + s0 + sw], kc == 0, kc == 7, [wb3.r, XB.r], pb)
                                    s_ = SL[sl[0] % 2]
                                    sl[0] += 1
                                    OP("act", "activation", _reads=[pa.r], _writes=[s_.r], _a=dict(out=s_[:, 0:sw], in_=pa[:, 0:sw], func=AF.Silu))
                                    if gate is not None:
                                        OP("pool", "tensor_tensor", _reads=[s_.r, gate.r], _writes=[s_.r], _a=dict(out=s_[:, 0:sw], in0=s_[:, 0:sw], in1=gate[:, t0 + s0:t0 + s0 + sw], op=ALU.mult))
                                    OP("dve", "tensor_tensor", _reads=[s_.r, pb.r], _writes=[G.r], _a=dict(out=G[:, g4 + j, s0:s0 + sw], in0=s_[:, 0:sw], in1=pb[:, 0:sw], op=ALU.mult))
                        for oc in range(8):
                            for (s0, sw) in subt:
                                py = next_ps()
                                for f in range(11):
                                    mm(py[:, 0:sw], W2H[:, f, oc * 128:(oc + 1) * 128], G[:, f, s0:s0 + sw], f == 0, f == 10, [W2H.r, G.r], py)
                                OP("dve", "tensor_tensor", _reads=[py.r, XRES.r], _writes=[XRES.r], _a=dict(out=XRES[:, oc, t0 + s0:t0 + s0 + sw], in0=XRES[:, oc, t0 + s0:t0 + s0 + sw],
                                                                                  in1=py[:, 0:sw], op=ALU.add))
                fw.barrier()
        gate_res = None

        def layer0_pass(b, own):
            pi = 8 if own else b
            with contextlib.ExitStack() as sA:
                XBA = sb(sA, "XBA", [128, 8, NA], BF16)
                if own:
                    fw.dma("pool", XBA[:], xo.rearrange("(k p) n -> p k n", p=128), writes=[XBA.r])
                else:
                    fw.dma("pool", XBA[:, :, 0:HALO + BLK], xT[:, b * BLK:b * BLK + HALO + BLK].rearrange("(k p) n -> p k n", p=128), writes=[XBA.r])
                    fw.dma("pool", XBA[:, :, HALO + BLK:NA], xo[:, HALO + BLK:NA].rearrange("(k p) n -> p k n", p=128), writes=[XBA.r])
                with contextlib.suppress(SkipBlock), contextlib.ExitStack() as s1:
                    if "pool" not in PARTS:
                        raise SkipBlock()
                    UP = sb(s1, "UP", [128, 4, 16 + BLK], F32)
                    UH = sb(s1, "UH", [128, 4, 4, 32], F32)
                    TA = sb(s1, "TA", [128, 16 + BLK], F32)
                    TB = sb(s1, "TB", [128, 16 + BLK], F32)
                    DD = sb(s1, "DD", [128, 4, NT], BF16)
                    FX = sb(s1, "FX", [128, 64], F32)
                    fw.dma("sp", FX[:], FIXd[pi], writes=[FX.r])
                    OP("dve", "memset", _writes=[UH.r], _p=(UH[:], 0.0,))
                    if own:
                        for s in range(4):
                            fw.dma("sp", UH[:, :, s, 1:16], sp_T[s].rearrange("(g p) t -> p g t", p=128), writes=[UH.r])
                    ucols = [(496, 512), (1008, 512), (1520, 512), (2032, 512), (2544, 80)]

                    def ev_u(o, m, c0, cw, ps):
                        g = (o - 1536) // 128
                        if c0 < 2544:
                            OP("act", "activation", _reads=[ps.r], _writes=[UP.r], _a=dict(out=UP[:, g, c0 - 496:c0 - 496 + cw], in_=ps[:, 0:cw], func=AF.Copy))
                        else:
                            OP("act", "activation", _reads=[ps.r], _writes=[UP.r], _a=dict(out=UP[:, g, 2048:2064], in_=ps[:, 0:16], func=AF.Copy))
                            OP("dve", "tensor_copy", _reads=[ps.r], _writes=[UH.r], _a=dict(out=UH[:, g, :, 16:32], in_=ps[:, 16:80].rearrange("p (s t) -> p s t", t=16)))
                    linear_fm(w_in_ab, 8, 1536, 512, lambda kc, c0, cw: XBA[:, kc, c0:c0 + cw], [XBA.r], ucols, ev_u)
                    if own:
                        for s in range(4):
                            fw.dma("sp", o_spool[s].rearrange("(g p) t -> p g t", p=128), UH[:, :, s, 17:32], reads=[UH.r], writes=[R_OUT["s_poolT"]])
                    if (not own) and b == NBLK - 1:
                        fw.dma("sp", o_ppool.rearrange("(g p) t -> p g t", p=128), UP[:, :, 16 + BLK - 15:16 + BLK], reads=[UP.r], writes=[R_OUT["p_poolT"]])
                    for g in range(4):
                        w = 2 << g
                        L = 16 + BLK
                        src = UP[:, g, :]
                        srcr = UP.r
                        bufs = [TA, TB]
                        step = 1
                        k = 0
                        lo = 0
                        while step < w:
                            dst = bufs[k % 2]
                            lo += step
                            OP("dve", "tensor_tensor", _reads=[srcr], _writes=[dst.r], _a=dict(out=dst[:, lo:L], in0=src[:, lo:L], in1=src[:, lo - step:L - step], op=ALU.add))
                            src = dst[:, :]
                            srcr = dst.r
                            step *= 2
                            k += 1
                        win = src
                        winr = srcr
                        other = bufs[k % 2]
                        OP("dve", "scalar_tensor_tensor", _reads=[winr, UP.r], _writes=[other.r], _a=dict(out=other[:, 16:L], in0=win[:, 16:L], scalar=1.0 / w, in1=UP[:, g, 16:L],
                                                                                            op0=ALU.mult, op1=ALU.subtract))
                        OP("dve", "tensor_tensor", _reads=[winr, FX.r], _writes=[winr], _a=dict(out=win[:, 16:32], in0=win[:, 16:32], in1=FX[:, g * 16:(g + 1) * 16], op=ALU.mult))
                        OP("dve", "tensor_tensor", _reads=[winr, UP.r, other.r], _writes=[other.r], _a=dict(out=other[:, 16:32], in0=win[:, 16:32], in1=UP[:, g, 16:32], op=ALU.subtract))
                        OP("act", "activation", _reads=[other.r], _writes=[DD.r], _a=dict(out=DD[:, g, 0:BLK], in_=other[:, 16:L], func=AF.Copy))
                        SA = TA[:, 0:128].rearrange("p (s t) -> p s t", t=32)
                        SB_ = TB[:, 0:128].rearrange("p (s t) -> p s t", t=32)
                        srcs = UH[:, g, :, :]
                        srcr = UH.r
                        sbufs = [(SA, TA.r), (SB_, TB.r)]
                        step = 1
                        k = 0
                        lo = 0
                        while step < w:
                            dst, dstr = sbufs[k % 2]
                            lo += step
                            OP("dve", "tensor_tensor", _reads=[srcr], _writes=[dstr], _a=dict(out=dst[:, :, lo:32], in0=srcs[:, :, lo:32], in1=srcs[:, :, lo - step:32 - step], op=ALU.add))
                            srcs = dst
                            srcr = dstr
                            step *= 2
                            k += 1
                        odst, odstr = sbufs[k % 2]
                        OP("dve", "scalar_tensor_tensor", _reads=[srcr, UH.r], _writes=[odstr], _a=dict(out=odst[:, :, 16:32], in0=srcs[:, :, 16:32], scalar=1.0 / w, in1=UH[:, g, :, 16:32],
                                                                                            op0=ALU.mult, op1=ALU.subtract))
                        OP("act", "activation", _reads=[odstr], _writes=[DD.r], _a=dict(out=DD[:, g, BLK:NT].rearrange("p (s t) -> p s t", t=16), in_=odst[:, :, 16:32], func=AF.Copy))
                        for (c0, cw) in COLT:
                            ps = next_ps()
                            mm(ps[:, 0:cw], PW[:, g, :], DD[:, g, c0:c0 + cw], True, True, [PW.r, DD.r], ps)
                            OP("act", "activation", _reads=[ps.r, PSC.r], _writes=[XB.r], _a=dict(out=XB[:, 4 + g, c0:c0 + cw], in_=ps[:, 0:cw], func=AF.Identity, scale=PSC[:, g:g + 1]))
                    fw.barrier()
                fw.barrier()
            with contextlib.ExitStack() as s2:
                QF = sb(s2, "QF", [128, 4, NT], BF16)
                KF = sb(s2, "KF", [128, 4, NA], BF16)
                VT = sb(s2, "VT", [128, 21, 512], BF16)
                VAL = sb(s2, "VAL", [128, 21, 64], BF16)
                VLT = sb(s2, "VLT", [128, 21], F32)
                ST32 = [sb(s2, f"st32_{i}", [128, 512], F32) for i in range(2)]
                stc = [0]
                if own:
                    VSN = sb(s2, "VSN", [16, 4, 512], BF16)
                    VSF = sb(s2, "VSF", [16, 4, 512], F32)
                sX = contextlib.ExitStack()
                XBA = sb(sX, "XBA2", [128, 8, NA], BF16)
                if own:
                    fw.dma("pool", XBA[:], xo.rearrange("(k p) n -> p k n", p=128), writes=[XBA.r])
                else:
                    fw.dma("pool", XBA[:, :, 0:HALO + BLK], xT[:, b * BLK:b * BLK + HALO + BLK].rearrange("(k p) n -> p k n", p=128), writes=[XBA.r])
                    fw.dma("pool", XBA[:, :, HALO + BLK:NA], xo[:, HALO + BLK:NA].rearrange("(k p) n -> p k n", p=128), writes=[XBA.r])
                fw.dma("sp", VLT[:], VALT[pi], writes=[VLT.r])
                OP("dve", "tensor_copy", _reads=[VLT.r], _writes=[VAL.r], _a=dict(out=VAL[:], in_=VLT[:].unsqueeze(2).to_broadcast([128, 21, 64])))

                def ev_q(o, m, c0, cw, ps):
                    OP("act", "activation", _reads=[ps.r], _writes=[QF.r], _a=dict(out=QF[:, o // 128, c0 - HALO:c0 - HALO + cw], in_=ps[:, 0:cw], func=AF.Copy))
                linear_fm(w_in_ab, 8, 0, 512, lambda kc, c0, cw: XBA[:, kc, c0:c0 + cw], [XBA.r], [(HALO + c, w_) for (c, w_) in COLT], ev_q)

                def ev_k(o, m, c0, cw, ps):
                    ch = (o - 512) // 128
                    OP("dve", "tensor_copy", _reads=[ps.r], _writes=[KF.r], _a=dict(out=KF[:, ch, c0:c0 + cw], in_=ps[:, 0:cw]))
                    if (not own) and b == NBLK - 1 and c0 == 2048:
                        s = ST32[stc[0] % 2]
                        stc[0] += 1
                        OP("act", "activation", _reads=[ps.r], _writes=[s.r], _a=dict(out=s[:, 0:512], in_=ps[:, 0:512], func=AF.Copy))
                        fw.dma("sp", o_pak[ch * 128:(ch + 1) * 128, :], s[:, 0:512], reads=[s.r], writes=[R_OUT["p_a_kT"]])
                    if own and c0 == 2560:
                        s = ST32[stc[0] % 2]
                        stc[0] += 1
                        OP("act", "activation", _reads=[ps.r], _writes=[s.r], _a=dict(out=s[:, 0:64], in_=ps[:, 0:64], func=AF.Copy))
                        fw.dma("sp", o_sak[ch * 128:(ch + 1) * 128, :], s[:, 0:64], reads=[s.r], writes=[R_OUT["s_a_kT"]])
                linear_fm(w_in_ab, 8, 512, 512, lambda kc, c0, cw: XBA[:, kc, c0:c0 + cw], [XBA.r], COLA, ev_k)

                tokt = [(t * 128, 128) for t in range(20)]

                def ev_v(ti, t0, tw, og, gw, ps):
                    OP("act", "activation", _reads=[ps.r], _writes=[VT.r], _a=dict(out=VT[:, ti, :], in_=ps[:, 0:512], func=AF.Copy))
                    if (not own) and b == NBLK - 1 and 16 <= ti < 20:
                        s = ST32[stc[0] % 2]
                        stc[0] += 1
                        OP("dve", "tensor_copy", _reads=[ps.r], _writes=[s.r], _a=dict(out=s[:, 0:512], in_=ps[:, 0:512]))
                        fw.dma("sp", o_pav[(ti - 16) * 128:(ti - 15) * 128, :], s[:, 0:512], reads=[s.r], writes=[R_OUT["p_a_v"]])
                linear_tm(w_in_ab, 8, 1024, 512, lambda kc, t0, tw: XBA[:, kc, t0:t0 + tw], [XBA.r], tokt, ev_v)
                if own:
                    wbv = next_wb()
                    wvv = wbv[:, 0:4096].rearrange("p (k n) -> p k n", n=512)
                    wload(wvv, w_in_ab[:, 1024:1536].rearrange("(k p) n -> p k n", p=128), wbv)
                    for s in range(4):
                        ps = next_ps()
                        for kc in range(8):
                            mm(ps[0:16, 0:512], XBA[:, kc, HALO + BLK + 16 * s:HALO + BLK + 16 * s + 16], wvv[:, kc, :], kc == 0, kc == 7, [XBA.r, wbv.r], ps)
                        OP("act", "activation", _reads=[ps.r], _writes=[VSN.r], _a=dict(out=VSN[:, s, :], in_=ps[0:16, 0:512], func=AF.Copy))
                        OP("dve", "tensor_copy", _reads=[ps.r], _writes=[VSF.r], _a=dict(out=VSF[:, s, :], in_=ps[0:16, 0:512]))
                    fw.dma("sp", o_sav.rearrange("(s t) n -> t s n", t=16), VSF[:], reads=[VSF.r], writes=[R_OUT["s_a_v"]])
                fw.barrier()
                sX.close()

                with contextlib.suppress(SkipBlock), contextlib.ExitStack() as s3:
                    if "band" not in PARTS:
                        raise SkipBlock()
                    BT = sb(s3, "BT", [128, 5, 8, 128], F32)
                    PB = sb(s3, "PB", [128, 5, 8, 128], BF16)
                    TMP = [sb(s3, f"batmp{i}", [128, 512], F32) for i in range(2)]
                    RD = sb(s3, "bard", [128, 512], F32)
                    tc_ = [0]
                    fw.dma("sp", BT[:], BTd.rearrange("p (j h q) -> p j h q", j=5, h=8), writes=[BT.r])
                    for i in range(16):
                        qc0 = i * 128
                        for j in range(5):
                            kt = i + j
                            for hg in range(2):
                                ps = next_ps((0, 1, 2, 3))
                                for hh in range(4):
                                    hd = 2 * hh + hg
                                    hp = (hd % 2) * 64
                                    mm(ps[:, hh * 128:(hh + 1) * 128], KF[hp:hp + 64, hd // 2, kt * 128:(kt + 1) * 128], QF[hp:hp + 64, hd // 2, qc0:qc0 + 128],
                                       True, True, [KF.r, QF.r], ps)
                                tm = TMP[tc_[0] % 2]
                                tc_[0] += 1
                                OP("dve", "scalar_tensor_tensor", _reads=[ps.r, BT.r], _writes=[tm.r], _a=dict(
                                    out=tm[:].rearrange("p (a q) -> p a q", q=128), in0=ps[:].rearrange("p (a q) -> p a q", q=128), scalar=0.125,
                                    in1=BT[:, j, hg * 4:(hg + 1) * 4, :], op0=ALU.mult, op1=ALU.add))
                                OP("act", "activation", _reads=[tm.r], _writes=[PB.r], _a=dict(out=PB[:, j, hg * 4:(hg + 1) * 4, :], in_=tm[:].rearrange("p (a q) -> p a q", q=128), func=AF.Exp))
                        po = next_ps((4, 5))
                        pd = next_ps((6, 7))
                        for hd in range(8):
                            hp = (hd % 2) * 64
                            cs = (hd // 2) * 128
                            for j in range(5):
                                mm(po[hp:hp + 64, cs:cs + 128], VT[:, i + j, hd * 64:(hd + 1) * 64], PB[:, j, (hd % 2) * 4 + hd // 2, :], j == 0, j == 4, [VT.r, PB.r], po)
                            for j in range(5):
                                mm(pd[hp:hp + 64, cs:cs + 128], VAL[:, i + j, :], PB[:, j, (hd % 2) * 4 + hd // 2, :], j == 0, j == 4, [VAL.r, PB.r], pd)
                        OP("dve", "reciprocal", _reads=[pd.r], _writes=[RD.r], _a=dict(out=RD[:], in_=pd[:]))
                        OP("dve", "tensor_tensor", _reads=[po.r, RD.r], _writes=[XB.r], _a=dict(out=XB[:, 0:4, qc0:qc0 + 128], in0=po[:].rearrange("p (a q) -> p a q", q=128),
                                                                             in1=RD[:].rearrange("p (a q) -> p a q", q=128), op=ALU.mult))
                    fw.barrier()
                if own:
                    with contextlib.suppress(SkipBlock), contextlib.ExitStack() as s3:
                        if "sband" not in PARTS:
                            raise SkipBlock()
                        KCA = sb(s3, "KCA", [128, 4, 512], BF16)
                        VCA = sb(s3, "VCA", [128, 4, 512], BF16)
                        BTS = sb(s3, "BTS", [128, 5, 8, 16], F32)
                        PSB = sb(s3, "PSB", [128, 5, 8, 16], BF16)
                        TMPS = sb(s3, "tmps", [128, 128], F32)
                        RDS = sb(s3, "rds", [128, 64], F32)
                        fw.dma("sp", BTS[:], BTSd.rearrange("p (j h q) -> p j h q", j=5, h=8), writes=[BTS.r])
                        OP("dve", "memset", _writes=[PSB.r], _p=(PSB[:], 0.0,))
                        for s in range(4):
                            fw.dma("pool", KCA[:], ca_k[s].rearrange("(k p) n -> p k n", p=128), writes=[KCA.r])
                            fw.dma("pool", VCA[:], ca_v[s].rearrange("(t p) n -> p t n", p=128), writes=[VCA.r])
                            qc0 = BLK + 16 * s
                            kn0 = HALO + BLK + 16 * s
                            for j in range(5):
                                kp = 128 if j < 4 else 16
                                for hg in range(2):
                                    ps = next_ps((0, 1, 2, 3))
                                    for hh in range(4):
                                        hd = 2 * hh + hg
                                        hp = hg * 64
                                        lhs = KCA[hp:hp + 64, hd // 2, j * 128:(j + 1) * 128] if j < 4 else KF[hp:hp + 64, hd // 2, kn0:kn0 + 16]
                                        mm(ps[0:kp, hh * 16:(hh + 1) * 16], lhs, QF[hp:hp + 64, hd // 2, qc0:qc0 + 16], True, True, [KCA.r, KF.r, QF.r], ps)
                                    OP("dve", "scalar_tensor_tensor", _reads=[ps.r, BTS.r], _writes=[TMPS.r], _a=dict(
                                        out=TMPS[0:kp, hg * 64:(hg + 1) * 64], in0=ps[0:kp, 0:64], scalar=0.125,
                                        in1=BTS[0:kp, j, hg * 4:(hg + 1) * 4, :].rearrange("p a q -> p (a q)"), op0=ALU.mult, op1=ALU.add))
                                OP("act", "activation", _reads=[TMPS.r], _writes=[PSB.r], _a=dict(out=PSB[0:kp, j, :, :].rearrange("p a q -> p (a q)"), in_=TMPS[0:kp, :], func=AF.Exp))
                            po = next_ps((4, 5))
                            pd = next_ps((6, 7))
                            for hd in range(8):
                                hp = (hd % 2) * 64
                                cs = (hd // 2) * 16
                                for j in range(5):
                                    kp = 128 if j < 4 else 16
                                    lv = VCA[:, j, hd * 64:(hd + 1) * 64] if j < 4 else VSN[0:16, s, hd * 64:(hd + 1) * 64]
                                    mm(po[hp:hp + 64, cs:cs + 16], lv, PSB[0:kp, j, (hd % 2) * 4 + hd // 2, :], j == 0, j == 4, [VCA.r, VSN.r, PSB.r], po)
                                for j in range(5):
                                    kp = 128 if j < 4 else 16
                                    mm(pd[hp:hp + 64, cs:cs + 16], CSTB[0:kp, 0:64], PSB[0:kp, j, (hd % 2) * 4 + hd // 2, :], j == 0, j == 4, [CSTB.r, PSB.r], pd)
                            OP("dve", "reciprocal", _reads=[pd.r], _writes=[RDS.r], _a=dict(out=RDS[:], in_=pd[:, 0:64]))
                            OP("dve", "tensor_tensor", _reads=[po.r, RDS.r], _writes=[XB.r], _a=dict(out=XB[:, 0:4, qc0:qc0 + 16], in0=po[:, 0:64].rearrange("p (a q) -> p a q", q=16),
                                                                                 in1=RDS[:].rearrange("p (a q) -> p a q", q=16), op=ALU.mult))
                        fw.barrier()
                else:
                    OP("dve", "tensor_copy", _reads=[QF.r], _writes=[XB.r], _a=dict(out=XB[:, 0:4, BLK:NT], in_=QF[:, :, BLK:NT]))
                fw.barrier()
            sB = contextlib.ExitStack()
            XRES = sb(sB, "XRES", [128, 8, NT], F32)
            if own:
                fw.dma("sp", XRES[:], xo[:, HALO:NA].rearrange("(k p) n -> p k n", p=128), writes=[XRES.r])
            else:
                fw.dma("sp", XRES[:, :, 0:BLK], xT[:, HALO + b * BLK:HALO + (b + 1) * BLK].rearrange("(k p) n -> p k n", p=128), writes=[XRES.r])
                fw.dma("sp", XRES[:, :, BLK:NT], xo[:, HALO + BLK:NA].rearrange("(k p) n -> p k n", p=128), writes=[XRES.r])
            if "outproj" in PARTS:
                linear_fm(w_out_ab, 8, 0, D, lambda kc, c0, cw: XB[:, kc, c0:c0 + cw], [XB.r], COLT, resid_evac(XRES))
            fw.barrier()
            if "ln1" in PARTS:
                layernorm(XRES, 0)
            if "xattn" in PARTS:
                cross_attention(XRES, 0, own)
            if "ln2" in PARTS:
                layernorm(XRES, 1)
            if "ffn" in PARTS:
                for kc in range(8):
                    OP("act", "activation", _reads=[XRES.r], _writes=[XRES.r], _a=dict(out=XRES[:, kc, :], in_=XRES[:, kc, :], func=AF.Identity, scale=ALPHA))
                ffn(XRES, ffn_w1, ffn_w3, ffn_w2)
            if "ln3" in PARTS:
                layernorm(XRES, 2)
            return sB, XRES

        XRES_holder = [None]

        def kvproj(b, own):
            with contextlib.ExitStack() as st:
                KST = [sb(st, f"kst{i}", [128, 512], BF16) for i in range(2)]
                KSF = [sb(st, f"ksf{i}", [128, 512], F32) for i in range(2)]
                VST = [sb(st, f"vst{i}", [128, 512], BF16) for i in range(2)]
                VSF_ = [sb(st, f"vsf{i}", [128, 512], F32) for i in range(2)]
                LFT = sb(st, "lft", [128, 17, 16], F32)
                c_ = [0]
                tok0 = b * BLK

                def ev_k(o, m, c0, cw, ps):
                    ch = (o - 1024) // 128
                    i = c_[0] % 2
                    c_[0] += 1
                    kb, kf = KST[i], KSF[i]
                    OP("act", "activation", _reads=[ps.r], _writes=[kb.r], _a=dict(out=kb[:, 0:cw], in_=ps[:, 0:cw], func=AF.Copy))
                    if own:
                        fw.dma("sp", KSO[ch, :, c0:c0 + cw], kb[:, 0:cw], reads=[kb.r], writes=[R_KSO])
                        if c0 == BLK:
                            OP("dve", "tensor_copy", _reads=[ps.r], _writes=[kf.r], _a=dict(out=kf[:, 0:cw], in_=ps[:, 0:cw]))
                            fw.dma("sp", o_sck[ch * 128:(ch + 1) * 128, :], kf[:, 0:cw], reads=[kf.r], writes=[R_OUT["s_c_kT"]])
                    elif c0 < BLK:
                        fw.dma("sp", KS[ch, :, tok0 + c0:tok0 + c0 + cw], kb[:, 0:cw], reads=[kb.r], writes=[R_KS])
                        OP("dve", "tensor_copy", _reads=[ps.r], _writes=[kf.r], _a=dict(out=kf[:, 0:cw], in_=ps[:, 0:cw]))
                        fw.dma("sp", o_pck[ch * 128:(ch + 1) * 128, tok0 + c0:tok0 + c0 + cw], kf[:, 0:cw], reads=[kf.r], writes=[R_OUT["p_c_kT"]])
                cols = COLT if own else COLT[:4]
                linear_fm(w_in_c, 8, 1024, 1024, lambda kc, c0, cw: XB[:, kc, c0:c0 + cw], [XB.r], cols, ev_k)

                def ev_v(ti, t0, tw, og, gw, ps):
                    i = c_[0] % 2
                    c_[0] += 1
                    vb, vf = VST[i], VSF_[i]
                    OP("act", "activation", _reads=[ps.r], _writes=[vb.r], _a=dict(out=vb[:, 0:gw], in_=ps[:, 0:gw], func=AF.Copy))
                    if own:
                        fw.dma("sp", VSO[t0:t0 + 128, og:og + gw], vb[:, 0:gw], reads=[vb.r], writes=[R_VSO])
                    else:
                        fw.dma("sp", VS[tok0 + t0:tok0 + t0 + 128, og:og + gw], vb[:, 0:gw], reads=[vb.r], writes=[R_VS])
                        OP("dve", "tensor_copy", _reads=[ps.r], _writes=[vf.r], _a=dict(out=vf[:, 0:gw], in_=ps[:, 0:gw]))
                        fw.dma("sp", o_pcv[tok0 + t0:tok0 + t0 + 128, og:og + gw], vf[:, 0:gw], reads=[vf.r], writes=[R_OUT["p_c_v"]])
                linear_tm(w_in_c, 8, 2048, 1024, lambda kc, t0, tw: XB[:, kc, t0:t0 + tw], [XB.r], [(t * 128, 128) for t in range(16)], ev_v)

                wb = next_wb()
                wv = wb[:, 0:128].rearrange("p (k n) -> p k n", n=16)
                wload(wv, w_in_c[:, 3072:3088].rearrange("(k p) n -> p k n", p=128), wb)
                ps = next_ps()
                for t in range(16):
                    for kc in range(8):
                        mm(ps[:, t * 16:(t + 1) * 16], XB[:, kc, t * 128:(t + 1) * 128], wv[:, kc, :], kc == 0, kc == 7, [XB.r, wb.r], ps)
                lfv = LFT[:, 0:16, :]
                LFD = LFO[:, :, :] if own else LF[:, b * 16:(b + 1) * 16, :]
                LFDr = LFO.r if own else LF.r
                OP("dve", "tensor_tensor", _reads=[ps.r, BFR.r], _writes=[LFT.r], _a=dict(out=lfv, in0=ps[:, 0:256].rearrange("p (t n) -> p t n", n=16),
                                                       in1=BFR[:].unsqueeze(1).to_broadcast([128, 16, 16]), op=ALU.add))
                OP("act", "activation", _reads=[LFT.r], _writes=[LFT.r], _a=dict(out=lfv, in_=lfv, func=AF.Exp, scale=-1.0))
                OP("act", "activation", _reads=[LFT.r], _writes=[LFT.r], _a=dict(out=lfv, in_=lfv, func=AF.Ln, bias=1.0))
                OP("dve", "tensor_scalar", _reads=[LFT.r], _writes=[LFDr], _a=dict(out=LFD, in0=lfv, scalar1=-1.0, scalar2=None, op0=ALU.mult))
                if not own:
                    fw.dma("sp", o_pclf[tok0:tok0 + BLK, :].rearrange("(t p) n -> p t n", p=128), LF[:, b * 16:(b + 1) * 16, :], reads=[LF.r], writes=[R_OUT["p_c_lf"]])
                else:
                    VN = sb(st, "VN", [16, 4, D], BF16)
                    VNF = sb(st, "VNF", [16, 4, D], F32)
                    psl = next_ps()
                    for s_ in range(4):
                        for kc in range(8):
                            mm(psl[0:16, s_ * 16:(s_ + 1) * 16], XB[:, kc, BLK + 16 * s_:BLK + 16 * s_ + 16], wv[:, kc, :], kc == 0, kc == 7, [XB.r, wb.r], psl)
                    lfn = LFT[0:16, 0:4, :]
                    OP("dve", "tensor_tensor", _reads=[psl.r, BFR.r], _writes=[LFT.r], _a=dict(out=lfn, in0=psl[0:16, 0:64].rearrange("p (t n) -> p t n", n=16),
                                                           in1=BFR[0:16, :].unsqueeze(1).to_broadcast([16, 4, 16]), op=ALU.add))
                    OP("act", "activation", _reads=[LFT.r], _writes=[LFT.r], _a=dict(out=lfn, in_=lfn, func=AF.Exp, scale=-1.0))
                    OP("act", "activation", _reads=[LFT.r], _writes=[LFT.r], _a=dict(out=lfn, in_=lfn, func=AF.Ln, bias=1.0))
                    OP("dve", "tensor_scalar", _reads=[LFT.r], _writes=[LFN.r], _a=dict(out=LFN[:], in0=lfn, scalar1=-1.0, scalar2=None, op0=ALU.mult))
                    fw.dma("sp", o_sclf.rearrange("(s t) n -> t s n", t=16), LFN[:], reads=[LFN.r], writes=[R_OUT["s_c_lf"]])
                    for og in range(0, D, 512):
                        wbv = next_wb()
                        wvv = wbv[:, 0:4096].rearrange("p (k n) -> p k n", n=512)
                        wload(wvv, w_in_c[:, 2048 + og:2048 + og + 512].rearrange("(k p) n -> p k n", p=128), wbv)
                        for s_ in range(4):
                            psv = next_ps()
                            for kc in range(8):
                                mm(psv[0:16, 0:512], XB[:, kc, BLK + 16 * s_:BLK + 16 * s_ + 16], wvv[:, kc, :], kc == 0, kc == 7, [XB.r, wbv.r], psv)
                            OP("act", "activation", _reads=[psv.r], _writes=[VN.r], _a=dict(out=VN[:, s_, og:og + 512], in_=psv[0:16, 0:512], func=AF.Copy))
                            OP("dve", "tensor_copy", _reads=[psv.r, VN.r], _writes=[VNF.r], _a=dict(out=VNF[:, s_, og:og + 512], in_=psv[0:16, 0:512]))
                    fw.dma("sp", o_scv.rearrange("(s t) n -> t s n", t=16), VNF[:], reads=[VNF.r], writes=[R_OUT["s_c_v"]])
                    fw.dma("sp", VSNd, VN[:], reads=[VN.r], writes=[R_VSNd])

                    def ev_q(o, m, c0, cw, ps):
                        i = c_[0] % 2
                        c_[0] += 1
                        kb = KST[i]
                        OP("act", "activation", _reads=[ps.r], _writes=[kb.r], _a=dict(out=kb[:, 0:cw], in_=ps[:, 0:cw], func=AF.Copy))
                        fw.dma("sp", QSO[o // 128, :, c0:c0 + cw], kb[:, 0:cw], reads=[kb.r], writes=[R_QSO])
                    linear_fm(w_in_c, 8, 0, 1024, lambda kc, c0, cw: XB[:, kc, c0:c0 + cw], [XB.r], COLT, ev_q)
                    fw.dma("sp", X1S, XRES_holder[0][:], reads=[XRES_holder[0].r], writes=[R_X1S])
                fw.barrier()

        for b in range(NBLK):
            if str(b) not in PASSES:
                continue
            sB, XRES = layer0_pass(b, False)
            if "kv" in PARTS:
                kvproj(b, False)
            sB.close()
            fw.barrier()
        if "own" in PASSES:
            sB, XRES = layer0_pass(0, True)
            XRES_holder[0] = XRES
            if "kv" in PARTS:
                kvproj(0, True)
            sB.close()
            fw.barrier()

        def prefix_tiles(st, TOTt, ntiles, name):
            A_ = sb(st, name + "_pa", [128, ntiles, 16], F32)
            B_ = sb(st, name + "_pb", [128, ntiles, 16], F32)
            cur = TOTt
            bufs = [A_, B_]
            k = 0
            step = 1
            while step < ntiles:
                dst = bufs[k % 2]
                OP("dve", "tensor_copy", _reads=[cur.r], _writes=[dst.r], _a=dict(out=dst[:, 0:step, :], in_=cur[:, 0:step, :]))
                OP("dve", "tensor_tensor", _reads=[cur.r], _writes=[dst.r], _a=dict(out=dst[:, step:ntiles, :], in0=cur[:, step:ntiles, :], in1=cur[:, 0:ntiles - step, :], op=ALU.add))
                cur = dst
                step *= 2
                k += 1
            return cur

        def tile_cumsum(st, LFsrc, LFres, ntiles, name, rows=128):
            TRI_ = sb(st, name + "_tri", [128, ntiles, 16], F32)
            TOT_ = sb(st, name + "_tot", [128, ntiles, 16], F32)
            for t0 in range(0, ntiles, 32):
                n = min(32, ntiles - t0)
                p1 = next_ps()
                p2 = next_ps()
                for t in range(n):
                    mm(p1[:, t * 16:(t + 1) * 16], TRI_F[0:rows, :], LFsrc[0:rows, t0 + t, :], True, True, [CST.r, LFres], p1)
                for t in range(n):
                    mm(p2[:, t * 16:(t + 1) * 16], ONES_F[0:rows, :], LFsrc[0:rows, t0 + t, :], True, True, [CST.r, LFres], p2)
                OP("dve", "tensor_copy", _reads=[p1.r], _writes=[TRI_.r], _a=dict(out=TRI_[:, t0:t0 + n, :], in_=p1[:, 0:n * 16].rearrange("p (t h) -> p t h", h=16)))
                OP("dve", "tensor_copy", _reads=[p2.r], _writes=[TOT_.r], _a=dict(out=TOT_[:, t0:t0 + n, :], in_=p2[:, 0:n * 16].rearrange("p (t h) -> p t h", h=16)))
            INC = prefix_tiles(st, TOT_, ntiles, name) if ntiles > 1 else TOT_
            CARX = sb(st, name + "_carx", [128, ntiles, 16], F32)
            OP("dve", "tensor_tensor", _reads=[INC.r, TOT_.r], _writes=[CARX.r], _a=dict(out=CARX[:], in0=INC[:], in1=TOT_[:], op=ALU.subtract))
            OP("dve", "tensor_tensor", _reads=[TRI_.r, CARX.r], _writes=[TRI_.r], _a=dict(out=TRI_[:], in0=TRI_[:], in1=CARX[:], op=ALU.add))
            return TRI_, CARX, INC

        def fox_prompt():
            with contextlib.ExitStack() as st:
                NB0 = sb(st, "NB0", [128, 128, 16], F32)
                NBq = sb(st, "NBq", [128, 128, 16], F32)
                DCO = sb(st, "DCO", [128, 16, 16], F32)
                XQ = sb(st, "XQ", [128, 16, 16], F32)
                NBOq = sb(st, "NBOq", [128, 16, 16], F32)
                with contextlib.ExitStack() as s1:
                    DC, CARX, INC = tile_cumsum(s1, LF, LF.r, 128, "g")
                    SELs = sb(s1, "SELs", [128, 128], F32)
                    INVs = sb(s1, "INVs", [128, 128], F32)
                    CREF = sb(s1, "CREF", [128, 16], F32)
                    TMPc = sb(s1, "TMPc", [128, 128, 16], F32)
                    fw.dma("sp", SELs[:], SELT, writes=[SELs.r])
                    fw.dma("sp", INVs[:], INVIS, writes=[INVs.r])
                    OP("dve", "tensor_tensor", _reads=[CARX.r, SELs.r], _writes=[TMPc.r], _a=dict(out=TMPc[:], in0=CARX[:], in1=SELs[:].unsqueeze(2).to_broadcast([128, 128, 16]), op=ALU.mult))
                    OP("dve", "tensor_reduce", _reads=[TMPc.r], _writes=[CREF.r], _a=dict(out=CREF[:], in_=TMPc[:].rearrange("p t h -> p h t"), axis=AX.X, op=ALU.add))
                    OP("dve", "tensor_tensor", _reads=[DC.r, CREF.r], _writes=[NB0.r], _a=dict(out=NB0[:], in0=CREF[:].unsqueeze(1).to_broadcast([128, 128, 16]), in1=DC[:], op=ALU.subtract))
                    OP("dve", "tensor_tensor", _reads=[NB0.r, INVs.r], _writes=[NB0.r], _a=dict(out=NB0[:], in0=NB0[:], in1=INVs[:].unsqueeze(2).to_broadcast([128, 128, 16]), op=ALU.add))
                    DCo_, CARXo, INCo = tile_cumsum(s1, LFO, LFO.r, 16, "o")
                    OP("dve", "tensor_copy", _reads=[DCo_.r], _writes=[DCO.r], _a=dict(out=DCO[:], in_=DCo_[:]))
                    OP("dve", "tensor_copy", _reads=[CARXo.r], _writes=[XQ.r], _a=dict(out=XQ[:], in_=CARXo[:]))
                    fw.barrier()
                if "foxp" not in L1:
                    return
                KH = sb(st, "KH", [128, SEQ], BF16)
                VHA = sb(st, "VHA", [128, 128, 2, 65], BF16)
                KHO = sb(st, "KHO", [128, BLK], BF16)
                VHOA = sb(st, "VHOA", [128, 16, 2, 65], BF16)
                QH = sb(st, "QH", [128, BLK], BF16)
                PT = [sb(st, f"PT{i}", [128, 512], BF16) for i in range(3)]
                OA = sb(st, "OA", [128, 512], F32)
                RDf = sb(st, "RDf", [64, 512], F32)
                ON = sb(st, "ON", [64, 512], BF16)
                OP("dve", "memset", _writes=[VHA.r], _p=(VHA[:], 1.0))
                OP("dve", "memset", _writes=[VHOA.r], _p=(VHOA[:], 1.0))
                pt_i = [0]
                for hp2 in range(int(os.environ.get("KFOXH", "8"))):
                    for q4 in range(4):
                        fw.dma("sp", KH[:, q4 * 4096:(q4 + 1) * 4096], KS[hp2, :, q4 * 4096:(q4 + 1) * 4096], reads=[R_KS], writes=[KH.r])
                    for q4 in range(8):
                        for h2 in range(2):
                            fw.dma("sp", VHA[:, q4 * 16:(q4 + 1) * 16, h2, 0:64],
                                   VS[q4 * 2048:(q4 + 1) * 2048, hp2 * 128 + h2 * 64:hp2 * 128 + h2 * 64 + 64].rearrange("(t p) d -> p t d", p=128), reads=[R_VS], writes=[VHA.r])
                    fw.dma("sp", KHO[:], KSO[hp2, :, 0:BLK], reads=[R_KSO], writes=[KHO.r])
                    for h2 in range(2):
                        fw.dma("sp", VHOA[:, :, h2, 0:64], VSO[:, hp2 * 128 + h2 * 64:hp2 * 128 + h2 * 64 + 64].rearrange("(t p) d -> p t d", p=128), reads=[R_VSO], writes=[VHOA.r])
                    fw.dma("sp", QH[:], QSO[hp2, :, 0:BLK], reads=[R_QSO], writes=[QH.r])
                    for hh in range(2):
                        hd = 2 * hp2 + hh
                        hpp = hh * 64
                        sbanks = (0, 1) if hh == 0 else (2, 3)
                        mbanks = (0, 1)
                        for qi in range(4):
                            xq = XQ[:, 4 * qi, :]
                            OP("dve", "tensor_tensor", _reads=[NB0.r, XQ.r], _writes=[NBq.r], _a=dict(out=NBq[:, :, hd:hd + 1], in0=NB0[:, :, hd:hd + 1],
                                                                                              in1=xq[:, hd:hd + 1].unsqueeze(1).to_broadcast([128, 128, 1]), op=ALU.add))
                            OP("dve", "tensor_tensor", _reads=[DCO.r, XQ.r], _writes=[NBOq.r], _a=dict(out=NBOq[:, :, hd:hd + 1], in0=xq[:, hd:hd + 1].unsqueeze(1).to_broadcast([128, 16, 1]),
                                                                                               in1=DCO[:, :, hd:hd + 1], op=ALU.subtract))
                            po = PS[4 + (qi % 2)]
                            pdn = PS[6 + (qi % 2)]
                            qs = qi * 512
                            nown = 4 * qi + 4
                            tiles = []
                            for kt in range(128 + nown):
                                own_t = kt >= 128
                                ko = kt - 128
                                d = max(0, ko - 4 * qi) if own_t else 0
                                if own_t:
                                    tiles.append(dict(c0=128 * d, lhs=KHO[hpp:hpp + 64, ko * 128:(ko + 1) * 128], kres=KHO.r, bias=NBOq[:, ko, hd:hd + 1], bres=NBOq.r,
                                                      vl=VHOA[:, ko, hh, :], vres=VHOA.r, mask=(ko >= 4 * qi)))
                                else:
                                    tiles.append(dict(c0=0, lhs=KH[hpp:hpp + 64, kt * 128:(kt + 1) * 128], kres=KH.r, bias=NBq[:, kt, hd:hd + 1], bres=NBq.r,
                                                      vl=VHA[:, kt, hh, :], vres=VHA.r, mask=False))

                            def emit_qk(tl):
                                ps = next_ps(sbanks)
                                mm(ps[:, tl["c0"]:512], tl["lhs"], QH[hpp:hpp + 64, qs + tl["c0"]:qs + 512], True, True, [tl["kres"], QH.r], ps)
                                tl["ps"] = ps
                            emit_qk(tiles[0])
                            ntl = len(tiles)
                            for ti_, tl in enumerate(tiles):
                                if ti_ + 1 < ntl:
                                    emit_qk(tiles[ti_ + 1])
                                c0 = tl["c0"]
                                ps = tl["ps"]
                                pt = PT[pt_i[0] % 3]
                                pt_i[0] += 1
                                OP("act", "activation", _reads=[ps.r, tl["bres"]], _writes=[pt.r], _a=dict(out=pt[:, c0:512], in_=ps[:, c0:512], func=AF.Exp, bias=tl["bias"], scale=0.125))
                                if tl["mask"]:
                                    OP("dve", "tensor_tensor", _reads=[pt.r, CSTB.r], _writes=[pt.r], _a=dict(out=pt[:, c0:c0 + 128], in0=pt[:, c0:c0 + 128], in1=TRI_B, op=ALU.mult))
                                mm(po[0:64, c0:512], tl["vl"][:, 0:64], pt[:, c0:512], ti_ == 0, ti_ == ntl - 1, [tl["vres"], pt.r], po)
                                mm(pdn[0:64, c0:512], ONES_B[:, 0:64], pt[:, c0:512], ti_ == 0, ti_ == ntl - 1, [CSTB.r, pt.r], pdn)
                            OP("dve", "reciprocal", _reads=[pdn.r], _writes=[RDf.r], _a=dict(out=RDf[:], in_=pdn[0:64, :]))
                            if hh == 0:
                                OP("dve", "tensor_tensor", _reads=[po.r, RDf.r], _writes=[XB.r], _a=dict(out=XB[0:64, hp2, qs:qs + 512], in0=po[0:64, :], in1=RDf[:], op=ALU.mult))
                            else:
                                OP("dve", "tensor_tensor", _reads=[po.r, RDf.r], _writes=[ON.r], _a=dict(out=ON[:], in0=po[0:64, :], in1=RDf[:], op=ALU.mult))
                                psh = next_ps(mbanks)
                                mm(psh[64:128, :], CSTB[0:64, 256:320], ON[:], True, True, [CSTB.r, ON.r], psh)
                                OP("act", "activation", _reads=[psh.r], _writes=[XB.r], _a=dict(out=XB[64:128, hp2, qs:qs + 512], in_=psh[64:128, :], func=AF.Copy))
                fw.barrier()

        def fox_sample():
            with contextlib.ExitStack() as st:
                LFC = sb(st, "LFC", [128, 32, 16], F32)
                NBc = sb(st, "NBc", [128, 32, 16], F32)
                NBn = sb(st, "NBn", [16, 16], F32)
                KC_ = sb(st, "KCc", [128, PAST], BF16)
                VC_ = sb(st, "VCc", [128, 32, 128], BF16)
                KN = sb(st, "KN", [128, NS], BF16)
                VN = sb(st, "VN2", [16, 4, D], BF16)
                QS_ = sb(st, "QS_", [128, NS], BF16)
                TMPs = sb(st, "TMPs2", [128, 32, 16], F32)
                PTs = sb(st, "PTs", [128, 33, 16], BF16)
                TN = sb(st, "TN", [16, 16], F32)
                RDs = sb(st, "RDs2", [128, 16], F32)
                fw.dma("sp", VN[:], VSNd, reads=[R_VSNd], writes=[VN.r])
                for s_ in range(4):
                    with contextlib.ExitStack() as s1:
                        fw.dma("sp", LFC[:], cc_lf[s_].rearrange("(t p) h -> p t h", p=128), writes=[LFC.r])
                        DCc, CARXc, INCc = tile_cumsum(s1, LFC, LFC.r, 32, f"c{s_}")
                        OP("dve", "tensor_tensor", _reads=[DCc.r, INCc.r], _writes=[NBc.r], _a=dict(out=NBc[:], in0=INCc[:, 31, :].unsqueeze(1).to_broadcast([128, 32, 16]), in1=DCc[:], op=ALU.subtract))
                        pn = next_ps()
                        mm(pn[0:16, 0:16], TRI_F[0:16, 0:16], LFN[0:16, s_, :], True, True, [CST.r, LFN.r], pn)
                        OP("dve", "tensor_scalar", _reads=[pn.r], _writes=[NBn.r], _a=dict(out=NBn[:], in0=pn[0:16, 0:16], scalar1=-1.0, scalar2=None, op0=ALU.mult))
                        fw.barrier()
                    for ch in range(int(os.environ.get("KFOXH", "8"))):
                        fw.dma("pool", KC_[:], cc_k[s_, ch * 128:(ch + 1) * 128, :], writes=[KC_.r], max_dma_last_dim=8192)
                        fw.dma("pool", VC_[:], cc_v[s_, :, ch * 128:(ch + 1) * 128].rearrange("(t p) c -> p t c", p=128), writes=[VC_.r])
                        fw.dma("sp", KN[:], KSO[ch, :, BLK:NT], reads=[R_KSO], writes=[KN.r])
                        fw.dma("sp", QS_[:], QSO[ch, :, BLK:NT], reads=[R_QSO], writes=[QS_.r])
                        for hh in range(2):
                            hd = 2 * ch + hh
                            hpp = hh * 64
                            sbanks = (0, 1) if hh == 0 else (2, 3)
                            ps = next_ps(sbanks)
                            q_ap = QS_[hpp:hpp + 64, 16 * s_:16 * s_ + 16]
                            for t in range(32):
                                mm(ps[:, t * 16:(t + 1) * 16], KC_[hpp:hpp + 64, t * 128:(t + 1) * 128], q_ap, True, True, [KC_.r, QS_.r], ps)
                            psn = next_ps(sbanks)
                            mm(psn[0:16, 0:16], KN[hpp:hpp + 64, 16 * s_:16 * s_ + 16], q_ap, True, True, [KN.r, QS_.r], psn)
                            OP("dve", "scalar_tensor_tensor", _reads=[ps.r, NBc.r], _writes=[TMPs.r], _a=dict(out=TMPs[:], in0=ps[:].rearrange("p (t q) -> p t q", q=16), scalar=0.125,
                                                                                              in1=NBc[:, :, hd:hd + 1].to_broadcast([128, 32, 16]), op0=ALU.mult, op1=ALU.add))
                            OP("act", "activation", _reads=[TMPs.r], _writes=[PTs.r], _a=dict(out=PTs[:, 0:32, :], in_=TMPs[:], func=AF.Exp))
                            OP("dve", "scalar_tensor_tensor", _reads=[psn.r, NBn.r], _writes=[TN.r], _a=dict(out=TN[:], in0=psn[0:16, 0:16], scalar=0.125,
                                                                                             in1=NBn[:, hd:hd + 1].to_broadcast([16, 16]), op0=ALU.mult, op1=ALU.add))
                            OP("act", "activation", _reads=[TN.r], _writes=[TN.r], _a=dict(out=TN[:], in_=TN[:], func=AF.Exp))
                            OP("dve", "tensor_tensor", _reads=[TN.r, CST.r, PTs.r], _writes=[PTs.r], _a=dict(out=PTs[0:16, 32, :], in0=TN[:], in1=TRI_F[0:16, 0:16], op=ALU.mult))
                            po = next_ps((4, 5))
                            pd = next_ps((6, 7))
                            for t in range(33):
                                lv = VC_[:, t, hpp:hpp + 64] if t < 32 else VN[0:16, s_, hd * 64:(hd + 1) * 64]
                                rp = PTs[:, t, :] if t < 32 else PTs[0:16, 32, :]
                                mm(po[hpp:hpp + 64, 0:16], lv, rp, t == 0, t == 32, [VC_.r, VN.r, PTs.r], po)
                            for t in range(33):
                                lo_ = ONES_B[:, 0:64] if t < 32 else CSTB[0:16, 0:64]
                                rp = PTs[:, t, :] if t < 32 else PTs[0:16, 32, :]
                                mm(pd[hpp:hpp + 64, 0:16], lo_, rp, t == 0, t == 32, [CSTB.r, PTs.r], pd)
                            OP("dve", "reciprocal", _reads=[pd.r], _writes=[RDs.r], _a=dict(out=RDs[hpp:hpp + 64, :], in_=pd[hpp:hpp + 64, 0:16]))
                            OP("dve", "tensor_tensor", _reads=[po.r, RDs.r], _writes=[XB.r], _a=dict(out=XB[hpp:hpp + 64, ch, BLK + 16 * s_:BLK + 16 * s_ + 16], in0=po[hpp:hpp + 64, 0:16],
                                                                                          in1=RDs[hpp:hpp + 64, :], op=ALU.mult))
                fw.barrier()

        def moe(XRES):
            with contextlib.ExitStack() as st:
                LG = sb(st, "LG", [128, 17, 8], F32)
                CMB = sb(st, "CMB", [128, 17, 8], F32)
                WR = sb(st, "WR", [128, 8, 8], F32)
                BRR = sb(st, "BRR", [128, 8], F32)
                M1 = sb(st, "M1", [128, 17], F32)
                M2 = sb(st, "M2", [128, 17], F32)
                T8 = sb(st, "T8", [128, 17, 8], F32)
                E8 = sb(st, "E8", [128, 17, 8], F32)
                GATE = sb(st, "GATE", [128, NT], BF16)
                DG = [sb(st, f"DG{i}", [128, 128], F32) for i in range(2)]
                fw.dma("sp", WR[:], w_router.rearrange("(k p) e -> p k e", p=128), writes=[WR.r])
                fw.dma("sp", BRR[:], br_rep, writes=[BRR.r])
                OP("dve", "memset", _writes=[LG.r], _p=(LG[:], 0.0))
                for t in range(17):
                    tw = 128 if t < 16 else NS
                    ps = next_ps()
                    for kc in range(8):
                        mm(ps[0:tw, 0:8], XRES[:, kc, t * 128:t * 128 + tw], WR[:, kc, :], kc == 0, kc == 7, [XRES.r, WR.r], ps)
                    OP("dve", "tensor_tensor", _reads=[ps.r, BRR.r], _writes=[LG.r], _a=dict(out=LG[0:tw, t, :], in0=ps[0:tw, 0:8], in1=BRR[0:tw, :], op=ALU.add))
                OP("dve", "tensor_reduce", _reads=[LG.r], _writes=[M1.r], _a=dict(out=M1[:], in_=LG[:], axis=AX.X, op=ALU.max))
                OP("dve", "tensor_tensor", _reads=[LG.r, M1.r], _writes=[T8.r], _a=dict(out=T8[:], in0=LG[:], in1=M1[:].unsqueeze(2).to_broadcast([128, 17, 8]), op=ALU.is_equal))
                OP("dve", "scalar_tensor_tensor", _reads=[T8.r, LG.r], _writes=[T8.r], _a=dict(out=T8[:], in0=T8[:], scalar=-1e30, in1=LG[:], op0=ALU.mult, op1=ALU.add))
                OP("dve", "tensor_reduce", _reads=[T8.r], _writes=[M2.r], _a=dict(out=M2[:], in_=T8[:], axis=AX.X, op=ALU.max))
                OP("dve", "tensor_tensor", _reads=[LG.r, M2.r], _writes=[T8.r], _a=dict(out=T8[:], in0=LG[:], in1=M2[:].unsqueeze(2).to_broadcast([128, 17, 8]), op=ALU.is_ge))
                OP("dve", "tensor_tensor", _reads=[LG.r, M1.r], _writes=[E8.r], _a=dict(out=E8[:], in0=LG[:], in1=M1[:].unsqueeze(2).to_broadcast([128, 17, 8]), op=ALU.subtract))
                OP("act", "activation", _reads=[E8.r], _writes=[E8.r], _a=dict(out=E8[:], in_=E8[:], func=AF.Exp))
                OP("dve", "tensor_tensor", _reads=[E8.r, T8.r], _writes=[E8.r], _a=dict(out=E8[:], in0=E8[:], in1=T8[:], op=ALU.mult))
                OP("dve", "tensor_reduce", _reads=[E8.r], _writes=[M2.r], _a=dict(out=M2[:], in_=E8[:], axis=AX.X, op=ALU.add))
                OP("dve", "reciprocal", _reads=[M2.r], _writes=[M2.r], _a=dict(out=M2[:], in_=M2[:]))
                OP("dve", "tensor_tensor", _reads=[E8.r, M2.r], _writes=[CMB.r], _a=dict(out=CMB[:], in0=E8[:], in1=M2[:].unsqueeze(2).to_broadcast([128, 17, 8]), op=ALU.mult))
                for kc in range(8):
                    OP("act", "activation", _reads=[XRES.r], _writes=[XRES.r], _a=dict(out=XRES[:, kc, :], in_=XRES[:, kc, :], func=AF.Identity, scale=ALPHA))
                global_gate = GATE
                for e in range(int(os.environ.get("KEXP", "8"))):
                    dg_i = 0
                    for t in range(17):
                        tw = 128 if t < 16 else NS
                        dg = DG[dg_i % 2]
                        dg_i += 1
                        OP("dve", "tensor_scalar", _reads=[CST.r, CMB.r], _writes=[dg.r], _a=dict(out=dg[0:tw, 0:tw], in0=IDENT_F[0:tw, 0:tw], scalar1=CMB[0:tw, t, e:e + 1], scalar2=None, op0=ALU.mult))
                        ps = next_ps()
                        mm(ps[:, 0:tw], ONES_F[0:tw, :], dg[0:tw, 0:tw], True, True, [CST.r, dg.r], ps)
                        OP("act", "activation", _reads=[ps.r], _writes=[GATE.r], _a=dict(out=GATE[:, t * 128:t * 128 + tw], in_=ps[:, 0:tw], func=AF.Copy))
                    ffn(XRES, moe_w1[e], moe_w3[e], moe_w2[e], gate=GATE)
                fw.barrier()

        L1 = set(os.environ.get("KL1", "foxp,foxs,rest").split(","))
        if "own" in PASSES and "l1" in PARTS:
            if "foxp" in L1 or "cum" in L1:
                fox_prompt()
            if "foxs" in L1:
                fox_sample()
            with contextlib.suppress(SkipBlock), contextlib.ExitStack() as sC:
                if "rest" not in L1:
                    raise SkipBlock()
                XRES = sb(sC, "XRES1", [128, 8, NT], F32)
                fw.dma("sp", XRES[:], X1S, reads=[R_X1S], writes=[XRES.r])
                linear_fm(w_out_c, 8, 0, D, lambda kc, c0, cw: XB[:, kc, c0:c0 + cw], [XB.r], COLT, resid_evac(XRES))
                fw.barrier()
                layernorm(XRES, 3)
                cross_attention(XRES, 1, True)
                layernorm(XRES, 4)
                moe(XRES)
                layernorm(XRES, 5)
                fw.dma("sp", o_yT.rearrange("(k p) n -> p k n", p=128), XRES[:], reads=[XRES.r], writes=[R_OUT["yT"]])
                fw.barrier()

        fw.barrier()
        for s_ in fw.sems.values():
            nc.gpsimd.sem_clear(s_)
        nc.all_engine_barrier()
        with nc.Block() as block:
            fw.emit(block)
        nc.all_engine_barrier()
        for s_ in fw.sems.values():
            nc.gpsimd.sem_clear(s_)
    return nc


def _host_inputs(inp):
    f = lambda a: np.ascontiguousarray(a, dtype=np.float32)
    xp = inp["x_prompt"][0]
    xT = np.zeros((D, HALO + SEQ), np.float32)
    xT[:, HALO:] = xp.T
    rel = inp["rel_bias_a"][0]
    kk = np.arange(128)[:, None]
    qq = np.arange(128)[None, :]
    BT = np.zeros((128, 5, 8, 128), np.float32)
    for j in range(5):
        kpos = 128 * j + kk
        qpos = 512 + qq
        relidx = np.clip(qpos - kpos, -128, 128) + 128
        cq = qpos // 64
        ck = kpos // 64
        vis = (ck >= cq - 8) & (ck <= cq)
        for h in range(8):
            BT[:, j, (h % 2) * 4 + h // 2, :] = np.where(vis, rel[h][relidx], NEG)
    BTS = np.full((128, 5, 8, 16), NEG, np.float32)
    for j in range(5):
        nk = 128 if j < 4 else 16
        kpos = 128 * j + np.arange(nk)[:, None]
        qpos = 512 + np.arange(16)[None, :]
        relidx = np.clip(qpos - kpos, -128, 128) + 128
        for h in range(8):
            BTS[:nk, j, (h % 2) * 4 + h // 2, :] = rel[h][relidx]
    ones = np.ones((128, 128), np.float32)
    tri = (np.arange(128)[:, None] <= np.arange(128)[None, :]).astype(np.float32)
    ident = np.eye(128, dtype=np.float32)
    sel65 = np.zeros((128, 128), np.float32)
    sel65[64, :] = 1.0
    CONST = np.concatenate([ones, tri, ident, sel65], axis=1)
    lng = f(inp["ln_g"].reshape(2, 3, 8, 128).transpose(3, 0, 1, 2).reshape(128, 48))
    lnb = f(inp["ln_b"].reshape(2, 3, 8, 128).transpose(3, 0, 1, 2).reshape(128, 48))
    common = {
        "xT": xT, "memT": f(inp["mem_prompt"][0].T), "w_in_ab": f(inp["w_in_ab"][0]),
        "BT": f(BT.reshape(128, -1)), "BTS": f(BTS.reshape(128, -1)), "pool_w": f(inp["pool_w"][0]),
        "pool_sc": f(inp["pool_scale"][0].reshape(4, 128).T), "w_out_ab": f(inp["w_out_ab"][0]),
        "w_in_c": f(inp["w_in_c"][0]), "bf_rep": f(np.broadcast_to(inp["b_f"][0][None, :], (128, 16))),
        "w_out_c": f(inp["w_out_c"][0]), "w_xq": f(inp["w_xq"]), "w_xk": f(inp["w_xk"]), "w_xv": f(inp["w_xv"]),
        "w_xo": f(inp["w_xo"]), "ln_g": lng, "ln_b": lnb, "ffn_w1": f(inp["ffn_w1"][0]), "ffn_w3": f(inp["ffn_w3"][0]),
        "ffn_w2": f(inp["ffn_w2"][0]), "w_router": f(inp["w_router"][0]),
        "br_rep": f(np.broadcast_to(inp["b_router"][0][None, :], (128, 8))),
        "moe_w1": f(inp["moe_w1"][0]), "moe_w3": f(inp["moe_w3"][0]), "moe_w2": f(inp["moe_w2"][0]), "CONST": CONST,
    }
    maps = []
    for c in range(NCORES):
        m = dict(common)
        sq = slice(4 * c, 4 * c + 4)
        xo = np.zeros((D, NA), np.float32)
        xo[:, 0:HALO + BLK] = xT[:, c * BLK:c * BLK + HALO + BLK]
        xo[:, HALO + BLK:] = inp["x_sample"][sq].reshape(NS, D).T
        m["xo"] = xo
        m["ca_k"] = f(inp["cache_a_k"][0, sq].reshape(4, 512, 512).transpose(0, 2, 1))
        m["ca_v"] = f(inp["cache_a_v"][0, sq].reshape(4, 512, 512))
        m["sp_T"] = f(inp["state_pool"][0, sq].transpose(0, 2, 1))
        m["cc_k"] = f(inp["cache_c_k"][0, sq].reshape(4, PAST, 1024).transpose(0, 2, 1))
        m["cc_v"] = f(inp["cache_c_v"][0, sq].reshape(4, PAST, 1024))
        m["cc_lf"] = f(inp["cache_c_logf"][0, sq])
        m["cm_k"] = f(inp["cache_mem_k"][:, sq].reshape(2, 4, 256, 1024).transpose(0, 1, 3, 2))
        m["cm_v"] = f(inp["cache_mem_v"][:, sq].reshape(2, 4, 256, 1024))
        valt = np.ones((9, 128, 21), np.float32)
        valt[0, :, 0:4] = 0.0
        if c == 0:
            valt[8, :, 0:4] = 0.0
        m["VALT"] = valt
        fix = np.zeros((9, 128, 4, 16), np.float32)
        for g, w in enumerate((2, 4, 8, 16)):
            fix[:, :, g, :] = 1.0 / w
            first = 1.0 / np.minimum(float(w), np.arange(16) + 1.0)
            fix[0, :, g, :] = first[None, :]
            if c == 0:
                fix[8, :, g, :] = first[None, :]
        m["FIX"] = fix.reshape(9, 128, 64)
        invis = np.zeros((128, 128), np.float32)
        invis[:, 16 * c:] = NEG
        m["INVIS"] = invis
        selt = np.zeros((128, 128), np.float32)
        selt[:, 16 * c] = 1.0
        m["SELT"] = selt
        maps.append(m)
    return maps


_NC_CACHE = {}


def kernel(**inputs):
    inp = {k: np.asarray(v) for k, v in inputs.items()}
    if "nc" not in _NC_CACHE:
        _NC_CACHE["nc"] = build_program()
    nc = _NC_CACHE["nc"]
    maps = _host_inputs(inp)
    res = run_bass_kernel_spmd(nc, maps, core_ids=list(range(NCORES)))
    R = res.results
    yT = np.stack([R[c]["yT"] for c in range(NCORES)])
    y_prompt = np.ascontiguousarray(yT[:, :, :BLK].transpose(0, 2, 1).reshape(1, SEQ, D))
    y_sample = np.ascontiguousarray(yT[:, :, BLK:].transpose(0, 2, 1).reshape(32, 16, D))
    r0 = R[0]
    p_a_k = np.ascontiguousarray(r0["p_a_kT"].T).reshape(1, 1, 512, 8, 64)
    p_a_v = r0["p_a_v"].reshape(1, 1, 512, 8, 64)
    p_pool = np.ascontiguousarray(r0["p_poolT"].T).reshape(1, 1, 15, 512)
    p_c_k = np.ascontiguousarray(r0["p_c_kT"].T).reshape(1, 1, SEQ, 16, 64)
    p_c_v = r0["p_c_v"].reshape(1, 1, SEQ, 16, 64)
    p_c_lf = r0["p_c_lf"].reshape(1, 1, SEQ, 16)
    p_mem_k = np.ascontiguousarray(r0["p_mem_kT"].transpose(0, 2, 1)).reshape(2, 1, 256, 4, 256)
    p_mem_v = r0["p_mem_v"].reshape(2, 1, 256, 4, 256)
    s_a_k = np.concatenate([R[c]["s_a_kT"].T for c in range(NCORES)], 0).reshape(1, 32, 16, 8, 64)
    s_a_v = np.concatenate([R[c]["s_a_v"] for c in range(NCORES)], 0).reshape(1, 32, 16, 8, 64)
    s_pool = np.concatenate([R[c]["s_poolT"].transpose(0, 2, 1) for c in range(NCORES)], 0).reshape(1, 32, 15, 512)
    s_c_k = np.concatenate([R[c]["s_c_kT"].T for c in range(NCORES)], 0).reshape(1, 32, 16, 16, 64)
    s_c_v = np.concatenate([R[c]["s_c_v"] for c in range(NCORES)], 0).reshape(1, 32, 16, 16, 64)
    s_c_lf = np.concatenate([R[c]["s_c_lf"] for c in range(NCORES)], 0).reshape(1, 32, 16, 16)
    outs = (y_prompt, y_sample, p_a_k, p_a_v, p_pool, p_c_k, p_c_v, p_c_lf, p_mem_k, p_mem_v,
            s_a_k, s_a_v, s_pool, s_c_k, s_c_v, s_c_lf)
    return tuple(np.ascontiguousarray(o, dtype=np.float32) for o in outs)
```

```python
import contextlib
import numpy as np
import concourse.bass as bass
import concourse.mybir as mybir
from concourse.bass_utils import run_bass_kernel_spmd

F32 = mybir.dt.float32
BF16 = mybir.dt.bfloat16
ALU = mybir.AluOpType
AF = mybir.ActivationFunctionType
AX = mybir.AxisListType

NCORES = 8
D = 1024
SEQ = 16384
BLK = 2048
NBLK = 8
NS = 64
NT = BLK + NS
HALO = 512
NA = HALO + NT
DFF = 2816
ALPHA = 4.0 ** 0.25
LN_EPS = 1e-5
NEG = -30000.0
PAST = 4096
COLT = [(0, 512), (512, 512), (1024, 512), (1536, 512), (2048, 64)]
COLA = [(0, 512), (512, 512), (1024, 512), (1536, 512), (2048, 512), (2560, 64)]
STAGE = 1
import os
PARTS = set(os.environ.get("KPARTS", "mem,pool,band,sband,outproj,ln1,xattn,ln2,ffn,ln3,kv,l1").split(","))
PASSES = os.environ.get("KPASSES", "0,1,2,3,4,5,6,7,own").split(",")


class Res:
    __slots__ = ("name", "w", "r", "excl")

    def __init__(self, name, excl=False):
        self.name = name
        self.w = None
        self.r = {}
        self.excl = excl


class SkipBlock(Exception):
    pass


class Eng:
    def __init__(self, name, sem):
        self.name = name
        self.sem = sem
        self.count = 0
        self.seen = {}
        self.ops = []
        self.dma_sems = []
        self.dma_rr = 0


class FW:
    def __init__(self, nc, stack, ndma=8):
        self.nc = nc
        self.engs = {}
        self.sems = {}
        for name in ("pe", "act", "dve", "pool", "sp"):
            s = stack.enter_context(nc.semaphore("prog_" + name))
            self.sems[id(s)] = s
            self.engs[name] = Eng(name, s)
        for name in ("sp", "pool"):
            e = self.engs[name]
            for i in range(ndma):
                s = stack.enter_context(nc.semaphore(f"dma_{name}_{i}"))
                self.sems[id(s)] = s
                e.dma_sems.append([s, 0])

    def _deps(self, eng, reads, writes, skip_self_waw=False):
        need = {}

        def add(tok):
            if tok is None:
                return
            k, v = tok
            if need.get(k, 0) < v:
                need[k] = v
        for r in reads:
            add(r.w)
            if r.excl:
                for k, v in r.r.items():
                    if k != id(eng.sem):
                        add((k, v))
        for w in writes:
            if not (skip_self_waw and w.w is not None and w.w[0] == id(eng.sem)):
                add(w.w)
            for k, v in w.r.items():
                add((k, v))
        waits = []
        for k, v in need.items():
            if eng.seen.get(k, 0) < v:
                eng.seen[k] = v
                waits.append((self.sems[k], v))
        return waits

    def _commit(self, tok, reads, writes):
        for w in writes:
            w.w = tok
            w.r = {}
        for r in reads:
            if r.r.get(tok[0], 0) < tok[1]:
                r.r[tok[0]] = tok[1]

    def op(self, engname, fn, reads=(), writes=(), accum=False):
        eng = self.engs[engname]
        waits = self._deps(eng, reads, writes, skip_self_waw=accum)
        eng.count += 1
        tok = (id(eng.sem), eng.count)
        eng.ops.append((waits, fn, (eng.sem, 1)))
        self._commit(tok, reads, writes)
        return tok

    def dma(self, engname, out, in_, reads=(), writes=(), **kw):
        eng = self.engs[engname]
        waits = self._deps(eng, reads, writes)
        slot = eng.dma_sems[eng.dma_rr % len(eng.dma_sems)]
        eng.dma_rr += 1
        sem, cnt = slot
        if cnt > 0 and eng.seen.get(id(sem), 0) < cnt:
            eng.seen[id(sem)] = cnt
            waits.append((sem, cnt))
        slot[1] = cnt + 16
        tok = (id(sem), cnt + 16)

        def fn(h, out=out, in_=in_, kw=kw):
            return h.dma_start(out=out, in_=in_, **kw)
        eng.ops.append((waits, fn, (sem, 16)))
        self._commit(tok, reads, writes)
        return tok

    def barrier(self):
        targets = []
        for e in self.engs.values():
            if e.count:
                targets.append((id(e.sem), e.count))
            for s, c in e.dma_sems:
                if c:
                    targets.append((id(s), c))
        for e in self.engs.values():
            waits = []
            for k, v in targets:
                if e.seen.get(k, 0) < v:
                    e.seen[k] = v
                    waits.append((self.sems[k], v))
            if waits:
                e.ops.append((waits, None, None))

    def emit(self, block):
        handles = {"pe": "tensor", "act": "scalar", "dve": "vector", "pool": "gpsimd", "sp": "sync"}
        for name, eng in self.engs.items():
            def body(h, eng=eng):
                for waits, fn, inc in eng.ops:
                    for (s, v) in waits:
                        h.wait_ge(s, v)
                    if fn is not None:
                        if isinstance(fn, tuple):
                            name_, args_, kw_ = fn
                            ins = getattr(h, name_)(*args_, **kw_)
                        else:
                            ins = fn(h)
                        ins.then_inc(inc[0], inc[1])
            getattr(block, handles[name])(body)


class T:
    def __init__(self, t, name, excl=False):
        self.t = t
        self.r = Res(name, excl)

    def __getitem__(self, k):
        return self.t[k]


def build_program():
    nc = bass.Bass("TRN2", target_bir_lowering=False)
    IN = {}
    OUT = {}

    def din(name, shape, dt=F32):
        IN[name] = nc.dram_tensor(name, list(shape), dt, kind="ExternalInput").ap()
        return IN[name]

    def dout(name, shape, dt=F32):
        OUT[name] = nc.dram_tensor(name, list(shape), dt, kind="ExternalOutput").ap()
        return OUT[name]

    xT = din("xT", [D, HALO + SEQ])
    xo = din("xo", [D, NA])
    ca_k = din("ca_k", [4, 512, 512])
    ca_v = din("ca_v", [4, 512, 512])
    sp_T = din("sp_T", [4, 512, 15])
    cc_k = din("cc_k", [4, 1024, PAST])
    cc_v = din("cc_v", [4, PAST, 1024])
    cc_lf = din("cc_lf", [4, PAST, 16])
    cm_k = din("cm_k", [2, 4, 1024, 256])
    cm_v = din("cm_v", [2, 4, 256, 1024])
    memT = din("memT", [D, 256])
    w_in_ab = din("w_in_ab", [D, 2048])
    BTd = din("BT", [128, 5 * 8 * 128])
    BTSd = din("BTS", [128, 5 * 8 * 16])
    pool_w = din("pool_w", [4, 128, 128])
    pool_sc = din("pool_sc", [128, 4])
    w_out_ab = din("w_out_ab", [D, D])
    w_in_c = din("w_in_c", [D, 3088])
    bf_rep = din("bf_rep", [128, 16])
    w_out_c = din("w_out_c", [D, D])
    w_xq = din("w_xq", [2, D, D])
    w_xk = din("w_xk", [2, D, D])
    w_xv = din("w_xv", [2, D, D])
    w_xo = din("w_xo", [2, D, D])
    ln_g = din("ln_g", [128, 48])
    ln_b = din("ln_b", [128, 48])
    ffn_w1 = din("ffn_w1", [D, DFF])
    ffn_w3 = din("ffn_w3", [D, DFF])
    ffn_w2 = din("ffn_w2", [DFF, D])
    w_router = din("w_router", [D, 8])
    br_rep = din("br_rep", [128, 8])
    moe_w1 = din("moe_w1", [8, D, DFF])
    moe_w3 = din("moe_w3", [8, D, DFF])
    moe_w2 = din("moe_w2", [8, DFF, D])
    VALT = din("VALT", [9, 128, 21])
    FIXd = din("FIX", [9, 128, 64])
    INVIS = din("INVIS", [128, 128])
    SELT = din("SELT", [128, 128])
    CONST = din("CONST", [128, 4 * 128])

    o_yT = dout("yT", [D, NT])
    o_pak = dout("p_a_kT", [512, 512])
    o_pav = dout("p_a_v", [512, 512])
    o_ppool = dout("p_poolT", [512, 15])
    o_pck = dout("p_c_kT", [D, SEQ])
    o_pcv = dout("p_c_v", [SEQ, D])
    o_pclf = dout("p_c_lf", [SEQ, 16])
    o_pmk = dout("p_mem_kT", [2, D, 256])
    o_pmv = dout("p_mem_v", [2, 256, D])
    o_sak = dout("s_a_kT", [512, NS])
    o_sav = dout("s_a_v", [NS, 512])
    o_spool = dout("s_poolT", [4, 512, 15])
    o_sck = dout("s_c_kT", [D, NS])
    o_scv = dout("s_c_v", [NS, D])
    o_sclf = dout("s_c_lf", [NS, 16])
    R_OUT = {k: Res("out_" + k) for k in OUT}

    KS = nc.dram_tensor("KS", [8, 128, SEQ], BF16).ap()
    VS = nc.dram_tensor("VS", [SEQ, D], BF16).ap()
    KSO = nc.dram_tensor("KSO", [8, 128, NT], BF16).ap()
    VSO = nc.dram_tensor("VSO", [BLK, D], BF16).ap()
    QSO = nc.dram_tensor("QSO", [8, 128, NT], BF16).ap()
    X1S = nc.dram_tensor("X1S", [128, 8, NT], F32).ap()
    MKD = nc.dram_tensor("MKD", [2, 128, 8, 256], BF16).ap()
    MVD = nc.dram_tensor("MVD", [2, 128, 2, 1024], BF16).ap()
    R_MKD, R_MVD = Res("MKD"), Res("MVD")
    VSNd = nc.dram_tensor("VSNd", [16, 4, D], BF16).ap()
    R_VSNd = Res("VSNd")
    R_KS, R_VS, R_KSO, R_VSO, R_QSO, R_X1S = (Res(n) for n in ("KS", "VS", "KSO", "VSO", "QSO", "X1S"))

    with contextlib.ExitStack() as top:
        fw = FW(nc, top)

        uid = [0]

        def sb(stack, name, shape, dt):
            uid[0] += 1
            nm = f"sb{uid[0]}_{name}"
            return T(stack.enter_context(nc.sbuf_tensor(nm, list(shape), dt)), nm)

        PS = [T(top.enter_context(nc.psum_tensor(f"ps{i}", [128, 512], F32)), f"ps{i}", excl=True) for i in range(8)]
        ps_rr = [0]

        def next_ps(pool=(0, 1, 2, 3, 4, 5, 6, 7)):
            p = PS[pool[ps_rr[0] % len(pool)]]
            ps_rr[0] += 1
            return p

        XB = sb(top, "XB", [128, 8, NT], BF16)
        WB = [sb(top, f"WB{i}", [128, 4096], BF16) for i in range(3)]
        wb_rr = [0]

        def next_wb():
            w = WB[wb_rr[0] % len(WB)]
            wb_rr[0] += 1
            return w
        CST = sb(top, "CST", [128, 512], F32)
        CSTB = sb(top, "CSTB", [128, 512], BF16)
        LNG = sb(top, "LNG", [128, 48], F32)
        LNB = sb(top, "LNB", [128, 48], F32)
        BFR = sb(top, "BFR", [128, 16], F32)
        PSC = sb(top, "PSC", [128, 4], F32)
        PW = sb(top, "PW", [128, 4, 128], BF16)
        LF = sb(top, "LF", [128, 128, 16], F32)
        LFO = sb(top, "LFO", [128, 16, 16], F32)
        LFN = sb(top, "LFN", [16, 4, 16], F32)

        TOUCH = sb(top, "TOUCH", [1, 8 * 64], F32)
        KDBG = int(os.environ.get("KDBG", "99"))
        for ti_, (nm_, ap_) in enumerate(IN.items()):
            if KDBG < 1:
                break
            a_ = ap_
            while a_.ndim > 2:
                a_ = a_[0]
            fw.dma("sp", TOUCH[0:1, ti_ * 8:ti_ * 8 + 4], a_[0:1, 0:4], writes=[TOUCH.r])
        fw.dma("sp", CST[:], CONST, writes=[CST.r])
        fw.dma("pool", CSTB[:], CONST, writes=[CSTB.r])
        fw.dma("sp", LNG[:], ln_g, writes=[LNG.r])
        fw.dma("sp", LNB[:], ln_b, writes=[LNB.r])
        fw.dma("sp", BFR[:], bf_rep, writes=[BFR.r])
        fw.dma("sp", PSC[:], pool_sc, writes=[PSC.r])
        fw.dma("pool", PW[:], pool_w.rearrange("g c e -> c g e"), writes=[PW.r])
        fw.op("dve", ("memset", (LF[:], 0.0), {}), writes=[LF.r])
        ONES_B = CSTB[:, 0:128]
        TRI_B = CSTB[:, 128:256]
        ONES_F = CST[:, 0:128]
        TRI_F = CST[:, 128:256]
        IDENT_F = CST[:, 256:384]

        def OP(eng, name, _reads=(), _writes=(), _accum=False, _a=None, _p=()):
            fw.op(eng, (name, tuple(_p), dict(_a or {})), reads=_reads, writes=_writes, accum=_accum)

        def mm(ps_ap, lhsT, rhs, start, stop, reads, psT):
            fw.op("pe", ("matmul", (ps_ap, lhsT, rhs), dict(start=start, stop=stop)), reads=reads, writes=[psT.r], accum=True)

        def wload(dst_ap, src_ap, wbT, eng="pool"):
            fw.dma(eng, dst_ap, src_ap, writes=[wbT.r])

        def linear_fm(Wap, KC, o0, n_out, rhs_fn, rhs_res, col_tiles, evac_fn, kpart=128):
            gw_max = (4096 // KC) // 128 * 128
            og = 0
            while og < n_out:
                gw = min(gw_max, n_out - og)
                wb = next_wb()
                wv = wb[0:kpart, 0:KC * gw].rearrange("p (k n) -> p k n", n=gw)
                wload(wv, Wap[:, o0 + og:o0 + og + gw].rearrange("(k p) n -> p k n", p=kpart), wb)
                for oc in range(0, gw, 128):
                    m = min(128, gw - oc)
                    for (c0, cw) in col_tiles:
                        if KDBG < 3:
                            continue
                        ps = next_ps()
                        for kc in range(KC):
                            mm(ps[0:m, 0:cw], wv[:, kc, oc:oc + m], rhs_fn(kc, c0, cw), kc == 0, kc == KC - 1,
                               [wb.r] + rhs_res, ps)
                        if KDBG >= 4:
                            evac_fn(o0 + og + oc, m, c0, cw, ps)
                og += gw

        def linear_tm(Wap, KC, o0, n_out, lhsT_fn, lhs_res, tok_tiles, evac_fn):
            og = 0
            while og < n_out:
                gw = min(512, n_out - og)
                wb = next_wb()
                wv = wb[:, 0:KC * gw].rearrange("p (k n) -> p k n", n=gw)
                wload(wv, Wap[:, o0 + og:o0 + og + gw].rearrange("(k p) n -> p k n", p=128), wb)
                for ti, (t0, tw) in enumerate(tok_tiles):
                    if KDBG < 3:
                        continue
                    ps = next_ps()
                    for kc in range(KC):
                        mm(ps[0:tw, 0:gw], lhsT_fn(kc, t0, tw), wv[:, kc, 0:gw], kc == 0, kc == KC - 1,
                           [wb.r] + lhs_res, ps)
                    if KDBG >= 4:
                        evac_fn(ti, t0, tw, og, gw, ps)
                og += gw

        def layernorm(XRES, lidx):
            with contextlib.ExitStack() as st:
                CB = sb(st, "ln_cb", [128, 8, 512], BF16)
                SQ = sb(st, "ln_sq", [128, 8, 512], BF16)
                LM = sb(st, "ln_m", [128, 512], F32)
                LV = sb(st, "ln_v", [128, 512], F32)
                LR = sb(st, "ln_r", [128, 512], F32)
                T0 = sb(st, "ln_t0", [128, 512], F32)
                T1 = sb(st, "ln_t1", [128, 512], F32)
                TT = [T0, T1]
                for (c0, cw) in COLT:
                    for kc in range(8):
                        OP("act", "activation", _reads=[XRES.r], _writes=[CB.r], _a=dict(out=CB[:, kc, 0:cw], in_=XRES[:, kc, c0:c0 + cw], func=AF.Copy))
                        OP("act", "activation", _reads=[XRES.r], _writes=[SQ.r], _a=dict(out=SQ[:, kc, 0:cw], in_=XRES[:, kc, c0:c0 + cw], func=AF.Square))
                    p1 = next_ps()
                    p2 = next_ps()
                    for kc in range(8):
                        mm(p1[:, 0:cw], ONES_B, CB[:, kc, 0:cw], kc == 0, kc == 7, [CB.r, CSTB.r], p1)
                    for kc in range(8):
                        mm(p2[:, 0:cw], ONES_B, SQ[:, kc, 0:cw], kc == 0, kc == 7, [SQ.r, CSTB.r], p2)
                    OP("dve", "tensor_scalar", _reads=[p1.r], _writes=[LM.r], _a=dict(out=LM[:, 0:cw], in0=p1[:, 0:cw], scalar1=1.0 / D, scalar2=None, op0=ALU.mult))
                    OP("dve", "tensor_tensor", _reads=[LM.r], _writes=[LV.r], _a=dict(out=LV[:, 0:cw], in0=LM[:, 0:cw], in1=LM[:, 0:cw], op=ALU.mult))
                    OP("dve", "scalar_tensor_tensor", _reads=[p2.r, LV.r], _writes=[LV.r], _a=dict(out=LV[:, 0:cw], in0=p2[:, 0:cw], scalar=1.0 / D, in1=LV[:, 0:cw],
                                                                   op0=ALU.mult, op1=ALU.subtract))
                    OP("dve", "tensor_scalar", _reads=[LV.r], _writes=[LV.r], _a=dict(out=LV[:, 0:cw], in0=LV[:, 0:cw], scalar1=0.0, scalar2=LN_EPS, op0=ALU.max, op1=ALU.add))
                    OP("act", "activation", _reads=[LV.r], _writes=[LV.r], _a=dict(out=LV[:, 0:cw], in_=LV[:, 0:cw], func=AF.Sqrt))
                    OP("dve", "reciprocal", _reads=[LV.r], _writes=[LR.r], _a=dict(out=LR[:, 0:cw], in_=LV[:, 0:cw]))
                    for kc in range(8):
                        tt = TT[kc % 2]
                        gi = lidx * 8 + kc
                        OP("dve", "tensor_tensor", _reads=[XRES.r, LM.r], _writes=[tt.r], _a=dict(out=tt[:, 0:cw], in0=XRES[:, kc, c0:c0 + cw], in1=LM[:, 0:cw], op=ALU.subtract))
                        OP("dve", "tensor_tensor", _reads=[tt.r, LR.r], _writes=[tt.r], _a=dict(out=tt[:, 0:cw], in0=tt[:, 0:cw], in1=LR[:, 0:cw], op=ALU.mult))
                        OP("dve", "tensor_scalar", _reads=[tt.r, LNG.r, LNB.r], _writes=[XRES.r], _a=dict(out=XRES[:, kc, c0:c0 + cw], in0=tt[:, 0:cw],
                                                                                 scalar1=LNG[:, gi:gi + 1], scalar2=LNB[:, gi:gi + 1],
                                                                                 op0=ALU.mult, op1=ALU.add))
                        OP("act", "activation", _reads=[tt.r, LNG.r, LNB.r], _writes=[XB.r], _a=dict(out=XB[:, kc, c0:c0 + cw], in_=tt[:, 0:cw], func=AF.Identity,
                                                                              scale=LNG[:, gi:gi + 1], bias=LNB[:, gi:gi + 1]))
                fw.barrier()

        def resid_evac(XRES):
            def ev(o, m, c0, cw, ps):
                kc = o // 128
                OP("dve", "scalar_tensor_tensor", _reads=[ps.r, XRES.r], _writes=[XRES.r], _a=dict(out=XRES[:, kc, c0:c0 + cw], in0=XRES[:, kc, c0:c0 + cw], scalar=ALPHA,
                                                               in1=ps[:, 0:cw], op0=ALU.mult, op1=ALU.add))
            return ev

        with contextlib.ExitStack() as st:
          if "mem" in PARTS and KDBG >= 2:
            MEMB = sb(st, "MEMB", [128, 8, 256], BF16)
            MKP = sb(st, "MKP", [128, 2, 8, 256], BF16)
            MVP = sb(st, "MVP", [128, 2, 2, 1024], BF16)
            STG = [sb(st, f"mstg{i}", [128, 512], F32) for i in range(2)]
            sg = [0]
            fw.dma("pool", MEMB[:], memT.rearrange("(k p) n -> p k n", p=128), writes=[MEMB.r])
            for l in range(2):
                def ev_k(o, m, c0, cw, ps, l=l):
                    s = STG[sg[0] % 2]
                    sg[0] += 1
                    OP("dve", "tensor_copy", _reads=[ps.r], _writes=[s.r], _a=dict(out=s[:, 0:256], in_=ps[:, 0:256]))
                    if KDBG >= 5:
                        if os.environ.get("KV") == "2":
                            OP("act", "activation", _reads=[s.r], _writes=[MKP.r], _a=dict(out=MKP[:, l, o // 128, :], in_=s[:, 0:256], func=AF.Copy))
                        else:
                            OP("act", "activation", _reads=[ps.r] + ([s.r] if os.environ.get("KV") == "3" else []), _writes=[MKP.r], _a=dict(out=MKP[:, l, o // 128, :], in_=ps[:, 0:256], func=AF.Copy))
                    if KDBG >= 6:
                        fw.dma("sp", o_pmk[l, o:o + 128, :], s[:, 0:256], reads=[s.r], writes=[R_OUT["p_mem_kT"]])
                linear_fm(w_xk[l], 8, 0, D, lambda kc, c0, cw: MEMB[:, kc, c0:c0 + cw], [MEMB.r], [(0, 256)], ev_k)

                def ev_v(ti, t0, tw, og, gw, ps, l=l):
                    s = STG[sg[0] % 2]
                    sg[0] += 1
                    OP("dve", "tensor_copy", _reads=[ps.r], _writes=[s.r], _a=dict(out=s[:, 0:gw], in_=ps[:, 0:gw]))
                    if KDBG >= 5:
                        if os.environ.get("KV") == "2":
                            OP("act", "activation", _reads=[s.r], _writes=[MVP.r], _a=dict(out=MVP[:, l, ti, og:og + gw], in_=s[:, 0:gw], func=AF.Copy))
                        else:
                            OP("act", "activation", _reads=[ps.r] + ([s.r] if os.environ.get("KV") == "3" else []), _writes=[MVP.r], _a=dict(out=MVP[:, l, ti, og:og + gw], in_=ps[:, 0:gw], func=AF.Copy))
                    if KDBG >= 6:
                        fw.dma("sp", o_pmv[l, t0:t0 + tw, og:og + gw], s[:, 0:gw], reads=[s.r], writes=[R_OUT["p_mem_v"]])
                linear_tm(w_xv[l], 8, 0, D, lambda kc, t0, tw: MEMB[:, kc, t0:t0 + tw], [MEMB.r], [(0, 128), (128, 128)], ev_v)
            if KDBG >= 7:
                fw.dma("sp", MKD.rearrange("l p k m -> p l k m"), MKP[:], reads=[MKP.r], writes=[R_MKD])
                fw.dma("sp", MVD.rearrange("l p t n -> p l t n"), MVP[:], reads=[MVP.r], writes=[R_MVD])
            fw.barrier()

        def cross_attention(XRES, l, own):
            with contextlib.ExitStack() as st:
                QX = sb(st, "QX", [128, 8, NT], BF16)
                MKP = sb(st, "MKPl", [128, 8, 256], BF16)
                MVP = sb(st, "MVPl", [128, 2, 1024], BF16)
                fw.dma("sp", MKP[:], MKD[l], reads=[R_MKD], writes=[MKP.r])
                fw.dma("sp", MVP[:], MVD[l], reads=[R_MVD], writes=[MVP.r])
                PX = sb(st, "PX", [128, 2, 512], BF16)
                RX = sb(st, "RX", [128, 512], F32)

                def ev_q(o, m, c0, cw, ps):
                    eng = "act" if (o // 128) % 2 else "dve"
                    if eng == "act":
                        OP("act", "activation", _reads=[ps.r], _writes=[QX.r], _a=dict(out=QX[:, o // 128, c0:c0 + cw], in_=ps[:, 0:cw], func=AF.Copy))
                    else:
                        OP("dve", "tensor_copy", _reads=[ps.r], _writes=[QX.r], _a=dict(out=QX[:, o // 128, c0:c0 + cw], in_=ps[:, 0:cw]))
                linear_fm(w_xq[l], 8, 0, D, lambda kc, c0, cw: XB[:, kc, c0:c0 + cw], [XB.r], COLT, ev_q)

                def group(c0, cw, mk_fn, mv_fn, mres):
                    for hd in range(4):
                        for mt in range(2):
                            ps = next_ps()
                            for c in range(2):
                                mm(ps[:, 0:cw], mk_fn(2 * hd + c, mt), QX[:, 2 * hd + c, c0:c0 + cw], c == 0, c == 1, mres + [QX.r], ps)
                            OP("act", "activation", _reads=[ps.r], _writes=[PX.r], _a=dict(out=PX[:, mt, 0:cw], in_=ps[:, 0:cw], func=AF.Exp, scale=1.0 / 16.0))
                        pd = next_ps()
                        for mt in range(2):
                            mm(pd[:, 0:cw], ONES_B, PX[:, mt, 0:cw], mt == 0, mt == 1, [PX.r, CSTB.r], pd)
                        OP("dve", "reciprocal", _reads=[pd.r], _writes=[RX.r], _a=dict(out=RX[:, 0:cw], in_=pd[:, 0:cw]))
                        for c in range(2):
                            po = next_ps()
                            for mt in range(2):
                                mm(po[:, 0:cw], mv_fn(2 * hd + c, mt), PX[:, mt, 0:cw], mt == 0, mt == 1, mres + [PX.r], po)
                            OP("dve", "tensor_tensor", _reads=[po.r, RX.r], _writes=[XB.r], _a=dict(out=XB[:, 2 * hd + c, c0:c0 + cw], in0=po[:, 0:cw], in1=RX[:, 0:cw], op=ALU.mult))
                for (c0, cw) in COLT[:4]:
                    group(c0, cw, lambda ch, mt: MKP[:, ch, mt * 128:(mt + 1) * 128],
                          lambda ch, mt: MVP[:, mt, ch * 128:(ch + 1) * 128], [MKP.r, MVP.r])
                if own:
                    MKS = sb(st, "MKS", [128, 8, 256], BF16)
                    MVS = sb(st, "MVS", [128, 2, 1024], BF16)
                    for s in range(4):
                        fw.dma("pool", MKS[:], cm_k[l, s].rearrange("(k p) m -> p k m", p=128), writes=[MKS.r])
                        fw.dma("pool", MVS[:], cm_v[l, s].rearrange("(t p) n -> p t n", p=128), writes=[MVS.r])
                        group(BLK + 16 * s, 16, lambda ch, mt: MKS[:, ch, mt * 128:(mt + 1) * 128],
                              lambda ch, mt: MVS[:, mt, ch * 128:(ch + 1) * 128], [MKS.r, MVS.r])
                else:
                    OP("dve", "tensor_copy", _reads=[QX.r], _writes=[XB.r], _a=dict(out=XB[:, :, BLK:NT], in_=QX[:, :, BLK:NT]))
                fw.barrier()
            linear_fm(w_xo[l], 8, 0, D, lambda kc, c0, cw: XB[:, kc, c0:c0 + cw], [XB.r], COLT, resid_evac(XRES))
            fw.barrier()

        def ffn(XRES, w1, w3, w2, gate=None):
            TT_ = [(0, 1056), (1056, 1056)]
            with contextlib.ExitStack() as st:
                W2H = sb(st, "W2H", [128, 11, 1024], BF16)
                G = sb(st, "G", [128, 11, 1056], BF16)
                SL = [sb(st, f"SL{i}", [128, 512], BF16) for i in range(2)]
                sl = [0]
                for (t0, tn) in TT_:
                    subt = [(0, 512), (512, 512), (1024, 32)]
                    for hf in range(2):
                        f0 = hf * 11
                        for g4 in range(0, 11, 4):
                            n4 = min(4, 11 - g4)
                            gw = n4 * 128
                            wb1 = next_wb()
                            wb3 = next_wb()
                            v1 = wb1[:, 0:8 * gw].rearrange("p (k n) -> p k n", n=gw)
                            v3 = wb3[:, 0:8 * gw].rearrange("p (k n) -> p k n", n=gw)
                            cc0 = (f0 + g4) * 128
                            wload(v1, w1[:, cc0:cc0 + gw].rearrange("(k p) n -> p k n", p=128), wb1)
                            wload(v3, w3[:, cc0:cc0 + gw].rearrange("(k p) n -> p k n", p=128), wb3)
                            if g4 == 0:
                                for g2 in range(0, 11, 4):
                                    n2 = min(4, 11 - g2)
                                    fw.dma("pool", W2H[:, g2:g2 + n2, :], w2[(f0 + g2) * 128:(f0 + g2 + n2) * 128, :].rearrange("(k p) n -> p k n", p=128),
                                           writes=[W2H.r])
                            for j in range(n4):
                                for (s0, sw) in subt:
                                    pa = next_ps()
                                    pb = next_ps()
                                    for kc in range(8):
                                        mm(pa[:, 0:sw], v1[:, kc, j * 128:(j + 1) * 128], XB[:, kc, t0 + s0:t0 + s0 + sw], kc == 0, kc == 7, [wb1.r, XB.r], pa)
                                    for kc in range(8):
                                        mm(pb[:, 0:sw], v3[:, kc, j * 128:(j + 1) * 128], XB[:, kc, t0 + s0:t0 + s0 + sw], kc == 0, kc == 7, [wb3.r, XB.r], pb)
                                    s_ = SL[sl[0] % 2]
                                    sl[0] += 1
                                    OP("act", "activation", _reads=[pa.r], _writes=[s_.r], _a=dict(out=s_[:, 0:sw], in_=pa[:, 0:sw], func=AF.Silu))
                                    if gate is not None:
                                        OP("pool", "tensor_tensor", _reads=[s_.r, gate.r], _writes=[s_.r], _a=dict(out=s_[:, 0:sw], in0=s_[:, 0:sw], in1=gate[:, t0 + s0:t0 + s0 + sw], op=ALU.mult))
                                    OP("dve", "tensor_tensor", _reads=[s_.r, pb.r], _writes=[G.r], _a=dict(out=G[:, g4 + j, s0:s0 + sw], in0=s_[:, 0:sw], in1=pb[:, 0:sw], op=ALU.mult))
                        for oc in range(8):
                            for (s0, sw) in subt:
                                py = next_ps()
                                for f in range(11):
                                    mm(py[:, 0:sw], W2H[:, f, oc * 128:(oc + 1) * 128], G[:, f, s0:s0 + sw], f == 0, f == 10, [W2H.r, G.r], py)
                                OP("dve", "tensor_tensor", _reads=[py.r, XRES.r], _writes=[XRES.r], _a=dict(out=XRES[:, oc, t0 + s0:t0 + s0 + sw], in0=XRES[:, oc, t0 + s0:t0 + s0 + sw],
                                                                                  in1=py[:, 0:sw], op=ALU.add))
                fw.barrier()
        gate_res = None

        def layer0_pass(b, own):
            pi = 8 if own else b
            with contextlib.ExitStack() as sA:
                XBA = sb(sA, "XBA", [128, 8, NA], BF16)
                if own:
                    fw.dma("pool", XBA[:], xo.rearrange("(k p) n -> p k n", p=128), writes=[XBA.r])
                else:
                    fw.dma("pool", XBA[:, :, 0:HALO + BLK], xT[:, b * BLK:b * BLK + HALO + BLK].rearrange("(k p) n -> p k n", p=128), writes=[XBA.r])
                    fw.dma("pool", XBA[:, :, HALO + BLK:NA], xo[:, HALO + BLK:NA].rearrange("(k p) n -> p k n", p=128), writes=[XBA.r])
                with contextlib.suppress(SkipBlock), contextlib.ExitStack() as s1:
                    if "pool" not in PARTS:
                        raise SkipBlock()
                    UP = sb(s1, "UP", [128, 4, 16 + BLK], F32)
                    UH = sb(s1, "UH", [128, 4, 4, 32], F32)
                    TA = sb(s1, "TA", [128, 16 + BLK], F32)
                    TB = sb(s1, "TB", [128, 16 + BLK], F32)
                    DD = sb(s1, "DD", [128, 4, NT], BF16)
                    FX = sb(s1, "FX", [128, 64], F32)
                    fw.dma("sp", FX[:], FIXd[pi], writes=[FX.r])
                    OP("dve", "memset", _writes=[UH.r], _p=(UH[:], 0.0,))
                    if own:
                        for s in range(4):
                            fw.dma("sp", UH[:, :, s, 1:16], sp_T[s].rearrange("(g p) t -> p g t", p=128), writes=[UH.r])
                    ucols = [(496, 512), (1008, 512), (1520, 512), (2032, 512), (2544, 80)]

                    def ev_u(o, m, c0, cw, ps):
                        g = (o - 1536) // 128
                        if c0 < 2544:
                            OP("act", "activation", _reads=[ps.r], _writes=[UP.r], _a=dict(out=UP[:, g, c0 - 496:c0 - 496 + cw], in_=ps[:, 0:cw], func=AF.Copy))
                        else:
                            OP("act", "activation", _reads=[ps.r], _writes=[UP.r], _a=dict(out=UP[:, g, 2048:2064], in_=ps[:, 0:16], func=AF.Copy))
                            OP("dve", "tensor_copy", _reads=[ps.r], _writes=[UH.r], _a=dict(out=UH[:, g, :, 16:32], in_=ps[:, 16:80].rearrange("p (s t) -> p s t", t=16)))
                    linear_fm(w_in_ab, 8, 1536, 512, lambda kc, c0, cw: XBA[:, kc, c0:c0 + cw], [XBA.r], ucols, ev_u)
                    if own:
                        for s in range(4):
                            fw.dma("sp", o_spool[s].rearrange("(g p) t -> p g t", p=128), UH[:, :, s, 17:32], reads=[UH.r], writes=[R_OUT["s_poolT"]])
                    if (not own) and b == NBLK - 1:
                        fw.dma("sp", o_ppool.rearrange("(g p) t -> p g t", p=128), UP[:, :, 16 + BLK - 15:16 + BLK], reads=[UP.r], writes=[R_OUT["p_poolT"]])
                    for g in range(4):
                        w = 2 << g
                        L = 16 + BLK
                        src = UP[:, g, :]
                        srcr = UP.r
                        bufs = [TA, TB]
                        step = 1
                        k = 0
                        lo = 0
                        while step < w:
                            dst = bufs[k % 2]
                            lo += step
                            OP("dve", "tensor_tensor", _reads=[srcr], _writes=[dst.r], _a=dict(out=dst[:, lo:L], in0=src[:, lo:L], in1=src[:, lo - step:L - step], op=ALU.add))
                            src = dst[:, :]
                            srcr = dst.r
                            step *= 2
                            k += 1
                        win = src
                        winr = srcr
                        other = bufs[k % 2]
                        OP("dve", "scalar_tensor_tensor", _reads=[winr, UP.r], _writes=[other.r], _a=dict(out=other[:, 16:L], in0=win[:, 16:L], scalar=1.0 / w, in1=UP[:, g, 16:L],
                                                                                            op0=ALU.mult, op1=ALU.subtract))
                        OP("dve", "tensor_tensor", _reads=[winr, FX.r], _writes=[winr], _a=dict(out=win[:, 16:32], in0=win[:, 16:32], in1=FX[:, g * 16:(g + 1) * 16], op=ALU.mult))
                        OP("dve", "tensor_tensor", _reads=[winr, UP.r, other.r], _writes=[other.r], _a=dict(out=other[:, 16:32], in0=win[:, 16:32], in1=UP[:, g, 16:32], op=ALU.subtract))
                        OP("act", "activation", _reads=[other.r], _writes=[DD.r], _a=dict(out=DD[:, g, 0:BLK], in_=other[:, 16:L], func=AF.Copy))
                        SA = TA[:, 0:128].rearrange("p (s t) -> p s t", t=32)
                        SB_ = TB[:, 0:128].rearrange("p (s t) -> p s t", t=32)
                        srcs = UH[:, g, :, :]
                        srcr = UH.r
                        sbufs = [(SA, TA.r), (SB_, TB.r)]
                        step = 1
                        k = 0
                        lo = 0
                        while step < w:
                            dst, dstr = sbufs[k % 2]
                            lo += step
                            OP("dve", "tensor_tensor", _reads=[srcr], _writes=[dstr], _a=dict(out=dst[:, :, lo:32], in0=srcs[:, :, lo:32], in1=srcs[:, :, lo - step:32 - step], op=ALU.add))
                            srcs = dst
                            srcr = dstr
                            step *= 2
                            k += 1
                        odst, odstr = sbufs[k % 2]
                        OP("dve", "scalar_tensor_tensor", _reads=[srcr, UH.r], _writes=[odstr], _a=dict(out=odst[:, :, 16:32], in0=srcs[:, :, 16:32], scalar=1.0 / w, in1=UH[:, g, :, 16:32],
                                                                                            op0=ALU.mult, op1=ALU.subtract))
                        OP("act", "activation", _reads=[odstr], _writes=[DD.r], _a=dict(out=DD[:, g, BLK:NT].rearrange("p (s t) -> p s t", t=16), in_=odst[:, :, 16:32], func=AF.Copy))
                        for (c0, cw) in COLT:
                            ps = next_ps()
                            mm(ps[:, 0:cw], PW[:, g, :], DD[:, g, c0:c0 + cw], True, True, [PW.r, DD.r], ps)
                            OP("act", "activation", _reads=[ps.r, PSC.r], _writes=[XB.r], _a=dict(out=XB[:, 4 + g, c0:c0 + cw], in_=ps[:, 0:cw], func=AF.Identity, scale=PSC[:, g:g + 1]))
                    fw.barrier()
                fw.barrier()
            with contextlib.ExitStack() as s2:
                QF = sb(s2, "QF", [128, 4, NT], BF16)
                KF = sb(s2, "KF", [128, 4, NA], BF16)
                VT = sb(s2, "VT", [128, 21, 512], BF16)
                VAL = sb(s2, "VAL", [128, 21, 64], BF16)
                VLT = sb(s2, "VLT", [128, 21], F32)
                ST32 = [sb(s2, f"st32_{i}", [128, 512], F32) for i in range(2)]
                stc = [0]
                if own:
                    VSN = sb(s2, "VSN", [16, 4, 512], BF16)
                    VSF = sb(s2, "VSF", [16, 4, 512], F32)
                sX = contextlib.ExitStack()
                XBA = sb(sX, "XBA2", [128, 8, NA], BF16)
                if own:
                    fw.dma("pool", XBA[:], xo.rearrange("(k p) n -> p k n", p=128), writes=[XBA.r])
                else:
                    fw.dma("pool", XBA[:, :, 0:HALO + BLK], xT[:, b * BLK:b * BLK + HALO + BLK].rearrange("(k p) n -> p k n", p=128), writes=[XBA.r])
                    fw.dma("pool", XBA[:, :, HALO + BLK:NA], xo[:, HALO + BLK:NA].rearrange("(k p) n -> p k n", p=128), writes=[XBA.r])
                fw.dma("sp", VLT[:], VALT[pi], writes=[VLT.r])
                OP("dve", "tensor_copy", _reads=[VLT.r], _writes=[VAL.r], _a=dict(out=VAL[:], in_=VLT[:].unsqueeze(2).to_broadcast([128, 21, 64])))

                def ev_q(o, m, c0, cw, ps):
                    OP("act", "activation", _reads=[ps.r], _writes=[QF.r], _a=dict(out=QF[:, o // 128, c0 - HALO:c0 - HALO + cw], in_=ps[:, 0:cw], func=AF.Copy))
                linear_fm(w_in_ab, 8, 0, 512, lambda kc, c0, cw: XBA[:, kc, c0:c0 + cw], [XBA.r], [(HALO + c, w_) for (c, w_) in COLT], ev_q)

                def ev_k(o, m, c0, cw, ps):
                    ch = (o - 512) // 128
                    OP("dve", "tensor_copy", _reads=[ps.r], _writes=[KF.r], _a=dict(out=KF[:, ch, c0:c0 + cw], in_=ps[:, 0:cw]))
                    if (not own) and b == NBLK - 1 and c0 == 2048:
                        s = ST32[stc[0] % 2]
                        stc[0] += 1
                        OP("act", "activation", _reads=[ps.r], _writes=[s.r], _a=dict(out=s[:, 0:512], in_=ps[:, 0:512], func=AF.Copy))
                        fw.dma("sp", o_pak[ch * 128:(ch + 1) * 128, :], s[:, 0:512], reads=[s.r], writes=[R_OUT["p_a_kT"]])
                    if own and c0 == 2560:
                        s = ST32[stc[0] % 2]
                        stc[0] += 1
                        OP("act", "activation", _reads=[ps.r], _writes=[s.r], _a=dict(out=s[:, 0:64], in_=ps[:, 0:64], func=AF.Copy))
                        fw.dma("sp", o_sak[ch * 128:(ch + 1) * 128, :], s[:, 0:64], reads=[s.r], writes=[R_OUT["s_a_kT"]])
                linear_fm(w_in_ab, 8, 512, 512, lambda kc, c0, cw: XBA[:, kc, c0:c0 + cw], [XBA.r], COLA, ev_k)

                tokt = [(t * 128, 128) for t in range(20)]

                def ev_v(ti, t0, tw, og, gw, ps):
                    OP("act", "activation", _reads=[ps.r], _writes=[VT.r], _a=dict(out=VT[:, ti, :], in_=ps[:, 0:512], func=AF.Copy))
                    if (not own) and b == NBLK - 1 and 16 <= ti < 20:
                        s = ST32[stc[0] % 2]
                        stc[0] += 1
                        OP("dve", "tensor_copy", _reads=[ps.r], _writes=[s.r], _a=dict(out=s[:, 0:512], in_=ps[:, 0:512]))
                        fw.dma("sp", o_pav[(ti - 16) * 128:(ti - 15) * 128, :], s[:, 0:512], reads=[s.r], writes=[R_OUT["p_a_v"]])
                linear_tm(w_in_ab, 8, 1024, 512, lambda kc, t0, tw: XBA[:, kc, t0:t0 + tw], [XBA.r], tokt, ev_v)
                if own:
                    wbv = next_wb()
                    wvv = wbv[:, 0:4096].rearrange("p (k n) -> p k n", n=512)
                    wload(wvv, w_in_ab[:, 1024:1536].rearrange("(k p) n -> p k n", p=128), wbv)
                    for s in range(4):
                        ps = next_ps()
                        for kc in range(8):
                            mm(ps[0:16, 0:512], XBA[:, kc, HALO + BLK + 16 * s:HALO + BLK + 16 * s + 16], wvv[:, kc, :], kc == 0, kc == 7, [XBA.r, wbv.r], ps)
                        OP("act", "activation", _reads=[ps.r], _writes=[VSN.r], _a=dict(out=VSN[:, s, :], in_=ps[0:16, 0:512], func=AF.Copy))
                        OP("dve", "tensor_copy", _reads=[ps.r], _writes=[VSF.r], _a=dict(out=VSF[:, s, :], in_=ps[0:16, 0:512]))
                    fw.dma("sp", o_sav.rearrange("(s t) n -> t s n", t=16), VSF[:], reads=[VSF.r], writes=[R_OUT["s_a_v"]])
                fw.barrier()
                sX.close()

                with contextlib.suppress(SkipBlock), contextlib.ExitStack() as s3:
                    if "band" not in PARTS:
                        raise SkipBlock()
                    BT = sb(s3, "BT", [128, 5, 8, 128], F32)
                    PB = sb(s3, "PB", [128, 5, 8, 128], BF16)
                    TMP = [sb(s3, f"batmp{i}", [128, 512], F32) for i in range(2)]
                    RD = sb(s3, "bard", [128, 512], F32)
                    tc_ = [0]
                    fw.dma("sp", BT[:], BTd.rearrange("p (j h q) -> p j h q", j=5, h=8), writes=[BT.r])
                    for i in range(16):
                        qc0 = i * 128
                        for j in range(5):
                            kt = i + j
                            for hg in range(2):
                                ps = next_ps((0, 1, 2, 3))
                                for hh in range(4):
                                    hd = 2 * hh + hg
                                    hp = (hd % 2) * 64
                                    mm(ps[:, hh * 128:(hh + 1) * 128], KF[hp:hp + 64, hd // 2, kt * 128:(kt + 1) * 128], QF[hp:hp + 64, hd // 2, qc0:qc0 + 128],
                                       True, True, [KF.r, QF.r], ps)
                                tm = TMP[tc_[0] % 2]
                                tc_[0] += 1
                                OP("dve", "scalar_tensor_tensor", _reads=[ps.r, BT.r], _writes=[tm.r], _a=dict(
                                    out=tm[:].rearrange("p (a q) -> p a q", q=128), in0=ps[:].rearrange("p (a q) -> p a q", q=128), scalar=0.125,
                                    in1=BT[:, j, hg * 4:(hg + 1) * 4, :], op0=ALU.mult, op1=ALU.add))
                                OP("act", "activation", _reads=[tm.r], _writes=[PB.r], _a=dict(out=PB[:, j, hg * 4:(hg + 1) * 4, :], in_=tm[:].rearrange("p (a q) -> p a q", q=128), func=AF.Exp))
                        po = next_ps((4, 5))
                        pd = next_ps((6, 7))
                        for hd in range(8):
                            hp = (hd % 2) * 64
                            cs = (hd // 2) * 128
                            for j in range(5):
                                mm(po[hp:hp + 64, cs:cs + 128], VT[:, i + j, hd * 64:(hd + 1) * 64], PB[:, j, (hd % 2) * 4 + hd // 2, :], j == 0, j == 4, [VT.r, PB.r], po)
                            for j in range(5):
                                mm(pd[hp:hp + 64, cs:cs + 128], VAL[:, i + j, :], PB[:, j, (hd % 2) * 4 + hd // 2, :], j == 0, j == 4, [VAL.r, PB.r], pd)
                        OP("dve", "reciprocal", _reads=[pd.r], _writes=[RD.r], _a=dict(out=RD[:], in_=pd[:]))
                        OP("dve", "tensor_tensor", _reads=[po.r, RD.r], _writes=[XB.r], _a=dict(out=XB[:, 0:4, qc0:qc0 + 128], in0=po[:].rearrange("p (a q) -> p a q", q=128),
                                                                             in1=RD[:].rearrange("p (a q) -> p a q", q=128), op=ALU.mult))
                    fw.barrier()
                if own:
                    with contextlib.suppress(SkipBlock), contextlib.ExitStack() as s3:
                        if "sband" not in PARTS:
                            raise SkipBlock()
                        KCA = sb(s3, "KCA", [128, 4, 512], BF16)
                        VCA = sb(s3, "VCA", [128, 4, 512], BF16)
                        BTS = sb(s3, "BTS", [128, 5, 8, 16], F32)
                        PSB = sb(s3, "PSB", [128, 5, 8, 16], BF16)
                        TMPS = sb(s3, "tmps", [128, 128], F32)
                        RDS = sb(s3, "rds", [128, 64], F32)
                        fw.dma("sp", BTS[:], BTSd.rearrange("p (j h q) -> p j h q", j=5, h=8), writes=[BTS.r])
                        OP("dve", "memset", _writes=[PSB.r], _p=(PSB[:], 0.0,))
                        for s in range(4):
                            fw.dma("pool", KCA[:], ca_k[s].rearrange("(k p) n -> p k n", p=128), writes=[KCA.r])
                            fw.dma("pool", VCA[:], ca_v[s].rearrange("(t p) n -> p t n", p=128), writes=[VCA.r])
                            qc0 = BLK + 16 * s
                            kn0 = HALO + BLK + 16 * s
                            for j in range(5):
                                kp = 128 if j < 4 else 16
                                for hg in range(2):
                                    ps = next_ps((0, 1, 2, 3))
                                    for hh in range(4):
                                        hd = 2 * hh + hg
                                        hp = hg * 64
                                        lhs = KCA[hp:hp + 64, hd // 2, j * 128:(j + 1) * 128] if j < 4 else KF[hp:hp + 64, hd // 2, kn0:kn0 + 16]
                                        mm(ps[0:kp, hh * 16:(hh + 1) * 16], lhs, QF[hp:hp + 64, hd // 2, qc0:qc0 + 16], True, True, [KCA.r, KF.r, QF.r], ps)
                                    OP("dve", "scalar_tensor_tensor", _reads=[ps.r, BTS.r], _writes=[TMPS.r], _a=dict(
                                        out=TMPS[0:kp, hg * 64:(hg + 1) * 64], in0=ps[0:kp, 0:64], scalar=0.125,
                                        in1=BTS[0:kp, j, hg * 4:(hg + 1) * 4, :].rearrange("p a q -> p (a q)"), op0=ALU.mult, op1=ALU.add))
                                OP("act", "activation", _reads=[TMPS.r], _writes=[PSB.r], _a=dict(out=PSB[0:kp, j, :, :].rearrange("p a q -> p (a q)"), in_=TMPS[0:kp, :], func=AF.Exp))
                            po = next_ps((4, 5))
                            pd = next_ps((6, 7))
                            for hd in range(8):
                                hp = (hd % 2) * 64
                                cs = (hd // 2) * 16
                                for j in range(5):
                                    kp = 128 if j < 4 else 16
                                    lv = VCA[:, j, hd * 64:(hd + 1) * 64] if j < 4 else VSN[0:16, s, hd * 64:(hd + 1) * 64]
                                    mm(po[hp:hp + 64, cs:cs + 16], lv, PSB[0:kp, j, (hd % 2) * 4 + hd // 2, :], j == 0, j == 4, [VCA.r, VSN.r, PSB.r], po)
                                for j in range(5):
                                    kp = 128 if j < 4 else 16
                                    mm(pd[hp:hp + 64, cs:cs + 16], CSTB[0:kp, 0:64], PSB[0:kp, j, (hd % 2) * 4 + hd // 2, :], j == 0, j == 4, [CSTB.r, PSB.r], pd)
                            OP("dve", "reciprocal", _reads=[pd.r], _writes=[RDS.r], _a=dict(out=RDS[:], in_=pd[:, 0:64]))
                            OP("dve", "tensor_tensor", _reads=[po.r, RDS.r], _writes=[XB.r], _a=dict(out=XB[:, 0:4, qc0:qc0 + 16], in0=po[:, 0:64].rearrange("p (a q) -> p a q", q=16),
                                                                                 in1=RDS[:].rearrange("p (a q) -> p a q", q=16), op=ALU.mult))
                        fw.barrier()
                else:
                    OP("dve", "tensor_copy", _reads=[QF.r], _writes=[XB.r], _a=dict(out=XB[:, 0:4, BLK:NT], in_=QF[:, :, BLK:NT]))
                fw.barrier()
            sB = contextlib.ExitStack()
            XRES = sb(sB, "XRES", [128, 8, NT], F32)
            if own:
                fw.dma("sp", XRES[:], xo[:, HALO:NA].rearrange("(k p) n -> p k n", p=128), writes=[XRES.r])
            else:
                fw.dma("sp", XRES[:, :, 0:BLK], xT[:, HALO + b * BLK:HALO + (b + 1) * BLK].rearrange("(k p) n -> p k n", p=128), writes=[XRES.r])
                fw.dma("sp", XRES[:, :, BLK:NT], xo[:, HALO + BLK:NA].rearrange("(k p) n -> p k n", p=128), writes=[XRES.r])
            if "outproj" in PARTS:
                linear_fm(w_out_ab, 8, 0, D, lambda kc, c0, cw: XB[:, kc, c0:c0 + cw], [XB.r], COLT, resid_evac(XRES))
            fw.barrier()
            if "ln1" in PARTS:
                layernorm(XRES, 0)
            if "xattn" in PARTS:
                cross_attention(XRES, 0, own)
            if "ln2" in PARTS:
                layernorm(XRES, 1)
            if "ffn" in PARTS:
                for kc in range(8):
                    OP("act", "activation", _reads=[XRES.r], _writes=[XRES.r], _a=dict(out=XRES[:, kc, :], in_=XRES[:, kc, :], func=AF.Identity, scale=ALPHA))
                ffn(XRES, ffn_w1, ffn_w3, ffn_w2)
            if "ln3" in PARTS:
                layernorm(XRES, 2)
            return sB, XRES

        XRES_holder = [None]

        def kvproj(b, own):
            with contextlib.ExitStack() as st:
                KST = [sb(st, f"kst{i}", [128, 512], BF16) for i in range(2)]
                KSF = [sb(st, f"ksf{i}", [128, 512], F32) for i in range(2)]
                VST = [sb(st, f"vst{i}", [128, 512], BF16) for i in range(2)]
                VSF_ = [sb(st, f"vsf{i}", [128, 512], F32) for i in range(2)]
                LFT = sb(st, "lft", [128, 17, 16], F32)
                c_ = [0]
                tok0 = b * BLK

                def ev_k(o, m, c0, cw, ps):
                    ch = (o - 1024) // 128
                    i = c_[0] % 2
                    c_[0] += 1
                    kb, kf = KST[i], KSF[i]
                    OP("act", "activation", _reads=[ps.r], _writes=[kb.r], _a=dict(out=kb[:, 0:cw], in_=ps[:, 0:cw], func=AF.Copy))
                    if own:
                        fw.dma("sp", KSO[ch, :, c0:c0 + cw], kb[:, 0:cw], reads=[kb.r], writes=[R_KSO])
                        if c0 == BLK:
                            OP("dve", "tensor_copy", _reads=[ps.r], _writes=[kf.r], _a=dict(out=kf[:, 0:cw], in_=ps[:, 0:cw]))
                            fw.dma("sp", o_sck[ch * 128:(ch + 1) * 128, :], kf[:, 0:cw], reads=[kf.r], writes=[R_OUT["s_c_kT"]])
                    elif c0 < BLK:
                        fw.dma("sp", KS[ch, :, tok0 + c0:tok0 + c0 + cw], kb[:, 0:cw], reads=[kb.r], writes=[R_KS])
                        OP("dve", "tensor_copy", _reads=[ps.r], _writes=[kf.r], _a=dict(out=kf[:, 0:cw], in_=ps[:, 0:cw]))
                        fw.dma("sp", o_pck[ch * 128:(ch + 1) * 128, tok0 + c0:tok0 + c0 + cw], kf[:, 0:cw], reads=[kf.r], writes=[R_OUT["p_c_kT"]])
                cols = COLT if own else COLT[:4]
                linear_fm(w_in_c, 8, 1024, 1024, lambda kc, c0, cw: XB[:, kc, c0:c0 + cw], [XB.r], cols, ev_k)

                def ev_v(ti, t0, tw, og, gw, ps):
                    i = c_[0] % 2
                    c_[0] += 1
                    vb, vf = VST[i], VSF_[i]
                    OP("act", "activation", _reads=[ps.r], _writes=[vb.r], _a=dict(out=vb[:, 0:gw], in_=ps[:, 0:gw], func=AF.Copy))
                    if own:
                        fw.dma("sp", VSO[t0:t0 + 128, og:og + gw], vb[:, 0:gw], reads=[vb.r], writes=[R_VSO])
                    else:
                        fw.dma("sp", VS[tok0 + t0:tok0 + t0 + 128, og:og + gw], vb[:, 0:gw], reads=[vb.r], writes=[R_VS])
                        OP("dve", "tensor_copy", _reads=[ps.r], _writes=[vf.r], _a=dict(out=vf[:, 0:gw], in_=ps[:, 0:gw]))
                        fw.dma("sp", o_pcv[tok0 + t0:tok0 + t0 + 128, og:og + gw], vf[:, 0:gw], reads=[vf.r], writes=[R_OUT["p_c_v"]])
                linear_tm(w_in_c, 8, 2048, 1024, lambda kc, t0, tw: XB[:, kc, t0:t0 + tw], [XB.r], [(t * 128, 128) for t in range(16)], ev_v)

                wb = next_wb()
                wv = wb[:, 0:128].rearrange("p (k n) -> p k n", n=16)
                wload(wv, w_in_c[:, 3072:3088].rearrange("(k p) n -> p k n", p=128), wb)
                ps = next_ps()
                for t in range(16):
                    for kc in range(8):
                        mm(ps[:, t * 16:(t + 1) * 16], XB[:, kc, t * 128:(t + 1) * 128], wv[:, kc, :], kc == 0, kc == 7, [XB.r, wb.r], ps)
                lfv = LFT[:, 0:16, :]
                LFD = LFO[:, :, :] if own else LF[:, b * 16:(b + 1) * 16, :]
                LFDr = LFO.r if own else LF.r
                OP("dve", "tensor_tensor", _reads=[ps.r, BFR.r], _writes=[LFT.r], _a=dict(out=lfv, in0=ps[:, 0:256].rearrange("p (t n) -> p t n", n=16),
                                                       in1=BFR[:].unsqueeze(1).to_broadcast([128, 16, 16]), op=ALU.add))
                OP("act", "activation", _reads=[LFT.r], _writes=[LFT.r], _a=dict(out=lfv, in_=lfv, func=AF.Exp, scale=-1.0))
                OP("act", "activation", _reads=[LFT.r], _writes=[LFT.r], _a=dict(out=lfv, in_=lfv, func=AF.Ln, bias=1.0))
                OP("dve", "tensor_scalar", _reads=[LFT.r], _writes=[LFDr], _a=dict(out=LFD, in0=lfv, scalar1=-1.0, scalar2=None, op0=ALU.mult))
                if not own:
                    fw.dma("sp", o_pclf[tok0:tok0 + BLK, :].rearrange("(t p) n -> p t n", p=128), LF[:, b * 16:(b + 1) * 16, :], reads=[LF.r], writes=[R_OUT["p_c_lf"]])
                else:
                    VN = sb(st, "VN", [16, 4, D], BF16)
                    VNF = sb(st, "VNF", [16, 4, D], F32)
                    psl = next_ps()
                    for s_ in range(4):
                        for kc in range(8):
                            mm(psl[0:16, s_ * 16:(s_ + 1) * 16], XB[:, kc, BLK + 16 * s_:BLK + 16 * s_ + 16], wv[:, kc, :], kc == 0, kc == 7, [XB.r, wb.r], psl)
                    lfn = LFT[0:16, 0:4, :]
                    OP("dve", "tensor_tensor", _reads=[psl.r, BFR.r], _writes=[LFT.r], _a=dict(out=lfn, in0=psl[0:16, 0:64].rearrange("p (t n) -> p t n", n=16),
                                                           in1=BFR[0:16, :].unsqueeze(1).to_broadcast([16, 4, 16]), op=ALU.add))
                    OP("act", "activation", _reads=[LFT.r], _writes=[LFT.r], _a=dict(out=lfn, in_=lfn, func=AF.Exp, scale=-1.0))
                    OP("act", "activation", _reads=[LFT.r], _writes=[LFT.r], _a=dict(out=lfn, in_=lfn, func=AF.Ln, bias=1.0))
                    OP("dve", "tensor_scalar", _reads=[LFT.r], _writes=[LFN.r], _a=dict(out=LFN[:], in0=lfn, scalar1=-1.0, scalar2=None, op0=ALU.mult))
                    fw.dma("sp", o_sclf.rearrange("(s t) n -> t s n", t=16), LFN[:], reads=[LFN.r], writes=[R_OUT["s_c_lf"]])
                    for og in range(0, D, 512):
                        wbv = next_wb()
                        wvv = wbv[:, 0:4096].rearrange("p (k n) -> p k n", n=512)
                        wload(wvv, w_in_c[:, 2048 + og:2048 + og + 512].rearrange("(k p) n -> p k n", p=128), wbv)
                        for s_ in range(4):
                            psv = next_ps()
                            for kc in range(8):
                                mm(psv[0:16, 0:512], XB[:, kc, BLK + 16 * s_:BLK + 16 * s_ + 16], wvv[:, kc, :], kc == 0, kc == 7, [XB.r, wbv.r], psv)
                            OP("act", "activation", _reads=[psv.r], _writes=[VN.r], _a=dict(out=VN[:, s_, og:og + 512], in_=psv[0:16, 0:512], func=AF.Copy))
                            OP("dve", "tensor_copy", _reads=[psv.r, VN.r], _writes=[VNF.r], _a=dict(out=VNF[:, s_, og:og + 512], in_=psv[0:16, 0:512]))
                    fw.dma("sp", o_scv.rearrange("(s t) n -> t s n", t=16), VNF[:], reads=[VNF.r], writes=[R_OUT["s_c_v"]])
                    fw.dma("sp", VSNd, VN[:], reads=[VN.r], writes=[R_VSNd])

                    def ev_q(o, m, c0, cw, ps):
                        i = c_[0] % 2
                        c_[0] += 1
                        kb = KST[i]
                        OP("act", "activation", _reads=[ps.r], _writes=[kb.r], _a=dict(out=kb[:, 0:cw], in_=ps[:, 0:cw], func=AF.Copy))
                        fw.dma("sp", QSO[o // 128, :, c0:c0 + cw], kb[:, 0:cw], reads=[kb.r], writes=[R_QSO])
                    linear_fm(w_in_c, 8, 0, 1024, lambda kc, c0, cw: XB[:, kc, c0:c0 + cw], [XB.r], COLT, ev_q)
                    fw.dma("sp", X1S, XRES_holder[0][:], reads=[XRES_holder[0].r], writes=[R_X1S])
                fw.barrier()

        for b in range(NBLK):
            if str(b) not in PASSES:
                continue
            sB, XRES = layer0_pass(b, False)
            if "kv" in PARTS:
                kvproj(b, False)
            sB.close()
            fw.barrier()
        if "own" in PASSES:
            sB, XRES = layer0_pass(0, True)
            XRES_holder[0] = XRES
            if "kv" in PARTS:
                kvproj(0, True)
            sB.close()
            fw.barrier()

        def prefix_tiles(st, TOTt, ntiles, name):
            A_ = sb(st, name + "_pa", [128, ntiles, 16], F32)
            B_ = sb(st, name + "_pb", [128, ntiles, 16], F32)
            cur = TOTt
            bufs = [A_, B_]
            k = 0
            step = 1
            while step < ntiles:
                dst = bufs[k % 2]
                OP("dve", "tensor_copy", _reads=[cur.r], _writes=[dst.r], _a=dict(out=dst[:, 0:step, :], in_=cur[:, 0:step, :]))
                OP("dve", "tensor_tensor", _reads=[cur.r], _writes=[dst.r], _a=dict(out=dst[:, step:ntiles, :], in0=cur[:, step:ntiles, :], in1=cur[:, 0:ntiles - step, :], op=ALU.add))
                cur = dst
                step *= 2
                k += 1
            return cur

        def tile_cumsum(st, LFsrc, LFres, ntiles, name, rows=128):
            TRI_ = sb(st, name + "_tri", [128, ntiles, 16], F32)
            TOT_ = sb(st, name + "_tot", [128, ntiles, 16], F32)
            for t0 in range(0, ntiles, 32):
                n = min(32, ntiles - t0)
                p1 = next_ps()
                p2 = next_ps()
                for t in range(n):
                    mm(p1[:, t * 16:(t + 1) * 16], TRI_F[0:rows, :], LFsrc[0:rows, t0 + t, :], True, True, [CST.r, LFres], p1)
                for t in range(n):
                    mm(p2[:, t * 16:(t + 1) * 16], ONES_F[0:rows, :], LFsrc[0:rows, t0 + t, :], True, True, [CST.r, LFres], p2)
                OP("dve", "tensor_copy", _reads=[p1.r], _writes=[TRI_.r], _a=dict(out=TRI_[:, t0:t0 + n, :], in_=p1[:, 0:n * 16].rearrange("p (t h) -> p t h", h=16)))
                OP("dve", "tensor_copy", _reads=[p2.r], _writes=[TOT_.r], _a=dict(out=TOT_[:, t0:t0 + n, :], in_=p2[:, 0:n * 16].rearrange("p (t h) -> p t h", h=16)))
            INC = prefix_tiles(st, TOT_, ntiles, name) if ntiles > 1 else TOT_
            CARX = sb(st, name + "_carx", [128, ntiles, 16], F32)
            OP("dve", "tensor_tensor", _reads=[INC.r, TOT_.r], _writes=[CARX.r], _a=dict(out=CARX[:], in0=INC[:], in1=TOT_[:], op=ALU.subtract))
            OP("dve", "tensor_tensor", _reads=[TRI_.r, CARX.r], _writes=[TRI_.r], _a=dict(out=TRI_[:], in0=TRI_[:], in1=CARX[:], op=ALU.add))
            return TRI_, CARX, INC

        def fox_prompt():
            with contextlib.ExitStack() as st:
                NB0 = sb(st, "NB0", [128, 128, 16], F32)
                NBq = sb(st, "NBq", [128, 128, 16], F32)
                DCO = sb(st, "DCO", [128, 16, 16], F32)
                XQ = sb(st, "XQ", [128, 16, 16], F32)
                NBOq = sb(st, "NBOq", [128, 16, 16], F32)
                with contextlib.ExitStack() as s1:
                    DC, CARX, INC = tile_cumsum(s1, LF, LF.r, 128, "g")
                    SELs = sb(s1, "SELs", [128, 128], F32)
                    INVs = sb(s1, "INVs", [128, 128], F32)
                    CREF = sb(s1, "CREF", [128, 16], F32)
                    TMPc = sb(s1, "TMPc", [128, 128, 16], F32)
                    fw.dma("sp", SELs[:], SELT, writes=[SELs.r])
                    fw.dma("sp", INVs[:], INVIS, writes=[INVs.r])
                    OP("dve", "tensor_tensor", _reads=[CARX.r, SELs.r], _writes=[TMPc.r], _a=dict(out=TMPc[:], in0=CARX[:], in1=SELs[:].unsqueeze(2).to_broadcast([128, 128, 16]), op=ALU.mult))
                    OP("dve", "tensor_reduce", _reads=[TMPc.r], _writes=[CREF.r], _a=dict(out=CREF[:], in_=TMPc[:].rearrange("p t h -> p h t"), axis=AX.X, op=ALU.add))
                    OP("dve", "tensor_tensor", _reads=[DC.r, CREF.r], _writes=[NB0.r], _a=dict(out=NB0[:], in0=CREF[:].unsqueeze(1).to_broadcast([128, 128, 16]), in1=DC[:], op=ALU.subtract))
                    OP("dve", "tensor_tensor", _reads=[NB0.r, INVs.r], _writes=[NB0.r], _a=dict(out=NB0[:], in0=NB0[:], in1=INVs[:].unsqueeze(2).to_broadcast([128, 128, 16]), op=ALU.add))
                    DCo_, CARXo, INCo = tile_cumsum(s1, LFO, LFO.r, 16, "o")
                    OP("dve", "tensor_copy", _reads=[DCo_.r], _writes=[DCO.r], _a=dict(out=DCO[:], in_=DCo_[:]))
                    OP("dve", "tensor_copy", _reads=[CARXo.r], _writes=[XQ.r], _a=dict(out=XQ[:], in_=CARXo[:]))
                    fw.barrier()
                if "foxp" not in L1:
                    return
                KH = sb(st, "KH", [128, SEQ], BF16)
                VHA = sb(st, "VHA", [128, 128, 2, 64], BF16)
                KHO = sb(st, "KHO", [128, BLK], BF16)
                VHOA = sb(st, "VHOA", [128, 16, 2, 64], BF16)
                QH = sb(st, "QH", [128, BLK], BF16)
                PT = [sb(st, f"PT{i}", [128, 512], BF16) for i in range(3)]
                OA = sb(st, "OA", [128, 512], F32)
                RDf = sb(st, "RDf", [64, 512], F32)
                ON = sb(st, "ON", [64, 512], BF16)
                pt_i = [0]
                for hp2 in range(int(os.environ.get("KFOXH", "8"))):
                    for q4 in range(4):
                        fw.dma("sp", KH[:, q4 * 4096:(q4 + 1) * 4096], KS[hp2, :, q4 * 4096:(q4 + 1) * 4096], reads=[R_KS], writes=[KH.r])
                    for q4 in range(8):
                        fw.dma("sp", VHA[:, q4 * 16:(q4 + 1) * 16, :, :].rearrange("p t h d -> p t (h d)"),
                               VS[q4 * 2048:(q4 + 1) * 2048, hp2 * 128:(hp2 + 1) * 128].rearrange("(t p) c -> p t c", p=128), reads=[R_VS], writes=[VHA.r])
                    fw.dma("sp", KHO[:], KSO[hp2, :, 0:BLK], reads=[R_KSO], writes=[KHO.r])
                    fw.dma("sp", VHOA[:].rearrange("p t h d -> p t (h d)"), VSO[:, hp2 * 128:(hp2 + 1) * 128].rearrange("(t p) c -> p t c", p=128), reads=[R_VSO], writes=[VHOA.r])
                    fw.dma("sp", QH[:], QSO[hp2, :, 0:BLK], reads=[R_QSO], writes=[QH.r])
                    for hh in range(2):
                        hd = 2 * hp2 + hh
                        hpp = hh * 64
                        sbanks = (0, 1) if hh == 0 else (2, 3)
                        mbanks = (0, 1)
                        for qi in range(4):
                            xq = XQ[:, 4 * qi, :]
                            OP("dve", "tensor_tensor", _reads=[NB0.r, XQ.r], _writes=[NBq.r], _a=dict(out=NBq[:, :, hd:hd + 1], in0=NB0[:, :, hd:hd + 1],
                                                                                              in1=xq[:, hd:hd + 1].unsqueeze(1).to_broadcast([128, 128, 1]), op=ALU.add))
                            OP("dve", "tensor_tensor", _reads=[DCO.r, XQ.r], _writes=[NBOq.r], _a=dict(out=NBOq[:, :, hd:hd + 1], in0=xq[:, hd:hd + 1].unsqueeze(1).to_broadcast([128, 16, 1]),
                                                                                               in1=DCO[:, :, hd:hd + 1], op=ALU.subtract))
                            po = PS[4 + (qi % 2)]
                            pdn = PS[6 + (qi % 2)]
                            qs = qi * 512
                            nown = 4 * qi + 4
                            tiles = []
                            for kt in range(128 + nown):
                                own_t = kt >= 128
                                ko = kt - 128
                                d = max(0, ko - 4 * qi) if own_t else 0
                                if own_t:
                                    tiles.append(dict(c0=128 * d, lhs=KHO[hpp:hpp + 64, ko * 128:(ko + 1) * 128], kres=KHO.r, bias=NBOq[:, ko, hd:hd + 1], bres=NBOq.r,
                                                      vl=VHOA[:, ko, hh, :], vres=VHOA.r, mask=(ko >= 4 * qi)))
                                else:
                                    tiles.append(dict(c0=0, lhs=KH[hpp:hpp + 64, kt * 128:(kt + 1) * 128], kres=KH.r, bias=NBq[:, kt, hd:hd + 1], bres=NBq.r,
                                                      vl=VHA[:, kt, hh, :], vres=VHA.r, mask=False))

                            def emit_qk(tl):
                                ps = next_ps(sbanks)
                                mm(ps[:, tl["c0"]:512], tl["lhs"], QH[hpp:hpp + 64, qs + tl["c0"]:qs + 512], True, True, [tl["kres"], QH.r], ps)
                                tl["ps"] = ps
                            emit_qk(tiles[0])
                            ntl = len(tiles)
                            for ti_, tl in enumerate(tiles):
                                if ti_ + 1 < ntl:
                                    emit_qk(tiles[ti_ + 1])
                                c0 = tl["c0"]
                                ps = tl["ps"]
                                pt = PT[pt_i[0] % 3]
                                pt_i[0] += 1
                                OP("act", "activation", _reads=[ps.r, tl["bres"]], _writes=[pt.r], _a=dict(out=pt[:, c0:512], in_=ps[:, c0:512], func=AF.Exp, bias=tl["bias"], scale=0.125))
                                if tl["mask"]:
                                    OP("dve", "tensor_tensor", _reads=[pt.r, CSTB.r], _writes=[pt.r], _a=dict(out=pt[:, c0:c0 + 128], in0=pt[:, c0:c0 + 128], in1=TRI_B, op=ALU.mult))
                                mm(po[0:64, c0:512], tl["vl"], pt[:, c0:512], ti_ == 0, ti_ == ntl - 1, [tl["vres"], pt.r], po)
                                mm(pdn[0:64, c0:512], ONES_B[:, 0:64], pt[:, c0:512], ti_ == 0, ti_ == ntl - 1, [CSTB.r, pt.r], pdn)
                            OP("dve", "reciprocal", _reads=[pdn.r], _writes=[RDf.r], _a=dict(out=RDf[:], in_=pdn[0:64, :]))
                            if hh == 0:
                                OP("dve", "tensor_tensor", _reads=[po.r, RDf.r], _writes=[XB.r], _a=dict(out=XB[0:64, hp2, qs:qs + 512], in0=po[0:64, :], in1=RDf[:], op=ALU.mult))
                            else:
                                OP("dve", "tensor_tensor", _reads=[po.r, RDf.r], _writes=[ON.r], _a=dict(out=ON[:], in0=po[0:64, :], in1=RDf[:], op=ALU.mult))
                                psh = next_ps(mbanks)
                                mm(psh[64:128, :], CSTB[0:64, 256:320], ON[:], True, True, [CSTB.r, ON.r], psh)
                                OP("act", "activation", _reads=[psh.r], _writes=[XB.r], _a=dict(out=XB[64:128, hp2, qs:qs + 512], in_=psh[64:128, :], func=AF.Copy))
                fw.barrier()

        def fox_sample():
            with contextlib.ExitStack() as st:
                LFC = sb(st, "LFC", [128, 32, 16], F32)
                NBc = sb(st, "NBc", [128, 32, 16], F32)
                NBn = sb(st, "NBn", [16, 16], F32)
                KC_ = sb(st, "KCc", [128, PAST], BF16)
                VC_ = sb(st, "VCc", [128, 32, 128], BF16)
                KN = sb(st, "KN", [128, NS], BF16)
                VN = sb(st, "VN2", [16, 4, D], BF16)
                QS_ = sb(st, "QS_", [128, NS], BF16)
                TMPs = sb(st, "TMPs2", [128, 32, 16], F32)
                PTs = sb(st, "PTs", [128, 33, 16], BF16)
                TN = sb(st, "TN", [16, 16], F32)
                RDs = sb(st, "RDs2", [128, 16], F32)
                fw.dma("sp", VN[:], VSNd, reads=[R_VSNd], writes=[VN.r])
                for s_ in range(4):
                    with contextlib.ExitStack() as s1:
                        fw.dma("sp", LFC[:], cc_lf[s_].rearrange("(t p) h -> p t h", p=128), writes=[LFC.r])
                        DCc, CARXc, INCc = tile_cumsum(s1, LFC, LFC.r, 32, f"c{s_}")
                        OP("dve", "tensor_tensor", _reads=[DCc.r, INCc.r], _writes=[NBc.r], _a=dict(out=NBc[:], in0=INCc[:, 31, :].unsqueeze(1).to_broadcast([128, 32, 16]), in1=DCc[:], op=ALU.subtract))
                        pn = next_ps()
                        mm(pn[0:16, 0:16], TRI_F[0:16, 0:16], LFN[0:16, s_, :], True, True, [CST.r, LFN.r], pn)
                        OP("dve", "tensor_scalar", _reads=[pn.r], _writes=[NBn.r], _a=dict(out=NBn[:], in0=pn[0:16, 0:16], scalar1=-1.0, scalar2=None, op0=ALU.mult))
                        fw.barrier()
                    for ch in range(int(os.environ.get("KFOXH", "8"))):
                        fw.dma("pool", KC_[:], cc_k[s_, ch * 128:(ch + 1) * 128, :], writes=[KC_.r], max_dma_last_dim=8192)
                        fw.dma("pool", VC_[:], cc_v[s_, :, ch * 128:(ch + 1) * 128].rearrange("(t p) c -> p t c", p=128), writes=[VC_.r])
                        fw.dma("sp", KN[:], KSO[ch, :, BLK:NT], reads=[R_KSO], writes=[KN.r])
                        fw.dma("sp", QS_[:], QSO[ch, :, BLK:NT], reads=[R_QSO], writes=[QS_.r])
                        for hh in range(2):
                            hd = 2 * ch + hh
                            hpp = hh * 64
                            sbanks = (0, 1) if hh == 0 else (2, 3)
                            ps = next_ps(sbanks)
                            q_ap = QS_[hpp:hpp + 64, 16 * s_:16 * s_ + 16]
                            for t in range(32):
                                mm(ps[:, t * 16:(t + 1) * 16], KC_[hpp:hpp + 64, t * 128:(t + 1) * 128], q_ap, True, True, [KC_.r, QS_.r], ps)
                            psn = next_ps(sbanks)
                            mm(psn[0:16, 0:16], KN[hpp:hpp + 64, 16 * s_:16 * s_ + 16], q_ap, True, True, [KN.r, QS_.r], psn)
                            OP("dve", "scalar_tensor_tensor", _reads=[ps.r, NBc.r], _writes=[TMPs.r], _a=dict(out=TMPs[:], in0=ps[:].rearrange("p (t q) -> p t q", q=16), scalar=0.125,
                                                                                              in1=NBc[:, :, hd:hd + 1].to_broadcast([128, 32, 16]), op0=ALU.mult, op1=ALU.add))
                            OP("act", "activation", _reads=[TMPs.r], _writes=[PTs.r], _a=dict(out=PTs[:, 0:32, :], in_=TMPs[:], func=AF.Exp))
                            OP("dve", "scalar_tensor_tensor", _reads=[psn.r, NBn.r], _writes=[TN.r], _a=dict(out=TN[:], in0=psn[0:16, 0:16], scalar=0.125,
                                                                                             in1=NBn[:, hd:hd + 1].to_broadcast([16, 16]), op0=ALU.mult, op1=ALU.add))
                            OP("act", "activation", _reads=[TN.r], _writes=[TN.r], _a=dict(out=TN[:], in_=TN[:], func=AF.Exp))
                            OP("dve", "tensor_tensor", _reads=[TN.r, CST.r, PTs.r], _writes=[PTs.r], _a=dict(out=PTs[0:16, 32, :], in0=TN[:], in1=TRI_F[0:16, 0:16], op=ALU.mult))
                            po = next_ps((4, 5))
                            pd = next_ps((6, 7))
                            for t in range(33):
                                lv = VC_[:, t, hpp:hpp + 64] if t < 32 else VN[0:16, s_, hd * 64:(hd + 1) * 64]
                                rp = PTs[:, t, :] if t < 32 else PTs[0:16, 32, :]
                                mm(po[hpp:hpp + 64, 0:16], lv, rp, t == 0, t == 32, [VC_.r, VN.r, PTs.r], po)
                            for t in range(33):
                                lo_ = ONES_B[:, 0:64] if t < 32 else CSTB[0:16, 0:64]
                                rp = PTs[:, t, :] if t < 32 else PTs[0:16, 32, :]
                                mm(pd[hpp:hpp + 64, 0:16], lo_, rp, t == 0, t == 32, [CSTB.r, PTs.r], pd)
                            OP("dve", "reciprocal", _reads=[pd.r], _writes=[RDs.r], _a=dict(out=RDs[hpp:hpp + 64, :], in_=pd[hpp:hpp + 64, 0:16]))
                            OP("dve", "tensor_tensor", _reads=[po.r, RDs.r], _writes=[XB.r], _a=dict(out=XB[hpp:hpp + 64, ch, BLK + 16 * s_:BLK + 16 * s_ + 16], in0=po[hpp:hpp + 64, 0:16],
                                                                                          in1=RDs[hpp:hpp + 64, :], op=ALU.mult))
                fw.barrier()

        def moe(XRES):
            with contextlib.ExitStack() as st:
                LG = sb(st, "LG", [128, 17, 8], F32)
                CMB = sb(st, "CMB", [128, 17, 8], F32)
                WR = sb(st, "WR", [128, 8, 8], F32)
                BRR = sb(st, "BRR", [128, 8], F32)
                M1 = sb(st, "M1", [128, 17], F32)
                M2 = sb(st, "M2", [128, 17], F32)
                T8 = sb(st, "T8", [128, 17, 8], F32)
                E8 = sb(st, "E8", [128, 17, 8], F32)
                GATE = sb(st, "GATE", [128, NT], BF16)
                DG = [sb(st, f"DG{i}", [128, 128], F32) for i in range(2)]
                fw.dma("sp", WR[:], w_router.rearrange("(k p) e -> p k e", p=128), writes=[WR.r])
                fw.dma("sp", BRR[:], br_rep, writes=[BRR.r])
                OP("dve", "memset", _writes=[LG.r], _p=(LG[:], 0.0))
                for t in range(17):
                    tw = 128 if t < 16 else NS
                    ps = next_ps()
                    for kc in range(8):
                        mm(ps[0:tw, 0:8], XRES[:, kc, t * 128:t * 128 + tw], WR[:, kc, :], kc == 0, kc == 7, [XRES.r, WR.r], ps)
                    OP("dve", "tensor_tensor", _reads=[ps.r, BRR.r], _writes=[LG.r], _a=dict(out=LG[0:tw, t, :], in0=ps[0:tw, 0:8], in1=BRR[0:tw, :], op=ALU.add))
                OP("dve", "tensor_reduce", _reads=[LG.r], _writes=[M1.r], _a=dict(out=M1[:], in_=LG[:], axis=AX.X, op=ALU.max))
                OP("dve", "tensor_tensor", _reads=[LG.r, M1.r], _writes=[T8.r], _a=dict(out=T8[:], in0=LG[:], in1=M1[:].unsqueeze(2).to_broadcast([128, 17, 8]), op=ALU.is_equal))
                OP("dve", "scalar_tensor_tensor", _reads=[T8.r, LG.r], _writes=[T8.r], _a=dict(out=T8[:], in0=T8[:], scalar=-1e30, in1=LG[:], op0=ALU.mult, op1=ALU.add))
                OP("dve", "tensor_reduce", _reads=[T8.r], _writes=[M2.r], _a=dict(out=M2[:], in_=T8[:], axis=AX.X, op=ALU.max))
                OP("dve", "tensor_tensor", _reads=[LG.r, M2.r], _writes=[T8.r], _a=dict(out=T8[:], in0=LG[:], in1=M2[:].unsqueeze(2).to_broadcast([128, 17, 8]), op=ALU.is_ge))
                OP("dve", "tensor_tensor", _reads=[LG.r, M1.r], _writes=[E8.r], _a=dict(out=E8[:], in0=LG[:], in1=M1[:].unsqueeze(2).to_broadcast([128, 17, 8]), op=ALU.subtract))
                OP("act", "activation", _reads=[E8.r], _writes=[E8.r], _a=dict(out=E8[:], in_=E8[:], func=AF.Exp))
                OP("dve", "tensor_tensor", _reads=[E8.r, T8.r], _writes=[E8.r], _a=dict(out=E8[:], in0=E8[:], in1=T8[:], op=ALU.mult))
                OP("dve", "tensor_reduce", _reads=[E8.r], _writes=[M2.r], _a=dict(out=M2[:], in_=E8[:], axis=AX.X, op=ALU.add))
                OP("dve", "reciprocal", _reads=[M2.r], _writes=[M2.r], _a=dict(out=M2[:], in_=M2[:]))
                OP("dve", "tensor_tensor", _reads=[E8.r, M2.r], _writes=[CMB.r], _a=dict(out=CMB[:], in0=E8[:], in1=M2[:].unsqueeze(2).to_broadcast([128, 17, 8]), op=ALU.mult))
                for kc in range(8):
                    OP("act", "activation", _reads=[XRES.r], _writes=[XRES.r], _a=dict(out=XRES[:, kc, :], in_=XRES[:, kc, :], func=AF.Identity, scale=ALPHA))
                global_gate = GATE
                for e in range(int(os.environ.get("KEXP", "8"))):
                    dg_i = 0
                    for t in range(17):
                        tw = 128 if t < 16 else NS
                        dg = DG[dg_i % 2]
                        dg_i += 1
                        OP("dve", "tensor_scalar", _reads=[CST.r, CMB.r], _writes=[dg.r], _a=dict(out=dg[0:tw, 0:tw], in0=IDENT_F[0:tw, 0:tw], scalar1=CMB[0:tw, t, e:e + 1], scalar2=None, op0=ALU.mult))
                        ps = next_ps()
                        mm(ps[:, 0:tw], ONES_F[0:tw, :], dg[0:tw, 0:tw], True, True, [CST.r, dg.r], ps)
                        OP("act", "activation", _reads=[ps.r], _writes=[GATE.r], _a=dict(out=GATE[:, t * 128:t * 128 + tw], in_=ps[:, 0:tw], func=AF.Copy))
                    ffn(XRES, moe_w1[e], moe_w3[e], moe_w2[e], gate=GATE)
                fw.barrier()

        L1 = set(os.environ.get("KL1", "foxp,foxs,rest").split(","))
        if "own" in PASSES and "l1" in PARTS:
            if "foxp" in L1 or "cum" in L1:
                fox_prompt()
            if "foxs" in L1:
                fox_sample()
            with contextlib.suppress(SkipBlock), contextlib.ExitStack() as sC:
                if "rest" not in L1:
                    raise SkipBlock()
                XRES = sb(sC, "XRES1", [128, 8, NT], F32)
                fw.dma("sp", XRES[:], X1S, reads=[R_X1S], writes=[XRES.r])
                linear_fm(w_out_c, 8, 0, D, lambda kc, c0, cw: XB[:, kc, c0:c0 + cw], [XB.r], COLT, resid_evac(XRES))
                fw.barrier()
                layernorm(XRES, 3)
                cross_attention(XRES, 1, True)
                layernorm(XRES, 4)
                moe(XRES)
                layernorm(XRES, 5)
                fw.dma("sp", o_yT.rearrange("(k p) n -> p k n", p=128), XRES[:], reads=[XRES.r], writes=[R_OUT["yT"]])
                fw.barrier()

        fw.barrier()
        for s_ in fw.sems.values():
            nc.gpsimd.sem_clear(s_)
        nc.all_engine_barrier()
        with nc.Block() as block:
            fw.emit(block)
        nc.all_engine_barrier()
        for s_ in fw.sems.values():
            nc.gpsimd.sem_clear(s_)
    return nc


def _host_inputs(inp):
    f = lambda a: np.ascontiguousarray(a, dtype=np.float32)
    xp = inp["x_prompt"][0]
    xT = np.zeros((D, HALO + SEQ), np.float32)
    xT[:, HALO:] = xp.T
    rel = inp["rel_bias_a"][0]
    kk = np.arange(128)[:, None]
    qq = np.arange(128)[None, :]
    BT = np.zeros((128, 5, 8, 128), np.float32)
    for j in range(5):
        kpos = 128 * j + kk
        qpos = 512 + qq
        relidx = np.clip(qpos - kpos, -128, 128) + 128
        cq = qpos // 64
        ck = kpos // 64
        vis = (ck >= cq - 8) & (ck <= cq)
        for h in range(8):
            BT[:, j, (h % 2) * 4 + h // 2, :] = np.where(vis, rel[h][relidx], NEG)
    BTS = np.full((128, 5, 8, 16), NEG, np.float32)
    for j in range(5):
        nk = 128 if j < 4 else 16
        kpos = 128 * j + np.arange(nk)[:, None]
        qpos = 512 + np.arange(16)[None, :]
        relidx = np.clip(qpos - kpos, -128, 128) + 128
        for h in range(8):
            BTS[:nk, j, (h % 2) * 4 + h // 2, :] = rel[h][relidx]
    ones = np.ones((128, 128), np.float32)
    tri = (np.arange(128)[:, None] <= np.arange(128)[None, :]).astype(np.float32)
    ident = np.eye(128, dtype=np.float32)
    sel65 = np.zeros((128, 128), np.float32)
    sel65[64, :] = 1.0
    CONST = np.concatenate([ones, tri, ident, sel65], axis=1)
    lng = f(inp["ln_g"].reshape(2, 3, 8, 128).transpose(3, 0, 1, 2).reshape(128, 48))
    lnb = f(inp["ln_b"].reshape(2, 3, 8, 128).transpose(3, 0, 1, 2).reshape(128, 48))
    common = {
        "xT": xT, "memT": f(inp["mem_prompt"][0].T), "w_in_ab": f(inp["w_in_ab"][0]),
        "BT": f(BT.reshape(128, -1)), "BTS": f(BTS.reshape(128, -1)), "pool_w": f(inp["pool_w"][0]),
        "pool_sc": f(inp["pool_scale"][0].reshape(4, 128).T), "w_out_ab": f(inp["w_out_ab"][0]),
        "w_in_c": f(inp["w_in_c"][0]), "bf_rep": f(np.broadcast_to(inp["b_f"][0][None, :], (128, 16))),
        "w_out_c": f(inp["w_out_c"][0]), "w_xq": f(inp["w_xq"]), "w_xk": f(inp["w_xk"]), "w_xv": f(inp["w_xv"]),
        "w_xo": f(inp["w_xo"]), "ln_g": lng, "ln_b": lnb, "ffn_w1": f(inp["ffn_w1"][0]), "ffn_w3": f(inp["ffn_w3"][0]),
        "ffn_w2": f(inp["ffn_w2"][0]), "w_router": f(inp["w_router"][0]),
        "br_rep": f(np.broadcast_to(inp["b_router"][0][None, :], (128, 8))),
        "moe_w1": f(inp["moe_w1"][0]), "moe_w3": f(inp["moe_w3"][0]), "moe_w2": f(inp["moe_w2"][0]), "CONST": CONST,
    }
    maps = []
    for c in range(NCORES):
        m = dict(common)
        sq = slice(4 * c, 4 * c + 4)
        xo = np.zeros((D, NA), np.float32)
        xo[:, 0:HALO + BLK] = xT[:, c * BLK:c * BLK + HALO + BLK]
        xo[:, HALO + BLK:] = inp["x_sample"][sq].reshape(NS, D).T
        m["xo"] = xo
        m["ca_k"] = f(inp["cache_a_k"][0, sq].reshape(4, 512, 512).transpose(0, 2, 1))
        m["ca_v"] = f(inp["cache_a_v"][0, sq].reshape(4, 512, 512))
        m["sp_T"] = f(inp["state_pool"][0, sq].transpose(0, 2, 1))
        m["cc_k"] = f(inp["cache_c_k"][0, sq].reshape(4, PAST, 1024).transpose(0, 2, 1))
        m["cc_v"] = f(inp["cache_c_v"][0, sq].reshape(4, PAST, 1024))
        m["cc_lf"] = f(inp["cache_c_logf"][0, sq])
        m["cm_k"] = f(inp["cache_mem_k"][:, sq].reshape(2, 4, 256, 1024).transpose(0, 1, 3, 2))
        m["cm_v"] = f(inp["cache_mem_v"][:, sq].reshape(2, 4, 256, 1024))
        valt = np.ones((9, 128, 21), np.float32)
        valt[0, :, 0:4] = 0.0
        if c == 0:
            valt[8, :, 0:4] = 0.0
        m["VALT"] = valt
        fix = np.zeros((9, 128, 4, 16), np.float32)
        for g, w in enumerate((2, 4, 8, 16)):
            fix[:, :, g, :] = 1.0 / w
            first = 1.0 / np.minimum(float(w), np.arange(16) + 1.0)
            fix[0, :, g, :] = first[None, :]
            if c == 0:
                fix[8, :, g, :] = first[None, :]
        m["FIX"] = fix.reshape(9, 128, 64)
        invis = np.zeros((128, 128), np.float32)
        invis[:, 16 * c:] = NEG
        m["INVIS"] = invis
        selt = np.zeros((128, 128), np.float32)
        selt[:, 16 * c] = 1.0
        m["SELT"] = selt
        maps.append(m)
    return maps


_NC_CACHE = {}


def kernel(**inputs):
    inp = {k: np.asarray(v) for k, v in inputs.items()}
    if "nc" not in _NC_CACHE:
        _NC_CACHE["nc"] = build_program()
    nc = _NC_CACHE["nc"]
    maps = _host_inputs(inp)
    res = run_bass_kernel_spmd(nc, maps, core_ids=list(range(NCORES)))
    R = res.results
    yT = np.stack([R[c]["yT"] for c in range(NCORES)])
    y_prompt = np.ascontiguousarray(yT[:, :, :BLK].transpose(0, 2, 1).reshape(1, SEQ, D))
    y_sample = np.ascontiguousarray(yT[:, :, BLK:].transpose(0, 2, 1).reshape(32, 16, D))
    r0 = R[0]
    p_a_k = np.ascontiguousarray(r0["p_a_kT"].T).reshape(1, 1, 512, 8, 64)
    p_a_v = r0["p_a_v"].reshape(1, 1, 512, 8, 64)
    p_pool = np.ascontiguousarray(r0["p_poolT"].T).reshape(1, 1, 15, 512)
    p_c_k = np.ascontiguousarray(r0["p_c_kT"].T).reshape(1, 1, SEQ, 16, 64)
    p_c_v = r0["p_c_v"].reshape(1, 1, SEQ, 16, 64)
    p_c_lf = r0["p_c_lf"].reshape(1, 1, SEQ, 16)
    p_mem_k = np.ascontiguousarray(r0["p_mem_kT"].transpose(0, 2, 1)).reshape(2, 1, 256, 4, 256)
    p_mem_v = r0["p_mem_v"].reshape(2, 1, 256, 4, 256)
    s_a_k = np.concatenate([R[c]["s_a_kT"].T for c in range(NCORES)], 0).reshape(1, 32, 16, 8, 64)
    s_a_v = np.concatenate([R[c]["s_a_v"] for c in range(NCORES)], 0).reshape(1, 32, 16, 8, 64)
    s_pool = np.concatenate([R[c]["s_poolT"].transpose(0, 2, 1) for c in range(NCORES)], 0).reshape(1, 32, 15, 512)
    s_c_k = np.concatenate([R[c]["s_c_kT"].T for c in range(NCORES)], 0).reshape(1, 32, 16, 16, 64)
    s_c_v = np.concatenate([R[c]["s_c_v"] for c in range(NCORES)], 0).reshape(1, 32, 16, 16, 64)
    s_c_lf = np.concatenate([R[c]["s_c_lf"] for c in range(NCORES)], 0).reshape(1, 32, 16, 16)
    outs = (y_prompt, y_sample, p_a_k, p_a_v, p_pool, p_c_k, p_c_v, p_c_lf, p_mem_k, p_mem_v,
            s_a_k, s_a_v, s_pool, s_c_k, s_c_v, s_c_lf)
    return tuple(np.ascontiguousarray(o, dtype=np.float32) for o in outs)
```
